# Optimizing a Trainium2 kernel written in Bass

```python
import jax
import jax.numpy as jnp
from jax import lax
import numpy as np

D_MODEL = 1024
BATCH = 8
SEQ = 4096
DEPTH = 2

HEAD_DIM = 64
EPS = 1e-6
MLA_HEADS = 8
MLA_NOPE = 64
MLA_ROPE = 32
MLA_V = 64
Q_LORA = 384
KV_LORA = 256
ROPE_THETA = 10000.0
Q_BLOCK = 128
DIL_HEADS = 8
DIL_PATTERNS = ((128, 1), (512, 4), (2048, 16))
DIL_BLOCK = max(w // d for (w, d) in DIL_PATTERNS)
MOBA_HEADS = 16
MOBA_BLOCK = 256
MOBA_TOPK = 3
MOBA_QCHUNK = 16
D_FF = 4 * D_MODEL
PLE_DIM = 256
N_EVEN = (DEPTH + 1) // 2
N_ODD = DEPTH // 2
EVEN_IN = Q_LORA + KV_LORA + MLA_ROPE + 3 * DIL_HEADS * HEAD_DIM
EVEN_MIX = MLA_HEADS * MLA_V + DIL_HEADS * HEAD_DIM
ODD_MIX = MOBA_HEADS * HEAD_DIM

kernel_name = 'hybrid_mla_dilated_moba_trunk'


def rms_norm(x, g):
    xf = x.astype(jnp.float32)
    y = xf * lax.rsqrt(jnp.mean(xf * xf, axis=-1, keepdims=True) + EPS)
    return (y * g.astype(jnp.float32)).astype(x.dtype)


def alibi_slopes(n):
    return 2.0 ** (-8.0 * jnp.arange(1, n + 1, dtype=jnp.float32) / n)


def apply_rope(x, positions):
    half = x.shape[-1] // 2
    inv_freq = ROPE_THETA ** (-jnp.arange(half, dtype=jnp.float32) / half)
    ang = positions.astype(jnp.float32)[..., None] * inv_freq
    ang = ang.reshape(ang.shape[:2] + (1,) * (x.ndim - 3) + (half,))
    cos, sin = jnp.cos(ang), jnp.sin(ang)
    xf = x.astype(jnp.float32)
    x1, x2 = xf[..., :half], xf[..., half:]
    return jnp.concatenate([x1 * cos - x2 * sin, x1 * sin + x2 * cos], axis=-1).astype(x.dtype)


def causal_attention_blocks(q, k, v, scale):
    B, S, H, Dk = q.shape
    nq = S // Q_BLOCK
    qb = q.reshape(B, nq, Q_BLOCK, H, Dk).swapaxes(0, 1)
    kpos = jnp.arange(S)

    def one(args):
        qi, i = args
        s = jnp.einsum('bqhd,bkhd->bhqk', qi, k, preferred_element_type=jnp.float32) * scale
        qpos = i * Q_BLOCK + jnp.arange(Q_BLOCK)
        s = jnp.where(kpos[None, :] <= qpos[:, None], s, -jnp.inf)
        pr = jax.nn.softmax(s, axis=-1).astype(v.dtype)
        return jnp.einsum('bhqk,bkhd->bqhd', pr, v)

    out = lax.map(one, (qb, jnp.arange(nq)))
    return out.swapaxes(0, 1).reshape(B, S, H, v.shape[-1])


def dilated_branch(q, k, v, slopes, scale, window, dil):
    B, S, H, Dh = q.shape
    L = S // dil
    nb = -(-L // DIL_BLOCK)
    Lp = nb * DIL_BLOCK
    reach = window // dil

    def to_class(a):
        a = a.reshape(B, L, dil, H, Dh).transpose(0, 2, 1, 3, 4)
        a = jnp.pad(a, ((0, 0), (0, 0), (0, Lp - L), (0, 0), (0, 0)))
        return a.reshape(B, dil, nb, DIL_BLOCK, H, Dh)

    def with_prev(a):
        prev = jnp.pad(a, ((0, 0), (0, 0), (1, 0), (0, 0), (0, 0), (0, 0)))[:, :, :nb]
        return jnp.concatenate([prev, a], axis=3)

    qc = to_class(q)
    kk = with_prev(to_class(k))
    vv = with_prev(to_class(v))
    s = jnp.einsum('bgnqhd,bgnkhd->bghnqk', qc, kk, preferred_element_type=jnp.float32) * scale
    qi = jnp.arange(DIL_BLOCK)[:, None]
    ki = jnp.arange(2 * DIL_BLOCK)[None, :]
    step = DIL_BLOCK + qi - ki
    key_idx = jnp.arange(nb)[:, None, None] * DIL_BLOCK - DIL_BLOCK + ki[None]
    mask = (step >= 0) & (step <= reach) & (key_idx >= 0)
    bias = slopes[:, None, None, None] * (step * dil).astype(jnp.float32)
    s = jnp.where(mask, s - bias, -jnp.inf)
    m = jnp.max(s, axis=-1, keepdims=True)
    e = jnp.exp(s - m)
    den = jnp.sum(e, axis=-1, keepdims=True)
    o = jnp.einsum('bghnqk,bgnkhd->bgnqhd', (e / den).astype(v.dtype), vv)
    lse = (m + jnp.log(den))[..., 0]
    o = o.reshape(B, dil, Lp, H, Dh)[:, :, :L].transpose(0, 2, 1, 3, 4).reshape(B, S, H, Dh)
    lse = lse.reshape(B, dil, H, Lp)[..., :L].transpose(0, 3, 1, 2).reshape(B, S, H)
    return o, lse


def dilated_attention(q, k, v, slopes, scale):
    outs, lses = [], []
    for (w, d) in DIL_PATTERNS:
        o, l = dilated_branch(q, k, v, slopes, scale, w, d)
        outs.append(o)
        lses.append(l)
    wts = jax.nn.softmax(jnp.stack(lses, axis=0), axis=0)
    out = jnp.einsum('pbsh,pbshd->bshd', wts, jnp.stack(outs, axis=0).astype(jnp.float32))
    return out.astype(q.dtype)


def moba_attention(q, k, v, slopes, scale):
    B, S, H, Dh = q.shape
    nb = -(-S // MOBA_BLOCK)
    Sp = nb * MOBA_BLOCK

    def to_blocks(a):
        a = jnp.pad(a, ((0, 0), (0, Sp - S), (0, 0), (0, 0)))
        return a.reshape(B, nb, MOBA_BLOCK, H, Dh).transpose(0, 3, 1, 2, 4)

    kb, vb = to_blocks(k), to_blocks(v)
    k_mean = jnp.mean(kb.astype(jnp.float32), axis=3)
    t = jnp.arange(S)
    own = t // MOBA_BLOCK
    gate = jnp.einsum('bshd,bhnd->bshn', q.astype(jnp.float32), k_mean)
    fully_past = jnp.arange(nb)[None, :] < own[:, None]
    gate = jnp.where(fully_past[None, :, None, :], gate, -jnp.inf)
    n_sel = min(MOBA_TOPK, nb)
    _, sel = lax.top_k(gate, n_sel)
    sel_ok = sel < own[None, :, None, None]
    nc = S // MOBA_QCHUNK

    def chunk(a):
        return a.reshape((B, nc, MOBA_QCHUNK) + a.shape[2:]).swapaxes(0, 1)

    b_ix = jnp.arange(B)[:, None, None, None]
    h_ix = jnp.arange(H)[None, None, :, None]
    k_in = jnp.arange(MOBA_BLOCK)
    n_sel_keys = n_sel * MOBA_BLOCK

    def one(args):
        qi, si, oki, ti = args
        ob = ti[0] // MOBA_BLOCK
        k_own = lax.dynamic_index_in_dim(kb, ob, axis=2, keepdims=False)
        v_own = lax.dynamic_index_in_dim(vb, ob, axis=2, keepdims=False)
        k_sel = kb[b_ix, h_ix, si]
        v_sel = vb[b_ix, h_ix, si]
        s_sel = jnp.einsum('bqhd,bqhjkd->bqhjk', qi, k_sel, preferred_element_type=jnp.float32) * scale
        s_own = jnp.einsum('bqhd,bhkd->bqhk', qi, k_own, preferred_element_type=jnp.float32) * scale
        d_sel = (ti[None, :, None, None, None] - (si[..., None] * MOBA_BLOCK + k_in)).astype(jnp.float32)
        s_sel = jnp.where(oki[..., None], s_sel - slopes[:, None, None] * d_sel, -jnp.inf)
        d_own = ti[:, None] - (ob * MOBA_BLOCK + k_in)[None, :]
        s_own = jnp.where((d_own >= 0)[None, :, None, :],
                          s_own - slopes[None, None, :, None] * d_own.astype(jnp.float32)[None, :, None, :],
                          -jnp.inf)
        s_all = jnp.concatenate([s_sel.reshape(s_sel.shape[:3] + (n_sel_keys,)), s_own], axis=-1)
        probs = jax.nn.softmax(s_all, axis=-1).astype(v.dtype)
        p_sel = probs[..., :n_sel_keys].reshape(s_sel.shape)
        p_own = probs[..., n_sel_keys:]
        return (jnp.einsum('bqhjk,bqhjkd->bqhd', p_sel, v_sel)
                + jnp.einsum('bqhk,bhkd->bqhd', p_own, v_own))

    out = lax.map(one, (chunk(q), chunk(sel), chunk(sel_ok), t.reshape(nc, MOBA_QCHUNK)))
    return out.swapaxes(0, 1).reshape(B, S, H, Dh)


def even_mixer(hn, positions, w_in, cq_norm, ckv_norm, w_uq, w_ukv, qn_nope, qn_rope,
               kn_nope, kn_rope, dil_qn, dil_kn, w_out):
    B, S, _ = hn.shape
    z = hn @ w_in
    o1 = Q_LORA
    o2 = o1 + KV_LORA
    o3 = o2 + MLA_ROPE
    dw = DIL_HEADS * HEAD_DIM
    c_q = rms_norm(z[..., :o1], cq_norm)
    c_kv = rms_norm(z[..., o1:o2], ckv_norm)
    q = (c_q @ w_uq).reshape(B, S, MLA_HEADS, MLA_NOPE + MLA_ROPE)
    q_nope = rms_norm(q[..., :MLA_NOPE], qn_nope)
    q_rope = apply_rope(rms_norm(q[..., MLA_NOPE:], qn_rope), positions)
    kv = (c_kv @ w_ukv).reshape(B, S, MLA_HEADS, MLA_NOPE + MLA_V)
    k_nope = rms_norm(kv[..., :MLA_NOPE], kn_nope)
    v_a = kv[..., MLA_NOPE:]
    k_rope = apply_rope(rms_norm(z[..., o2:o3], kn_rope), positions)
    k_rope = jnp.broadcast_to(k_rope[:, :, None, :], (B, S, MLA_HEADS, MLA_ROPE))
    q_a = jnp.concatenate([q_nope, q_rope], axis=-1)
    k_a = jnp.concatenate([k_nope, k_rope], axis=-1)
    out_a = causal_attention_blocks(q_a, k_a, v_a, (MLA_NOPE + MLA_ROPE) ** -0.5)
    q_b = rms_norm(z[..., o3:o3 + dw].reshape(B, S, DIL_HEADS, HEAD_DIM), dil_qn)
    k_b = rms_norm(z[..., o3 + dw:o3 + 2 * dw].reshape(B, S, DIL_HEADS, HEAD_DIM), dil_kn)
    v_b = z[..., o3 + 2 * dw:o3 + 3 * dw].reshape(B, S, DIL_HEADS, HEAD_DIM)
    out_b = dilated_attention(q_b, k_b, v_b, alibi_slopes(DIL_HEADS), HEAD_DIM ** -0.5)
    mixed = jnp.concatenate([out_a.reshape(B, S, MLA_HEADS * MLA_V),
                             out_b.reshape(B, S, dw)], axis=-1)
    return mixed @ w_out


def odd_mixer(hn, w_in, qn, kn, w_out):
    B, S, _ = hn.shape
    z = (hn @ w_in).reshape(B, S, 3, MOBA_HEADS, HEAD_DIM)
    q = rms_norm(z[:, :, 0], qn)
    k = rms_norm(z[:, :, 1], kn)
    v = z[:, :, 2]
    o = moba_attention(q, k, v, alibi_slopes(MOBA_HEADS), HEAD_DIM ** -0.5)
    return o.reshape(B, S, ODD_MIX) @ w_out


def setup_inputs(seed: int = 0) -> dict:
    key = jax.random.key(seed)
    ks = iter(jax.random.split(key, 32))

    def w(shape, fan_in):
        return jax.random.normal(next(ks), shape, jnp.float32) * (fan_in ** -0.5)

    def gain(shape):
        return 1.0 + 0.05 * jax.random.normal(next(ks), shape, jnp.float32)

    x = jax.random.normal(next(ks), (BATCH, SEQ, D_MODEL), jnp.float32)
    p = jax.random.normal(next(ks), (DEPTH, BATCH, SEQ, PLE_DIM), jnp.float32)
    positions = jnp.broadcast_to(jnp.arange(SEQ, dtype=jnp.int32), (BATCH, SEQ))
    return {
        'x': x,
        'p': p,
        'positions': positions,
        'e_w_in': w((N_EVEN, D_MODEL, EVEN_IN), D_MODEL),
        'e_cq_norm': gain((N_EVEN, Q_LORA)),
        'e_ckv_norm': gain((N_EVEN, KV_LORA)),
        'e_w_uq': w((N_EVEN, Q_LORA, MLA_HEADS * (MLA_NOPE + MLA_ROPE)), Q_LORA),
        'e_w_ukv': w((N_EVEN, KV_LORA, MLA_HEADS * (MLA_NOPE + MLA_V)), KV_LORA),
        'e_qn_nope': gain((N_EVEN, MLA_NOPE)),
        'e_qn_rope': gain((N_EVEN, MLA_ROPE)),
        'e_kn_nope': gain((N_EVEN, MLA_NOPE)),
        'e_kn_rope': gain((N_EVEN, MLA_ROPE)),
        'e_dil_qn': gain((N_EVEN, HEAD_DIM)),
        'e_dil_kn': gain((N_EVEN, HEAD_DIM)),
        'e_w_out': w((N_EVEN, EVEN_MIX, D_MODEL), EVEN_MIX),
        'o_w_in': w((N_ODD, D_MODEL, 3 * ODD_MIX), D_MODEL),
        'o_qn': gain((N_ODD, HEAD_DIM)),
        'o_kn': gain((N_ODD, HEAD_DIM)),
        'o_w_out': w((N_ODD, ODD_MIX, D_MODEL), ODD_MIX),
        'mix_norm': gain((DEPTH, D_MODEL)),
        'ff_norm': gain((DEPTH, D_MODEL)),
        'w_ff1': w((DEPTH, D_MODEL, D_FF), D_MODEL),
        'w_ff2': w((DEPTH, D_FF, D_MODEL), D_FF),
        'ple_norm': gain((DEPTH, D_MODEL)),
        'w_ple_gate': w((DEPTH, D_MODEL, D_MODEL), D_MODEL),
        'w_ple_proj': w((DEPTH, PLE_DIM, D_MODEL), PLE_DIM),
    }


def reference(x, p, positions, e_w_in, e_cq_norm, e_ckv_norm, e_w_uq, e_w_ukv, e_qn_nope,
              e_qn_rope, e_kn_nope, e_kn_rope, e_dil_qn, e_dil_kn, e_w_out, o_w_in, o_qn,
              o_kn, o_w_out, mix_norm, ff_norm, w_ff1, w_ff2, ple_norm, w_ple_gate, w_ple_proj):
    h = x
    for i in range(DEPTH):
        j = i // 2
        hn = rms_norm(h, mix_norm[i])
        if i % 2 == 0:
            mix = even_mixer(hn, positions, e_w_in[j], e_cq_norm[j], e_ckv_norm[j], e_w_uq[j],
                             e_w_ukv[j], e_qn_nope[j], e_qn_rope[j], e_kn_nope[j], e_kn_rope[j],
                             e_dil_qn[j], e_dil_kn[j], e_w_out[j])
        else:
            mix = odd_mixer(hn, o_w_in[j], o_qn[j], o_kn[j], o_w_out[j])
        h = h + mix
        u = jax.nn.relu(rms_norm(h, ff_norm[i]) @ w_ff1[i])
        h = h + (u * u) @ w_ff2[i]
        g = jax.nn.sigmoid(rms_norm(h, ple_norm[i]) @ w_ple_gate[i])
        h = h + g * (p[i] @ w_ple_proj[i])
    return h
```

```python
import numpy as np
from contextlib import ExitStack
import concourse.bass as bass
import concourse.mybir as mybir
from concourse.bass_utils import run_bass_kernel_spmd

F32 = mybir.dt.float32
BF16 = mybir.dt.bfloat16
I32 = mybir.dt.int32
ALU = mybir.AluOpType
AF = mybir.ActivationFunctionType
AX = mybir.AxisListType

S = 4096
D = 1024
NT = 32
EPS = 1e-6
BIG = 4096.0
NEG = -1.0e30
MAGIC = 12582912.0
TWO_PI = 2.0 * np.pi


class Buf:
    def __init__(self, t, name=''):
        self.t = t
        self.name = name
        self.w = None
        self.r = {}

    def __getitem__(self, idx):
        return self.t[idx]


class KB:
    ENG = ['pe', 'act', 'dve', 'pool', 'sp']

    def __init__(self, nc, es, n_dsem=20):
        self.nc = nc
        self.es = es
        self.e = {'pe': nc.tensor, 'act': nc.scalar, 'dve': nc.vector, 'pool': nc.gpsimd, 'sp': nc.sync}
        self.sem = {n: es.enter_context(nc.semaphore('s_' + n)) for n in self.ENG}
        self.cnt = {n: 0 for n in self.ENG}
        self.seen = {n: {} for n in self.ENG}
        self.dsems = [[es.enter_context(nc.semaphore('d_%d' % i)), 0] for i in range(n_dsem)]
        self.dnames = {}
        self.nbuf = 0
        self.pes = None
        self.ninst = 0

    def begin_phase(self):
        self.pes = ExitStack()
        self.dnames = {}

    def end_phase(self):
        self.barrier()
        self.pes.close()
        self.pes = None

    def sbuf(self, shape, dt, name=None, glob=False):
        self.nbuf += 1
        name = (name or 'b') + '_%d' % self.nbuf
        st = self.es if glob else self.pes
        t = st.enter_context(self.nc.sbuf_tensor(name, list(shape), dt))
        return Buf(t, name)

    def psum(self, shape, dt, name=None):
        self.nbuf += 1
        name = (name or 'p') + '_%d' % self.nbuf
        t = self.pes.enter_context(self.nc.psum_tensor(name, list(shape), dt))
        return t

    def dsem(self, name):
        if name not in self.dnames:
            self.dnames[name] = len(self.dnames)
            assert len(self.dnames) <= len(self.dsems), 'too many dma sems'
        return self.dnames[name]

    def _need(self, eng, deps):
        E = self.e[eng]
        best = {}
        for key, val in deps:
            if val > best.get(key, 0):
                best[key] = val
        for key, val in best.items():
            if key == ('e', 'pe') and eng == 'pe':
                continue
            if self.seen[eng].get(key, 0) >= val:
                continue
            if key[0] == 'e':
                E.wait_ge(self.sem[key[1]], val)
            else:
                E.wait_ge(self.dsems[key[1]][0], val)
            self.seen[eng][key] = val
            self.ninst += 1

    @staticmethod
    def _deps(reads, writes):
        deps = []
        for b in reads:
            if b.w is not None:
                deps.append((b.w[0:2], b.w[2]))
        for b in writes:
            if b.w is not None:
                deps.append((b.w[0:2], b.w[2]))
            for key, val in b.r.items():
                deps.append((key, val))
        return deps

    def op(self, eng, fn, reads=(), writes=()):
        self._need(eng, self._deps(reads, writes))
        ins = fn(self.e[eng])
        self.cnt[eng] += 1
        c = self.cnt[eng]
        ins.then_inc(self.sem[eng], 1)
        self.ninst += 1
        for b in reads:
            b.r[('e', eng)] = c
        for b in writes:
            b.w = ('e', eng, c)
            b.r = {}
        return ins

    def dma(self, q, out_ap, in_ap, sem, out_b=None, in_b=None, **kw):
        self._need(q, self._deps([in_b] if in_b else [], [out_b] if out_b else []))
        ins = self.e[q].dma_start(out=out_ap, in_=in_ap, **kw)
        si = self.dsem(sem)
        s = self.dsems[si]
        s[1] += 16
        ins.then_inc(s[0], 16)
        self.ninst += 1
        if in_b is not None:
            in_b.r[('d', si)] = s[1]
        if out_b is not None:
            out_b.w = ('d', si, s[1])
            out_b.r = {}
        return ins

    def barrier(self):
        for eng in self.ENG:
            deps = [(('e', x), self.cnt[x]) for x in self.ENG if x != eng and self.cnt[x] > 0]
            deps += [(('d', i), s[1]) for i, s in enumerate(self.dsems) if s[1] > 0]
            self._need(eng, deps)


def _bf16_round(x):
    x = np.asarray(x, np.float32)
    u = x.view(np.uint32)
    r = ((u >> 16) & 1) + 0x7FFF
    return ((u + r) & 0xFFFF0000).view(np.float32)


def _slopes(n):
    return (2.0 ** (-8.0 * np.arange(1, n + 1, dtype=np.float64) / n))


def host_consts():
    c = {}
    kk = np.arange(128)[:, None]
    qq = np.arange(128)[None, :]
    blocks = []
    for dl in range(-3, 4):
        dist = 128 * dl + qq - kk
        blocks.append(np.where(dist >= 0, 0.0, -BIG))
    c['c_maskc'] = np.concatenate(blocks, 1).astype(np.float32)
    blocks = []
    for dl in range(-3, 20):
        dist = 128 * dl + qq - kk
        m = ((dist >= 0) & (dist <= 128)).astype(np.float64) + ((dist >= 0) & (dist <= 512) & (dist % 4 == 0)) \
            + ((dist >= 0) & (dist <= 2048) & (dist % 16 == 0))
        with np.errstate(divide='ignore'):
            v = np.where(m > 0, 8.0 * np.log(np.maximum(m, 1e-30)), -BIG)
        blocks.append(v)
    c['c_maskd'] = np.concatenate(blocks, 1).astype(np.float32)
    half = 16
    inv = (10000.0 ** (-np.arange(half, dtype=np.float32) / half)).astype(np.float32)
    c['c_invf'] = (inv / np.float32(TWO_PI)).astype(np.float32).reshape(1, 16)
    p = np.arange(128, dtype=np.float32)[:, None]
    t = np.arange(32, dtype=np.float32)[None, :]
    z = np.zeros((128, 32), np.float32)
    c['c_tokq'] = np.stack([-(p + z), -(p + z), -128.0 * (t + z), -128.0 * (t + z)], -1).astype(np.float32)
    c['c_tokk'] = np.stack([p + z, p + z, t + z, t + z], -1).astype(np.float32)
    for nm, nh in (('0', 8), ('1', 16)):
        M = 8.0 * _slopes(nh)
        hi = _bf16_round(M.astype(np.float32))
        lo = _bf16_round((M - hi.astype(np.float64)).astype(np.float32))
        c['c_hq' + nm] = np.stack([hi, lo, 128.0 * hi, 128.0 * lo], -1).astype(np.float32).reshape(1, nh * 4)
        c['c_hk' + nm] = np.stack([hi, lo, hi, lo], -1).astype(np.float32).reshape(1, nh * 4)
    return c


CONST_SHAPES = {
    'c_maskc': [128, 7 * 128], 'c_maskd': [128, 23 * 128], 'c_invf': [1, 16],
    'c_tokq': [128, 32, 4], 'c_tokk': [128, 32, 4],
    'c_hq0': [1, 32], 'c_hk0': [1, 32], 'c_hq1': [1, 64], 'c_hk1': [1, 64],
}

IN_SHAPES = {
    'x': ([S, D], F32), 'p': ([2, S, 256], F32), 'positions': ([128, 32], I32),
    'e_w_in': ([1, 1024, 2208], F32), 'e_cq_norm': ([1, 384], F32), 'e_ckv_norm': ([1, 256], F32),
    'e_w_uq': ([1, 384, 768], F32), 'e_w_ukv': ([1, 256, 1024], F32),
    'e_qn_nope': ([1, 64], F32), 'e_qn_rope': ([1, 32], F32), 'e_kn_nope': ([1, 64], F32), 'e_kn_rope': ([1, 32], F32),
    'e_dil_qn': ([1, 64], F32), 'e_dil_kn': ([1, 64], F32), 'e_w_out': ([1, 1024, 1024], F32),
    'o_w_in': ([1, 1024, 3072], F32), 'o_qn': ([1, 64], F32), 'o_kn': ([1, 64], F32), 'o_w_out': ([1, 1024, 1024], F32),
    'mix_norm': ([2, 1024], F32), 'ff_norm': ([2, 1024], F32), 'w_ff1': ([2, 1024, 4096], F32),
    'w_ff2': ([2, 4096, 1024], F32), 'ple_norm': ([2, 1024], F32), 'w_ple_gate': ([2, 1024, 1024], F32),
    'w_ple_proj': ([2, 256, 1024], F32),
}


class Prog:
    def __init__(self, cfg=None):
        cfg = cfg or {}
        self.cfg = cfg
        self.nc = bass.Bass("TRN2", target_bir_lowering=False)
        nc = self.nc
        self.I = {}
        for name, (shape, dt) in IN_SHAPES.items():
            self.I[name] = nc.dram_tensor(name, shape, dt, kind="ExternalInput")
        for name, shape in CONST_SHAPES.items():
            self.I[name] = nc.dram_tensor(name, shape, F32, kind="ExternalInput")
        self.y = nc.dram_tensor("y", [S, D], F32, kind="ExternalOutput")
        ext_in = cfg.get('scr_in', ())
        ext_out = cfg.get('scr_out', ())

        def scr(name, shape, dt):
            if name in ext_in:
                return nc.dram_tensor(name, shape, dt, kind="ExternalInput")
            if name in ext_out:
                return nc.dram_tensor(name, shape, dt, kind="ExternalOutput")
            return nc.dram_tensor(name, shape, dt)

        self.hnT = scr('hnT', [8, 128, S], BF16)
        self.QT = scr('QT', [16, 128, S], BF16)
        self.KT = scr('KT', [16, 128, S], BF16)
        self.V = scr('V', [16, S, 128], BF16)
        self.mixT = scr('mixT', [8, 128, S], BF16)
        self.hA = scr('hA', [S, D], F32)
        self.hB = scr('hB', [S, D], F32)
        self.hC = scr('hC', [S, D], F32)

    def load_bc(self, k, dst, dst_ap, row_ap, n, sem='cst'):
        k.dma('sp', dst_ap, row_ap.partition_broadcast(128), sem, out_b=dst)

    def load_w(self, k, dst, w2d, KC, N, sem='w'):
        src = w2d.rearrange("(kc p) n -> p kc n", p=128)
        si = k.dsem(sem)
        vals = []
        for k0 in range(0, KC, 8):
            k1 = min(KC, k0 + 8)
            for c0 in range(0, N, 512):
                n = min(512, N - c0)
                if len(vals) >= 6:
                    k._need('pool', [(('d', si), vals[-6])])
                k.dma('pool', dst[:, k0:k1, c0:c0 + n], src[:, k0:k1, c0:c0 + n], sem, out_b=dst)
                vals.append(k.dsems[si][1])

    def rstd(self, k, ss, ss_ap, out, out_ap, n):
        k.op('act', lambda e: e.activation(out=out_ap, in_=ss_ap, func=AF.Sqrt, scale=1.0 / n, bias=self.epsb[:, 0:1]),
             reads=[ss, self.epsb], writes=[out])
        k.op('dve', lambda e: e.reciprocal(out=out_ap, in_=out_ap), reads=[out], writes=[out])

    def setup_globals(self, k):
        self.ident = k.sbuf([128, 128], BF16, 'ident', glob=True)
        self.onesf = k.sbuf([128, 128], F32, 'onesf', glob=True)
        self.epsb = k.sbuf([128, 1], F32, 'epsb', glob=True)
        identf = k.sbuf([128, 128], F32, 'identf', glob=True)
        k.op('pool', lambda e: e.memset(self.onesf[:, :], 1.0), writes=[self.onesf])
        k.op('pool', lambda e: e.memset(self.epsb[:, :], EPS), writes=[self.epsb])
        k.op('pool', lambda e: e.affine_select(out=identf[:, :], in_=self.onesf[:, :], pattern=[[-1, 128]],
                                               compare_op=ALU.is_equal, fill=0.0, base=0, channel_multiplier=1),
             reads=[self.onesf], writes=[identf])
        k.op('dve', lambda e: e.tensor_copy(out=self.ident[:, :], in_=identf[:, :]), reads=[identf], writes=[self.ident])

    class NormT:
        def __init__(self, P, k, psB):
            self.P = P
            self.k = k
            self.junk = [k.sbuf([128, 1024], BF16, 'nj') for _ in range(2)]
            self.ss = [k.sbuf([128, 2], F32, 'nss') for _ in range(2)]
            self.hn = [k.sbuf([128, 1024], BF16, 'nhn') for _ in range(2)]
            self.ps = [Buf(psB[:, i * 1024:(i + 1) * 1024], 'npsT%d' % i) for i in range(psB.shape[1] // 1024)]
            self.i = 0

        def run_a(self, hbuf, hap, gain):
            k, P = self.k, self.P
            i = self.i
            self.i += 1
            junk, ss, hn = self.junk[i % 2], self.ss[i % 2], self.hn[i % 2]
            k.op('act', lambda e: e.activation(out=junk[:, :], in_=hap, func=AF.Square, accum_out=ss[:, 0:1]),
                 reads=[hbuf], writes=[junk, ss])
            P.rstd(k, ss, ss[:, 0:1], ss, ss[:, 1:2], 1024)
            k.op('dve', lambda e: e.scalar_tensor_tensor(out=hn[:, :], in0=hap, scalar=ss[:, 1:2], in1=gain[:, :],
                                                         op0=ALU.mult, op1=ALU.mult), reads=[hbuf, ss, gain], writes=[hn])
            return i

        def run_b(self, i, stage, col0):
            k, P = self.k, self.P
            hn = self.hn[i % 2]
            ps = self.ps[i % len(self.ps)]
            for c in range(8):
                k.op('pe', lambda e, c=c: e.transpose(out=ps[:, c * 128:(c + 1) * 128], in_=hn[:, c * 128:(c + 1) * 128],
                                                      identity=P.ident[:, :]), reads=[hn, P.ident], writes=[ps])
            k.op('act', lambda e: e.copy(out=stage[:, :, col0:col0 + 128], in_=ps[:, :].rearrange("p (c t) -> p c t", c=8)),
                 reads=[ps], writes=[stage])

        def run(self, hbuf, hap, gain, stage, col0):
            i = self.run_a(hbuf, hap, gain)
            self.run_b(i, stage, col0)

    def phase_norm0(self, k):
        I = self.I
        k.begin_phase()
        gain = k.sbuf([128, 1024], F32, 'gain')
        self.load_bc(k, gain, gain[:, :], I['mix_norm'][0, :], 1024)
        psB = k.psum([128, 2048], BF16)
        nt = self.NormT(self, k, psB)
        hts = [k.sbuf([128, 1024], F32, 'ht') for _ in range(3)]
        stages = [k.sbuf([128, 8, 512], BF16, 'stg') for _ in range(2)]
        dstT = self.hnT[:, :, :].rearrange("c p t -> p c t")
        deferred = []
        for T in range(8):
            stage = stages[T % 2]
            for s in range(4):
                t = 4 * T + s
                ht = hts[t % 3]
                k.dma('sp', ht[:, :], I['x'][t * 128:(t + 1) * 128, :], 'ld%d' % (t % 3), out_b=ht)
                i = nt.run_a(ht, ht[:, :], gain)
                for f in deferred:
                    f()
                deferred = [lambda i=i, stage=stage, s=s: nt.run_b(i, stage, s * 128)]
            deferred.append(lambda T=T, stage=stage: k.dma('sp', dstT[:, :, T * 512:(T + 1) * 512], stage[:, :, :], 'st%d' % (T % 2), in_b=stage))
        for f in deferred:
            f()
        k.end_phase()

    def phase_p0(self, k):
        I = self.I
        k.begin_phase()
        Win = k.sbuf([128, 8, 2208], BF16, 'Win')
        Wuq = k.sbuf([128, 3, 768], BF16, 'Wuq')
        Wukv = k.sbuf([128, 2, 1024], BF16, 'Wukv')
        self.load_w(k, Win, I['e_w_in'][0], 8, 2208)
        self.load_w(k, Wuq, I['e_w_uq'][0], 3, 768)
        self.load_w(k, Wukv, I['e_w_ukv'][0], 2, 1024)
        g_cq = k.sbuf([128, 384], F32); self.load_bc(k, g_cq, g_cq[:, :], I['e_cq_norm'][0, :], 384)
        g_ckv = k.sbuf([128, 256], F32); self.load_bc(k, g_ckv, g_ckv[:, :], I['e_ckv_norm'][0, :], 256)
        g64 = k.sbuf([128, 4, 64], F32)
        for i, nm in enumerate(['e_qn_nope', 'e_kn_nope', 'e_dil_qn', 'e_dil_kn']):
            self.load_bc(k, g64, g64[:, i, :], I[nm][0, :], 64)
        g32 = k.sbuf([128, 2, 32], F32)
        for i, nm in enumerate(['e_qn_rope', 'e_kn_rope']):
            self.load_bc(k, g32, g32[:, i, :], I[nm][0, :], 32)
        tokq = k.sbuf([128, 32, 4], F32); k.dma('sp', tokq[:, :, :], I['c_tokq'][:, :, :], 'cst', out_b=tokq)
        tokk = k.sbuf([128, 32, 4], F32); k.dma('sp', tokk[:, :, :], I['c_tokk'][:, :, :], 'cst', out_b=tokk)
        hq = k.sbuf([128, 8, 4], F32); self.load_bc(k, hq, hq[:, :, :].rearrange("p h c -> p (h c)"), I['c_hq0'][0, :], 32)
        hk = k.sbuf([128, 8, 4], F32); self.load_bc(k, hk, hk[:, :, :].rearrange("p h c -> p (h c)"), I['c_hk0'][0, :], 32)
        invf = k.sbuf([128, 16], F32); self.load_bc(k, invf, invf[:, :], I['c_invf'][0, :], 16)
        posi = k.sbuf([128, 32], I32); k.dma('sp', posi[:, :], I['positions'][:, :], 'cst', out_b=posi)
        posf = k.sbuf([128, 32], F32)
        k.op('dve', lambda e: e.tensor_copy(out=posf[:, :], in_=posi[:, :]), reads=[posi], writes=[posf])
        xt = k.sbuf([128, 32, 16], F32)
        k.op('dve', lambda e: e.tensor_tensor(out=xt[:, :, :], in0=posf[:, :].unsqueeze(2).to_broadcast([128, 32, 16]),
                                              in1=invf[:, :].unsqueeze(1).to_broadcast([128, 32, 16]), op=ALU.mult),
             reads=[posf, invf], writes=[xt])
        sin_all = k.sbuf([128, 32, 16], F32)
        cos_all = k.sbuf([128, 32, 16], F32)
        t1 = k.sbuf([128, 32, 16], F32)
        t2 = k.sbuf([128, 32, 16], F32)
        for dst, shift in ((sin_all, 0.0), (cos_all, 0.25)):
            k.op('dve', lambda e, shift=shift: e.tensor_scalar(out=t2[:, :, :], in0=xt[:, :, :], scalar1=shift, scalar2=None, op0=ALU.add),
                 reads=[xt], writes=[t2])
            k.op('dve', lambda e: e.tensor_scalar(out=t1[:, :, :], in0=t2[:, :, :], scalar1=MAGIC, scalar2=None, op0=ALU.add),
                 reads=[t2], writes=[t1])
            k.op('dve', lambda e: e.scalar_tensor_tensor(out=t1[:, :, :], in0=t1[:, :, :], scalar=MAGIC, in1=t2[:, :, :],
                                                         op0=ALU.subtract, op1=ALU.subtract), reads=[t1, t2], writes=[t1])
            k.op('act', lambda e, dst=dst: e.activation(out=dst[:, :, :], in_=t1[:, :, :], func=AF.Sin, scale=-TWO_PI * (1 - 1e-6)),
                 reads=[t1], writes=[dst])

        psF = k.psum([128, 3584], F32)
        psB = k.psum([128, 1024], BF16)
        bank = [Buf(psF[:, i * 512:(i + 1) * 512], 'bank%d' % i) for i in range(7)]
        psT = Buf(psB[:, :], 'psT')

        hnTs = [k.sbuf([128, 8, 256], BF16, 'hnTs') for _ in range(2)]
        QTs = [k.sbuf([128, 16, 256], BF16, 'QTs') for _ in range(2)]
        KTs = [k.sbuf([128, 16, 256], BF16, 'KTs') for _ in range(2)]
        Vtm = [k.sbuf([128, 16, 128], BF16, 'Vtm') for _ in range(2)]
        Qtm = [k.sbuf([128, 16, 96], BF16, 'Qtm') for _ in range(2)]
        Ktm = [k.sbuf([128, 16, 96], BF16, 'Ktm') for _ in range(2)]
        for i in range(2):
            k.op('pool', lambda e, i=i: e.memset(Vtm[i][:, :, 64:128], 1.0), writes=[Vtm[i]])
            k.op('pool', lambda e, i=i: e.memset(Qtm[i][:, :, :], 0.0), writes=[Qtm[i]])
            k.op('pool', lambda e, i=i: e.memset(Ktm[i][:, :, :], 0.0), writes=[Ktm[i]])
            k.op('dve', lambda e, i=i: e.tensor_copy(out=Qtm[i][:, 8:16, 64:68], in_=hq[:, :, :]), reads=[hq], writes=[Qtm[i]])
            k.op('dve', lambda e, i=i: e.tensor_copy(out=Ktm[i][:, 8:16, 68:72], in_=hk[:, :, :]), reads=[hk], writes=[Ktm[i]])
        junk = k.sbuf([128, 512], F32, 'junk')
        ssA = k.sbuf([128, 8], F32, 'ssA')
        krs = k.sbuf([128, 32], F32, 'krs')
        krn = k.sbuf([128, 32], F32, 'krn')
        krr_l = [k.sbuf([128, 32], F32, 'krr') for _ in range(2)]
        rka = k.sbuf([128, 1, 16], F32, 'rka')
        rkb = k.sbuf([128, 1, 16], F32, 'rkb')
        cq_bf = k.sbuf([128, 384], BF16, 'cq_bf')
        ckv_bf = k.sbuf([128, 256], BF16, 'ckv_bf')
        cT = k.sbuf([128, 5, 128], BF16, 'cT')
        qs_l = [k.sbuf([128, 768], F32, 'qs') for _ in range(2)]
        kvs_l = [k.sbuf([128, 1024], F32, 'kvs') for _ in range(2)]
        dqs_l = [k.sbuf([128, 512], F32, 'dqs') for _ in range(2)]
        dks_l = [k.sbuf([128, 512], F32, 'dks') for _ in range(2)]
        tmpA = k.sbuf([128, 1024], F32, 'tmpA')
        tmpB = k.sbuf([128, 1024], F32, 'tmpB')
        ssq = k.sbuf([128, 40], F32, 'ssq')
        rsq = k.sbuf([128, 40], F32, 'rsq')
        qrn = k.sbuf([128, 8, 32], F32, 'qrn')
        ra = k.sbuf([128, 8, 16], F32, 'ra')
        rb = k.sbuf([128, 8, 16], F32, 'rb')
        rc = k.sbuf([128, 8, 16], F32, 'rc')
        rd = k.sbuf([128, 8, 16], F32, 'rd')

        srcT = self.hnT[:, :, :].rearrange("c p t -> p c t")
        QTd = self.QT[:, :, :].rearrange("h r t -> r h t")
        KTd = self.KT[:, :, :].rearrange("h r t -> r h t")
        Vd = self.V[:, :, :].rearrange("h t c -> t h c")

        def hview(ap, h, d):
            return ap.rearrange("p (h d) -> p h d", h=h)

        def sumsq(src3, H, dh, dst_ap, tmp):
            t3 = tmp[:, 0:H * dh].rearrange("p (h d) -> p h d", h=H)
            k.op('dve', lambda e: e.tensor_tensor(out=t3, in0=src3[1], in1=src3[1], op=ALU.mult), reads=[src3[0]], writes=[tmp])
            k.op('dve', lambda e: e.tensor_reduce(out=dst_ap, in_=t3, axis=AX.X, op=ALU.add), reads=[tmp], writes=[ssq])

        def headnorm(src3, H, dh, rs_ap, gain_ap, dst, dst_ap, tmp, eng2='pool'):
            t3 = tmp[:, 0:H * dh].rearrange("p (h d) -> p h d", h=H)
            k.op('dve', lambda e: e.tensor_tensor(out=t3, in0=src3[1], in1=rs_ap.unsqueeze(2).to_broadcast([128, H, dh]), op=ALU.mult),
                 reads=[src3[0], rsq], writes=[tmp])
            k.op(eng2, lambda e: e.tensor_tensor(out=dst_ap, in0=t3, in1=gain_ap.unsqueeze(1).to_broadcast([128, H, dh]), op=ALU.mult),
                 reads=[tmp, g64, g32], writes=[dst])

        k.dma('sp', hnTs[0][:, :, :], srcT[:, :, 0:256], 'ld0', out_b=hnTs[0])

        def stageA(t):
            T2, s = divmod(t, 2)
            hs = hnTs[T2 % 2]
            if s == 0 and T2 + 1 < 16:
                k.dma('sp', hnTs[(T2 + 1) % 2][:, :, :], srcT[:, :, (T2 + 1) * 256:(T2 + 2) * 256], 'ld%d' % ((T2 + 1) % 2),
                      out_b=hnTs[(T2 + 1) % 2])
            vt = Vtm[t % 2]
            qs, kvs, dqs, dks, krr = qs_l[t % 2], kvs_l[t % 2], dqs_l[t % 2], dks_l[t % 2], krr_l[t % 2]
            ra, rb = rka, rkb
            for (bk, c0, n) in ((0, 0, 384), (1, 384, 288), (4, 672, 512), (5, 1184, 512), (6, 1696, 512)):
                for kc in range(8):
                    k.op('pe', lambda e, bk=bk, c0=c0, n=n, kc=kc: e.matmul(
                        bank[bk][:, 0:n], lhsT=hs[:, kc, s * 128:(s + 1) * 128], rhs=Win[:, kc, c0:c0 + n],
                        start=(kc == 0), stop=(kc == 7)), reads=[hs, Win], writes=[bank[bk]])
            k.op('act', lambda e: e.activation(out=junk[:, 0:384], in_=bank[0][:, 0:384], func=AF.Square, accum_out=ssA[:, 0:1]),
                 reads=[bank[0]], writes=[junk, ssA])
            k.op('act', lambda e: e.activation(out=junk[:, 0:256], in_=bank[1][:, 0:256], func=AF.Square, accum_out=ssA[:, 1:2]),
                 reads=[bank[1]], writes=[junk, ssA])
            k.op('act', lambda e: e.copy(out=krs[:, :], in_=bank[1][:, 256:288]), reads=[bank[1]], writes=[krs])
            k.op('act', lambda e: e.activation(out=junk[:, 0:32], in_=krs[:, :], func=AF.Square, accum_out=ssA[:, 2:3]),
                 reads=[krs], writes=[junk, ssA])
            self.rstd(k, ssA, ssA[:, 0:1], ssA, ssA[:, 4:5], 384)
            self.rstd(k, ssA, ssA[:, 1:2], ssA, ssA[:, 5:6], 256)
            self.rstd(k, ssA, ssA[:, 2:3], ssA, ssA[:, 6:7], 32)
            k.op('dve', lambda e: e.scalar_tensor_tensor(out=cq_bf[:, :], in0=bank[0][:, 0:384], scalar=ssA[:, 4:5], in1=g_cq[:, :],
                                                         op0=ALU.mult, op1=ALU.mult), reads=[bank[0], ssA, g_cq], writes=[cq_bf])
            k.op('dve', lambda e: e.scalar_tensor_tensor(out=ckv_bf[:, :], in0=bank[1][:, 0:256], scalar=ssA[:, 5:6], in1=g_ckv[:, :],
                                                         op0=ALU.mult, op1=ALU.mult), reads=[bank[1], ssA, g_ckv], writes=[ckv_bf])
            k.op('dve', lambda e: e.scalar_tensor_tensor(out=krn[:, :], in0=krs[:, :], scalar=ssA[:, 6:7], in1=g32[:, 1, :],
                                                         op0=ALU.mult, op1=ALU.mult), reads=[krs, ssA, g32], writes=[krn])
            cs = cos_all[:, t, :]
            sn = sin_all[:, t, :]
            k.op('pool', lambda e: e.tensor_tensor(out=ra[:, 0, :], in0=krn[:, 0:16], in1=cs, op=ALU.mult), reads=[krn, cos_all], writes=[ra])
            k.op('pool', lambda e: e.tensor_tensor(out=rb[:, 0, :], in0=krn[:, 16:32], in1=sn, op=ALU.mult), reads=[krn, sin_all], writes=[rb])
            k.op('pool', lambda e: e.tensor_tensor(out=krr[:, 0:16], in0=ra[:, 0, :], in1=rb[:, 0, :], op=ALU.subtract), reads=[ra, rb], writes=[krr])
            k.op('pool', lambda e: e.tensor_tensor(out=ra[:, 0, :], in0=krn[:, 0:16], in1=sn, op=ALU.mult), reads=[krn, sin_all], writes=[ra])
            k.op('pool', lambda e: e.tensor_tensor(out=rb[:, 0, :], in0=krn[:, 16:32], in1=cs, op=ALU.mult), reads=[krn, cos_all], writes=[rb])
            k.op('pool', lambda e: e.tensor_tensor(out=krr[:, 16:32], in0=ra[:, 0, :], in1=rb[:, 0, :], op=ALU.add), reads=[ra, rb], writes=[krr])
            for c in range(3):
                k.op('pe', lambda e, c=c: e.transpose(out=psT[:, c * 128:(c + 1) * 128], in_=cq_bf[:, c * 128:(c + 1) * 128],
                                                      identity=self.ident[:, :]), reads=[cq_bf, self.ident], writes=[psT])
            for c in range(2):
                k.op('pe', lambda e, c=c: e.transpose(out=psT[:, (3 + c) * 128:(4 + c) * 128], in_=ckv_bf[:, c * 128:(c + 1) * 128],
                                                      identity=self.ident[:, :]), reads=[ckv_bf, self.ident], writes=[psT])
            k.op('act', lambda e: e.copy(out=cT[:, :, :], in_=psT[:, 0:640].rearrange("p (c t) -> p c t", c=5)), reads=[psT], writes=[cT])
            for (bk, c0, n) in ((0, 0, 512), (1, 512, 256)):
                for kc in range(3):
                    k.op('pe', lambda e, bk=bk, c0=c0, n=n, kc=kc: e.matmul(
                        bank[bk][:, 0:n], lhsT=cT[:, kc, :], rhs=Wuq[:, kc, c0:c0 + n], start=(kc == 0), stop=(kc == 2)),
                        reads=[cT, Wuq], writes=[bank[bk]])
            for (bk, c0, n) in ((2, 0, 512), (3, 512, 512)):
                for kc in range(2):
                    k.op('pe', lambda e, bk=bk, c0=c0, n=n, kc=kc: e.matmul(
                        bank[bk][:, 0:n], lhsT=cT[:, 3 + kc, :], rhs=Wukv[:, kc, c0:c0 + n], start=(kc == 0), stop=(kc == 1)),
                        reads=[cT, Wukv], writes=[bank[bk]])
            k.op('act', lambda e: e.copy(out=qs[:, 0:512], in_=bank[0][:, 0:512]), reads=[bank[0]], writes=[qs])
            k.op('act', lambda e: e.copy(out=qs[:, 512:768], in_=bank[1][:, 0:256]), reads=[bank[1]], writes=[qs])
            k.op('act', lambda e: e.copy(out=kvs[:, 0:512], in_=bank[2][:, 0:512]), reads=[bank[2]], writes=[kvs])
            k.op('act', lambda e: e.copy(out=kvs[:, 512:1024], in_=bank[3][:, 0:512]), reads=[bank[3]], writes=[kvs])
            k.op('act', lambda e: e.copy(out=dqs[:, :], in_=bank[4][:, 0:512]), reads=[bank[4]], writes=[dqs])
            k.op('act', lambda e: e.copy(out=dks[:, :], in_=bank[5][:, 0:512]), reads=[bank[5]], writes=[dks])
            k.op('act', lambda e: e.copy(out=vt[:, 8:16, 0:64], in_=bank[6][:, 0:512].rearrange("p (h d) -> p h d", h=8)),
                 reads=[bank[6]], writes=[vt])
            q3 = qs[:, :].rearrange("p (h d) -> p h d", h=8)
            kv3 = kvs[:, :].rearrange("p (h d) -> p h d", h=8)
            dq3 = dqs[:, :].rearrange("p (h d) -> p h d", h=8)
            dk3 = dks[:, :].rearrange("p (h d) -> p h d", h=8)
            k.op('pool', lambda e: e.tensor_copy(out=vt[:, 0:8, 0:64], in_=kv3[:, :, 64:128]), reads=[kvs], writes=[vt])

        def stageB(t):
            T2, s = divmod(t, 2)
            qts, kts = QTs[T2 % 2], KTs[T2 % 2]
            vt, qt, kt = Vtm[t % 2], Qtm[t % 2], Ktm[t % 2]
            qs, kvs, dqs, dks, krr = qs_l[t % 2], kvs_l[t % 2], dqs_l[t % 2], dks_l[t % 2], krr_l[t % 2]
            cs = cos_all[:, t, :]
            sn = sin_all[:, t, :]
            q3 = qs[:, :].rearrange("p (h d) -> p h d", h=8)
            kv3 = kvs[:, :].rearrange("p (h d) -> p h d", h=8)
            dq3 = dqs[:, :].rearrange("p (h d) -> p h d", h=8)
            dk3 = dks[:, :].rearrange("p (h d) -> p h d", h=8)
            sumsq((qs, q3[:, :, 0:64]), 8, 64, ssq[:, 0:8], tmpA)
            sumsq((kvs, kv3[:, :, 0:64]), 8, 64, ssq[:, 8:16], tmpA)
            sumsq((dqs, dq3), 8, 64, ssq[:, 16:24], tmpA)
            sumsq((dks, dk3), 8, 64, ssq[:, 24:32], tmpA)
            sumsq((qs, q3[:, :, 64:96]), 8, 32, ssq[:, 32:40], tmpA)
            self.rstd(k, ssq, ssq[:, 0:32], rsq, rsq[:, 0:32], 64)
            self.rstd(k, ssq, ssq[:, 32:40], rsq, rsq[:, 32:40], 32)
            headnorm((qs, q3[:, :, 0:64]), 8, 64, rsq[:, 0:8], g64[:, 0, :], qt, qt[:, 0:8, 0:64], tmpA)
            headnorm((kvs, kv3[:, :, 0:64]), 8, 64, rsq[:, 8:16], g64[:, 1, :], kt, kt[:, 0:8, 0:64], tmpB)
            headnorm((dqs, dq3), 8, 64, rsq[:, 16:24], g64[:, 2, :], qt, qt[:, 8:16, 0:64], tmpA)
            headnorm((dks, dk3), 8, 64, rsq[:, 24:32], g64[:, 3, :], kt, kt[:, 8:16, 0:64], tmpB)
            headnorm((qs, q3[:, :, 64:96]), 8, 32, rsq[:, 32:40], g32[:, 0, :], qrn, qrn[:, :, :], tmpA)
            csb = cs.unsqueeze(1).to_broadcast([128, 8, 16])
            snb = sn.unsqueeze(1).to_broadcast([128, 8, 16])
            k.op('dve', lambda e: e.tensor_tensor(out=ra[:, :, :], in0=qrn[:, :, 0:16], in1=csb, op=ALU.mult), reads=[qrn, cos_all], writes=[ra])
            k.op('dve', lambda e: e.tensor_tensor(out=rb[:, :, :], in0=qrn[:, :, 16:32], in1=snb, op=ALU.mult), reads=[qrn, sin_all], writes=[rb])
            k.op('pool', lambda e: e.tensor_tensor(out=rc[:, :, :], in0=qrn[:, :, 0:16], in1=snb, op=ALU.mult), reads=[qrn, sin_all], writes=[rc])
            k.op('pool', lambda e: e.tensor_tensor(out=rd[:, :, :], in0=qrn[:, :, 16:32], in1=csb, op=ALU.mult), reads=[qrn, cos_all], writes=[rd])
            k.op('dve', lambda e: e.tensor_tensor(out=qt[:, 0:8, 64:80], in0=ra[:, :, :], in1=rb[:, :, :], op=ALU.subtract), reads=[ra, rb], writes=[qt])
            k.op('pool', lambda e: e.tensor_tensor(out=qt[:, 0:8, 80:96], in0=rc[:, :, :], in1=rd[:, :, :], op=ALU.add), reads=[rc, rd], writes=[qt])
            k.op('pool', lambda e: e.tensor_copy(out=kt[:, 0:8, 64:96], in_=krr[:, :].unsqueeze(1).to_broadcast([128, 8, 32])),
                 reads=[krr], writes=[kt])
            k.op('pool', lambda e: e.tensor_copy(out=qt[:, 8:16, 68:72], in_=tokq[:, t, :].unsqueeze(1).to_broadcast([128, 8, 4])),
                 reads=[tokq], writes=[qt])
            k.op('pool', lambda e: e.tensor_copy(out=kt[:, 8:16, 64:68], in_=tokk[:, t, :].unsqueeze(1).to_broadcast([128, 8, 4])),
                 reads=[tokk], writes=[kt])
            for (src, dst, h0, R) in ((qt, qts, 0, 96), (qt, qts, 8, 72), (kt, kts, 0, 96), (kt, kts, 8, 72)):
                for hh in range(8):
                    k.op('pe', lambda e, src=src, h0=h0, hh=hh, R=R: e.transpose(
                        out=psT[0:R, hh * 128:(hh + 1) * 128], in_=src[:, h0 + hh, 0:R], identity=self.ident[:, :]),
                        reads=[src, self.ident], writes=[psT])
                k.op('act', lambda e, dst=dst, h0=h0, R=R: e.copy(
                    out=dst[0:R, h0:h0 + 8, s * 128:(s + 1) * 128], in_=psT[0:R, :].rearrange("p (h t) -> p h t", h=8)),
                    reads=[psT], writes=[dst])
            k.dma('sp', Vd[t * 128:(t + 1) * 128, :, :], vt[:, :, :], 'stv%d' % (t % 2), in_b=vt)
            if s == 1:
                c0 = T2 * 256
                k.dma('sp', QTd[0:96, 0:8, c0:c0 + 256], qts[0:96, 0:8, :], 'stq%d' % (T2 % 2), in_b=qts)
                k.dma('sp', QTd[0:72, 8:16, c0:c0 + 256], qts[0:72, 8:16, :], 'stq%d' % (T2 % 2), in_b=qts)
                k.dma('sp', KTd[0:96, 0:8, c0:c0 + 256], kts[0:96, 0:8, :], 'stk%d' % (T2 % 2), in_b=kts)
                k.dma('sp', KTd[0:72, 8:16, c0:c0 + 256], kts[0:72, 8:16, :], 'stk%d' % (T2 % 2), in_b=kts)

        for i in range(33):
            if i < 32:
                stageA(i)
            if i >= 1:
                stageB(i - 1)
        k.end_phase()

    def phase_attn(self, k, heads):
        I = self.I
        k.begin_phase()
        maskc = k.sbuf([128, 7 * 128], BF16, 'maskc')
        k.dma('pool', maskc[:, :], I['c_maskc'][:, :], 'cst', out_b=maskc)
        use_d = any(h['mask'] == 'd' for h in heads)
        if use_d:
            maskd = k.sbuf([128, 23 * 128], BF16, 'maskd')
            for c0 in range(0, 23 * 128, 512):
                n = min(512, 23 * 128 - c0)
                k.dma('pool', maskd[:, c0:c0 + n], I['c_maskd'][:, c0:c0 + n], 'cst', out_b=maskd)
        psF = k.psum([128, 5 * 512], F32)
        Sb = [Buf(psF[:, i * 512:(i + 1) * 512], 'S%d' % i) for i in range(3)]
        Ob = [Buf(psF[:, (3 + i) * 512:(4 + i) * 512], 'O%d' % i) for i in range(2)]
        Pb = [k.sbuf([128, 512], BF16, 'P') for _ in range(3)]
        QTh = [k.sbuf([128, S], BF16, 'QTh') for _ in range(2)]
        KTh = [k.sbuf([128, S], BF16, 'KTh') for _ in range(2)]
        Vh = [k.sbuf([128, 32, 128], BF16, 'Vh') for _ in range(2)]
        rden = [k.sbuf([128, 512], F32, 'rden') for _ in range(2)]
        ost = [k.sbuf([64, S], BF16, 'ost') for _ in range(2)]

        def load_head(h):
            R = heads[h]['R']
            i = h % 2
            k.dma('sp', QTh[i][0:R, :], self.QT[h, 0:R, :], 'ldq%d' % i, out_b=QTh[i])
            k.dma('sp', KTh[i][0:R, :], self.KT[h, 0:R, :], 'ldk%d' % i, out_b=KTh[i])
            k.dma('sp', Vh[i][:, :, :], self.V[h, :, :].rearrange("(b p) c -> p b c", p=128), 'ldv%d' % i, out_b=Vh[i])

        load_head(0)
        gi = 0
        for h, hd in enumerate(heads):
            if h + 1 < len(heads):
                load_head(h + 1)
            R, scale, mk = hd['R'], hd['scale'], hd['mask']
            qT, kT, vv = QTh[h % 2], KTh[h % 2], Vh[h % 2]
            os_ = ost[h % 2]
            pairs = []
            for g in range(8):
                jlo = max(0, 4 * g - 16) if mk == 'd' else 0
                js = list(range(jlo, 4 * g + 4))
                for j in js:
                    pairs.append((g, j, j == js[0], j == js[-1]))

            def cols(g, j):
                d0 = 4 * g - j
                b0 = max(0, -d0)
                b1 = 4 if mk != 'd' else min(4, 17 - d0)
                return b0 * 128, b1 * 128

            def emit_qk(i):
                g, j, first, last = pairs[i]
                Sx = Sb[i % 3]
                d0 = 4 * g - j
                c0, c1 = cols(g, j)
                if mk == 'd':
                    need_mask, tab = True, maskd
                else:
                    need_mask, tab = (d0 <= 0), maskc
                off = (d0 + 3) * 128
                k.op('pe', lambda e: e.matmul(Sx[:, c0:c1], lhsT=kT[0:R, j * 128:(j + 1) * 128], rhs=qT[0:R, g * 512 + c0:g * 512 + c1],
                                              start=True, stop=not need_mask), reads=[kT, qT], writes=[Sx])
                if need_mask:
                    k.op('pe', lambda e: e.matmul(Sx[:, c0:c1], lhsT=self.ident[:, :], rhs=tab[:, off + c0:off + c1], start=False, stop=True),
                         reads=[self.ident, tab], writes=[Sx])
                Px = Pb[i % 3]
                k.op('act', lambda e: e.activation(out=Px[:, c0:c1], in_=Sx[:, c0:c1], func=AF.Exp, scale=float(scale)), reads=[Sx], writes=[Px])

            def emit_pv(i):
                nonlocal gi
                g, j, first, last = pairs[i]
                c0, c1 = cols(g, j)
                Px = Pb[i % 3]
                Ox = Ob[(gi) % 2]
                k.op('pe', lambda e: e.matmul(Ox[:, c0:c1], lhsT=vv[:, j, :], rhs=Px[:, c0:c1], start=first, stop=last), reads=[vv, Px], writes=[Ox])
                if last:
                    rd = rden[gi % 2]
                    k.op('dve', lambda e: e.reciprocal(out=rd[64:128, :], in_=Ox[64:128, :]), reads=[Ox], writes=[rd])
                    k.op('dve', lambda e: e.tensor_tensor(out=os_[0:64, g * 512:(g + 1) * 512], in0=Ox[0:64, :], in1=rd[64:128, :], op=ALU.mult),
                         reads=[Ox, rd], writes=[os_])
                    gi += 1

            LA = 2
            n = len(pairs)
            for i in range(n + LA):
                if i < n:
                    emit_qk(i)
                if i - LA >= 0:
                    emit_pv(i - LA)
            k.dma('sp', self.mixT[h // 2, (h % 2) * 64:(h % 2) * 64 + 64, :], os_[0:64, :], 'sto%d' % (h % 2), in_b=os_)
        k.end_phase()

    def phase_o1(self, k, w_out, h_in, h_out, gain_row):
        k.begin_phase()
        Wo = k.sbuf([128, 8, 1024], BF16, 'Wo')
        self.load_w(k, Wo, w_out, 8, 1024)
        gain = k.sbuf([128, 1024], F32, 'gain')
        self.load_bc(k, gain, gain[:, :], gain_row, 1024)
        psF = k.psum([128, 2048], F32)
        psB = k.psum([128, 2048], BF16)
        mixb = [Buf(psF[:, i * 1024:(i + 1) * 1024], 'mix%d' % i) for i in range(2)]
        nt = self.NormT(self, k, psB)
        mTs = [k.sbuf([128, 8, 512], BF16, 'mTs') for _ in range(2)]
        hts = [k.sbuf([128, 1024], F32, 'ht') for _ in range(3)]
        h1s = [k.sbuf([128, 1024], F32, 'h1') for _ in range(3)]
        stages = [k.sbuf([128, 8, 512], BF16, 'stg') for _ in range(2)]
        srcT = self.mixT[:, :, :].rearrange("c p t -> p c t")
        dstT = self.hnT[:, :, :].rearrange("c p t -> p c t")
        k.dma('sp', mTs[0][:, :, :], srcT[:, :, 0:512], 'ldm0', out_b=mTs[0])
        deferred = []
        for T in range(8):
            if T + 1 < 8:
                k.dma('sp', mTs[(T + 1) % 2][:, :, :], srcT[:, :, (T + 1) * 512:(T + 2) * 512], 'ldm%d' % ((T + 1) % 2), out_b=mTs[(T + 1) % 2])
            ms = mTs[T % 2]
            stage = stages[T % 2]
            for s in range(4):
                t = 4 * T + s
                ht, h1 = hts[t % 3], h1s[t % 3]
                mb = mixb[t % 2]
                k.dma('sp', ht[:, :], h_in[t * 128:(t + 1) * 128, :], 'ldh%d' % (t % 3), out_b=ht)
                for half in range(2):
                    for c in range(8):
                        k.op('pe', lambda e, half=half, c=c: e.matmul(
                            mb[:, half * 512:(half + 1) * 512], lhsT=ms[:, c, s * 128:(s + 1) * 128],
                            rhs=Wo[:, c, half * 512:(half + 1) * 512], start=(c == 0), stop=(c == 7)), reads=[ms, Wo], writes=[mb])
                for f in deferred:
                    f()
                deferred = []
                for half in range(2):
                    hsl = slice(half * 512, (half + 1) * 512)
                    k.op('dve', lambda e, hsl=hsl: e.tensor_tensor(out=h1[:, hsl], in0=mb[:, hsl], in1=ht[:, hsl], op=ALU.add), reads=[mb, ht], writes=[h1])
                k.dma('sp', h_out[t * 128:(t + 1) * 128, :], h1[:, :], 'sth%d' % (t % 3), in_b=h1)
                i = nt.run_a(h1, h1[:, :], gain)
                deferred.append(lambda i=i, stage=stage, s=s: nt.run_b(i, stage, s * 128))
            deferred.append(lambda T=T, stage=stage: k.dma('sp', dstT[:, :, T * 512:(T + 1) * 512], stage[:, :, :], 'st%d' % (T % 2), in_b=stage))
        for f in deferred:
            f()
        k.end_phase()

    def phase_o2(self, k, w1, w2, h_in, h_out):
        k.begin_phase()
        W1 = k.sbuf([128, 8, 4096], BF16, 'W1')
        W2 = k.sbuf([128, 32, 1024], BF16, 'W2')
        self.load_w(k, W1, w1, 8, 4096)
        self.load_w(k, W2, w2, 32, 1024)
        psF = k.psum([128, 4096], F32)
        f1 = [Buf(psF[:, i * 512:i * 512 + 256], 'f1_%d' % i) for i in range(4)]
        f2 = [Buf(psF[:, 2048 + i * 1024:3072 + i * 1024], 'f2_%d' % i) for i in range(2)]
        xTs = [k.sbuf([128, 8, 256], BF16, 'xTs') for _ in range(2)]
        uT = k.sbuf([128, 32, 256], BF16, 'uT')
        rl = [k.sbuf([128, 256], F32, 'rl') for _ in range(2)]
        hts = [k.sbuf([128, 1024], F32, 'ht') for _ in range(2)]
        h2s = [k.sbuf([128, 1024], F32, 'h2') for _ in range(2)]
        srcT = self.hnT[:, :, :].rearrange("c p t -> p c t")
        dbg = self.cfg.get('o2_dbg', 3)
        k.dma('sp', xTs[0][:, :, :], srcT[:, :, 0:256], 'ldx0', out_b=xTs[0])
        for T2 in range(16 if dbg > 1 else 0):
            if T2 + 1 < 16:
                k.dma('sp', xTs[(T2 + 1) % 2][:, :, :], srcT[:, :, (T2 + 1) * 256:(T2 + 2) * 256], 'ldx%d' % ((T2 + 1) % 2),
                      out_b=xTs[(T2 + 1) % 2])
            xs = xTs[T2 % 2]
            for fc in range(32):
                fb = f1[fc % 4]
                r = rl[fc % 2]
                for kc in range(8):
                    k.op('pe', lambda e, fc=fc, kc=kc, fb=fb: e.matmul(fb[:, :], lhsT=W1[:, kc, fc * 128:(fc + 1) * 128], rhs=xs[:, kc, :],
                                                                       start=(kc == 0), stop=(kc == 7)), reads=[W1, xs], writes=[fb])
                k.op('act', lambda e, fb=fb, r=r: e.activation(out=r[:, :], in_=fb[:, :], func=AF.Relu), reads=[fb], writes=[r])
                k.op('dve', lambda e, fb=fb, r=r, fc=fc: e.tensor_tensor(out=uT[:, fc, :], in0=fb[:, :], in1=r[:, :], op=ALU.mult),
                     reads=[fb, r], writes=[uT])
            for s in range(2 if dbg > 2 else 0):
                t = 2 * T2 + s
                ht, h2 = hts[t % 2], h2s[t % 2]
                ob = f2[t % 2]
                k.dma('sp', ht[:, :], h_in[t * 128:(t + 1) * 128, :], 'ldh%d' % (t % 2), out_b=ht)
                for half in range(2):
                    for fc in range(32):
                        k.op('pe', lambda e, half=half, fc=fc: e.matmul(
                            ob[:, half * 512:(half + 1) * 512], lhsT=uT[:, fc, s * 128:(s + 1) * 128],
                            rhs=W2[:, fc, half * 512:(half + 1) * 512], start=(fc == 0), stop=(fc == 31)), reads=[uT, W2], writes=[ob])
                for half in range(2):
                    hsl = slice(half * 512, (half + 1) * 512)
                    k.op('dve', lambda e, hsl=hsl: e.tensor_tensor(out=h2[:, hsl], in0=ob[:, hsl], in1=ht[:, hsl], op=ALU.add), reads=[ob, ht], writes=[h2])
                k.dma('sp', h_out[t * 128:(t + 1) * 128, :], h2[:, :], 'sth%d' % (t % 2), in_b=h2)
        k.end_phase()

    def phase_o3(self, k, wg, wp, p_in, h_in, h_out, ple_gain_row, next_gain_row):
        k.begin_phase()
        Wg = k.sbuf([128, 8, 1024], BF16, 'Wg')
        Wp = k.sbuf([128, 2, 1024], BF16, 'Wp')
        self.load_w(k, Wg, wg, 8, 1024)
        self.load_w(k, Wp, wp, 2, 1024)
        gple = k.sbuf([128, 1024], F32, 'gple')
        self.load_bc(k, gple, gple[:, :], ple_gain_row, 1024)
        gain = None
        if next_gain_row is not None:
            gain = k.sbuf([128, 1024], F32, 'gain')
            self.load_bc(k, gain, gain[:, :], next_gain_row, 1024)
        psF = k.psum([128, 2048], F32)
        psB = k.psum([128, 3072], BF16)
        gb = Buf(psF[:, 0:1024], 'gb')
        pb = Buf(psF[:, 1024:2048], 'pb')
        psP = Buf(psB[:, 2048:2304], 'psP')
        nt = self.NormT(self, k, psB[:, 0:2048])
        xst = [k.sbuf([128, 8, 128], BF16, 'xst') for _ in range(2)]
        pts = [k.sbuf([128, 256], F32, 'pt') for _ in range(2)]
        pbf = [k.sbuf([128, 256], BF16, 'pbf') for _ in range(2)]
        pT = [k.sbuf([128, 2, 128], BF16, 'pT') for _ in range(2)]
        gs = [k.sbuf([128, 1024], F32, 'gs') for _ in range(2)]
        hts = [k.sbuf([128, 1024], F32, 'ht') for _ in range(2)]
        h3s = [k.sbuf([128, 1024], F32, 'h3') for _ in range(2)]
        stages = [k.sbuf([128, 8, 512], BF16, 'stg') for _ in range(2)] if gain is not None else None
        dstT = self.hnT[:, :, :].rearrange("c p t -> p c t")
        deferred = []
        for T in range(8):
            for s in range(4):
                t = 4 * T + s
                i2 = t % 2
                xs = xst[i2]
                k.dma('sp', pts[i2][:, :], p_in[t * 128:(t + 1) * 128, :], 'ldp%d' % i2, out_b=pts[i2])
                k.dma('sp', hts[i2][:, :], h_in[t * 128:(t + 1) * 128, :], 'ldh%d' % i2, out_b=hts[i2])
                nt.run(hts[i2], hts[i2][:, :], gple, xs, 0)
                k.op('pool', lambda e: e.tensor_copy(out=pbf[i2][:, :], in_=pts[i2][:, :]), reads=[pts[i2]], writes=[pbf[i2]])
                for c in range(2):
                    k.op('pe', lambda e, c=c: e.transpose(out=psP[:, c * 128:(c + 1) * 128], in_=pbf[i2][:, c * 128:(c + 1) * 128],
                                                          identity=self.ident[:, :]), reads=[pbf[i2], self.ident], writes=[psP])
                k.op('act', lambda e: e.copy(out=pT[i2][:, :, :], in_=psP[:, :].rearrange("p (c t) -> p c t", c=2)), reads=[psP], writes=[pT[i2]])
                for half in range(2):
                    for c in range(8):
                        k.op('pe', lambda e, half=half, c=c: e.matmul(
                            gb[:, half * 512:(half + 1) * 512], lhsT=xs[:, c, :],
                            rhs=Wg[:, c, half * 512:(half + 1) * 512], start=(c == 0), stop=(c == 7)), reads=[xs, Wg], writes=[gb])
                for half in range(2):
                    for c in range(2):
                        k.op('pe', lambda e, half=half, c=c: e.matmul(
                            pb[:, half * 512:(half + 1) * 512], lhsT=pT[i2][:, c, :],
                            rhs=Wp[:, c, half * 512:(half + 1) * 512], start=(c == 0), stop=(c == 1)), reads=[pT[i2], Wp], writes=[pb])
                for f in deferred:
                    f()
                deferred = []
                for half in range(2):
                    hsl = slice(half * 512, (half + 1) * 512)
                    k.op('act', lambda e, hsl=hsl: e.activation(out=gs[i2][:, hsl], in_=gb[:, hsl], func=AF.Sigmoid), reads=[gb], writes=[gs[i2]])
                for half in range(2):
                    hsl = slice(half * 512, (half + 1) * 512)
                    k.op('dve', lambda e, hsl=hsl: e.tensor_tensor(out=gs[i2][:, hsl], in0=pb[:, hsl], in1=gs[i2][:, hsl], op=ALU.mult), reads=[pb, gs[i2]], writes=[gs[i2]])
                k.op('pool', lambda e: e.tensor_tensor(out=h3s[i2][:, :], in0=gs[i2][:, :], in1=hts[i2][:, :], op=ALU.add),
                     reads=[gs[i2], hts[i2]], writes=[h3s[i2]])
                k.dma('sp', h_out[t * 128:(t + 1) * 128, :], h3s[i2][:, :], 'sth%d' % i2, in_b=h3s[i2])
                if gain is not None:
                    i = nt.run_a(h3s[i2], h3s[i2][:, :], gain)
                    deferred.append(lambda i=i, T=T, s=s: nt.run_b(i, stages[T % 2], s * 128))
            if gain is not None:
                deferred.append(lambda T=T: k.dma('sp', dstT[:, :, T * 512:(T + 1) * 512], stages[T % 2][:, :, :], 'st%d' % (T % 2), in_b=stages[T % 2]))
        for f in deferred:
            f()
        k.end_phase()

    def phase_p1(self, k):
        I = self.I
        k.begin_phase()
        Win = k.sbuf([128, 8, 3072], BF16, 'Win')
        self.load_w(k, Win, I['o_w_in'][0], 8, 3072)
        g64 = k.sbuf([128, 2, 64], F32)
        for i, nm in enumerate(['o_qn', 'o_kn']):
            self.load_bc(k, g64, g64[:, i, :], I[nm][0, :], 64)
        tokq = k.sbuf([128, 32, 4], F32); k.dma('sp', tokq[:, :, :], I['c_tokq'][:, :, :], 'cst', out_b=tokq)
        tokk = k.sbuf([128, 32, 4], F32); k.dma('sp', tokk[:, :, :], I['c_tokk'][:, :, :], 'cst', out_b=tokk)
        hq = k.sbuf([128, 16, 4], F32); self.load_bc(k, hq, hq[:, :, :].rearrange("p h c -> p (h c)"), I['c_hq1'][0, :], 64)
        hk = k.sbuf([128, 16, 4], F32); self.load_bc(k, hk, hk[:, :, :].rearrange("p h c -> p (h c)"), I['c_hk1'][0, :], 64)
        psF = k.psum([128, 3584], F32)
        psB = k.psum([128, 1024], BF16)
        bank = [Buf(psF[:, i * 512:(i + 1) * 512], 'bank%d' % i) for i in range(7)]
        psT = Buf(psB[:, :], 'psT')
        psKM = bank[6]
        psG = bank[6]
        R = 88
        hnTs = [k.sbuf([128, 8, 256], BF16, 'hnTs') for _ in range(2)]
        QTs = [k.sbuf([128, 16, 256], BF16, 'QTs') for _ in range(2)]
        KTs = [k.sbuf([128, 16, 256], BF16, 'KTs') for _ in range(2)]
        Vtm = [k.sbuf([128, 16, 128], BF16, 'Vtm') for _ in range(2)]
        Qtm = [k.sbuf([128, 16, 96], BF16, 'Qtm') for _ in range(2)]
        Ktm = [k.sbuf([128, 16, 96], BF16, 'Ktm') for _ in range(2)]
        for i in range(2):
            k.op('pool', lambda e, i=i: e.memset(Vtm[i][:, :, 64:128], 1.0), writes=[Vtm[i]])
            k.op('pool', lambda e, i=i: e.memset(Qtm[i][:, :, :], 0.0), writes=[Qtm[i]])
            k.op('pool', lambda e, i=i: e.memset(Ktm[i][:, :, :], 0.0), writes=[Ktm[i]])
            k.op('dve', lambda e, i=i: e.tensor_copy(out=Qtm[i][:, :, 80:84], in_=hq[:, :, :]), reads=[hq], writes=[Qtm[i]])
            k.op('dve', lambda e, i=i: e.tensor_copy(out=Ktm[i][:, :, 84:88], in_=hk[:, :, :]), reads=[hk], writes=[Ktm[i]])
        qs_l = [k.sbuf([128, 1024], F32, 'qs') for _ in range(2)]
        ks_l = [k.sbuf([128, 1024], F32, 'ks') for _ in range(2)]
        Kf = k.sbuf([128, 1024], F32, 'Kf')
        tmpA = k.sbuf([128, 1024], F32, 'tmpA')
        tmpB = k.sbuf([128, 1024], F32, 'tmpB')
        ssq = k.sbuf([128, 32], F32, 'ssq')
        rsq = k.sbuf([128, 32], F32, 'rsq')
        QgT = k.sbuf([64, 16, 128], BF16, 'QgT')
        kmA = k.sbuf([64, 16], F32, 'kmA')
        kmS = k.sbuf([64, 16], F32, 'kmS')
        kmH = k.sbuf([64, 16], F32, 'kmH')
        KmHi = k.sbuf([64, 16, 16], BF16, 'KmHi')
        KmLo = k.sbuf([64, 16, 16], BF16, 'KmLo')
        gate = k.sbuf([128, 16, 16], F32, 'gate')
        mx = k.sbuf([128, 16, 8], F32, 'mx')
        sel = k.sbuf([128, 16, 16], F32, 'sel')
        k.op('pool', lambda e: e.memset(gate[:, :, :], NEG), writes=[gate])
        k.op('pool', lambda e: e.memset(KmHi[:, :, :], 0.0), writes=[KmHi])
        k.op('pool', lambda e: e.memset(KmLo[:, :, :], 0.0), writes=[KmLo])

        srcT = self.hnT[:, :, :].rearrange("c p t -> p c t")
        QTd = self.QT[:, :, :].rearrange("h r t -> r h t")
        KTd = self.KT[:, :, :].rearrange("h r t -> r h t")
        Vd = self.V[:, :, :].rearrange("h t c -> t h c")

        k.dma('sp', hnTs[0][:, :, :], srcT[:, :, 0:256], 'ld0', out_b=hnTs[0])

        def stageA(t):
            T2, s = divmod(t, 2)
            hs = hnTs[T2 % 2]
            if s == 0 and T2 + 1 < 16:
                k.dma('sp', hnTs[(T2 + 1) % 2][:, :, :], srcT[:, :, (T2 + 1) * 256:(T2 + 2) * 256], 'ld%d' % ((T2 + 1) % 2),
                      out_b=hnTs[(T2 + 1) % 2])
            vt = Vtm[t % 2]
            qs, ks = qs_l[t % 2], ks_l[t % 2]
            for bk in range(6):
                for kc in range(8):
                    k.op('pe', lambda e, bk=bk, kc=kc: e.matmul(
                        bank[bk][:, :], lhsT=hs[:, kc, s * 128:(s + 1) * 128], rhs=Win[:, kc, bk * 512:(bk + 1) * 512],
                        start=(kc == 0), stop=(kc == 7)), reads=[hs, Win], writes=[bank[bk]])
            k.op('act', lambda e: e.copy(out=qs[:, 0:512], in_=bank[0][:, :]), reads=[bank[0]], writes=[qs])
            k.op('act', lambda e: e.copy(out=qs[:, 512:1024], in_=bank[1][:, :]), reads=[bank[1]], writes=[qs])
            k.op('act', lambda e: e.copy(out=ks[:, 0:512], in_=bank[2][:, :]), reads=[bank[2]], writes=[ks])
            k.op('act', lambda e: e.copy(out=ks[:, 512:1024], in_=bank[3][:, :]), reads=[bank[3]], writes=[ks])
            k.op('act', lambda e: e.copy(out=vt[:, 0:8, 0:64], in_=bank[4][:, :].rearrange("p (h d) -> p h d", h=8)), reads=[bank[4]], writes=[vt])
            k.op('act', lambda e: e.copy(out=vt[:, 8:16, 0:64], in_=bank[5][:, :].rearrange("p (h d) -> p h d", h=8)), reads=[bank[5]], writes=[vt])

        def stageB(t):
            T2, s = divmod(t, 2)
            own = T2
            qts, kts = QTs[T2 % 2], KTs[T2 % 2]
            vt, qt, kt = Vtm[t % 2], Qtm[t % 2], Ktm[t % 2]
            qs, ks = qs_l[t % 2], ks_l[t % 2]
            q3 = qs[:, :].rearrange("p (h d) -> p h d", h=16)
            k3 = ks[:, :].rearrange("p (h d) -> p h d", h=16)
            kf3 = Kf[:, :].rearrange("p (h d) -> p h d", h=16)
            tA3 = tmpA[:, :].rearrange("p (h d) -> p h d", h=16)
            tB3 = tmpB[:, :].rearrange("p (h d) -> p h d", h=16)
            k.op('dve', lambda e: e.tensor_tensor(out=tA3, in0=q3, in1=q3, op=ALU.mult), reads=[qs], writes=[tmpA])
            k.op('dve', lambda e: e.tensor_reduce(out=ssq[:, 0:16], in_=tA3, axis=AX.X, op=ALU.add), reads=[tmpA], writes=[ssq])
            k.op('pool', lambda e: e.tensor_tensor(out=tB3, in0=k3, in1=k3, op=ALU.mult), reads=[ks], writes=[tmpB])
            k.op('dve', lambda e: e.tensor_reduce(out=ssq[:, 16:32], in_=tB3, axis=AX.X, op=ALU.add), reads=[tmpB], writes=[ssq])
            self.rstd(k, ssq, ssq[:, :], rsq, rsq[:, :], 64)
            k.op('dve', lambda e: e.tensor_tensor(out=tA3, in0=q3, in1=rsq[:, 0:16].unsqueeze(2).to_broadcast([128, 16, 64]), op=ALU.mult),
                 reads=[qs, rsq], writes=[tmpA])
            k.op('pool', lambda e: e.tensor_tensor(out=qt[:, :, 0:64], in0=tA3, in1=g64[:, 0, :].unsqueeze(1).to_broadcast([128, 16, 64]), op=ALU.mult),
                 reads=[tmpA, g64], writes=[qt])
            k.op('dve', lambda e: e.tensor_tensor(out=tB3, in0=k3, in1=rsq[:, 16:32].unsqueeze(2).to_broadcast([128, 16, 64]), op=ALU.mult),
                 reads=[ks, rsq], writes=[tmpB])
            k.op('pool', lambda e: e.tensor_tensor(out=kf3, in0=tB3, in1=g64[:, 1, :].unsqueeze(1).to_broadcast([128, 16, 64]), op=ALU.mult),
                 reads=[tmpB, g64], writes=[Kf])
            k.op('pool', lambda e: e.tensor_copy(out=kt[:, :, 0:64], in_=kf3), reads=[Kf], writes=[kt])
            k.op('pool', lambda e: e.memset(kt[:, :, 64:80], 0.0), writes=[kt])
            k.op('pool', lambda e: e.memset(kt[:, :, 64 + own:65 + own], 1.0), writes=[kt])
            k.op('pool', lambda e: e.tensor_copy(out=kt[:, :, 80:84], in_=tokk[:, t, :].unsqueeze(1).to_broadcast([128, 16, 4])),
                 reads=[tokk], writes=[kt])
            k.op('pool', lambda e: e.tensor_copy(out=qt[:, :, 84:88], in_=tokq[:, t, :].unsqueeze(1).to_broadcast([128, 16, 4])),
                 reads=[tokq], writes=[qt])
            if own == 0:
                k.op('pool', lambda e: e.memset(qt[:, :, 64:80], 0.0), writes=[qt])
            else:
                for r in range(2):
                    for hh in range(8):
                        k.op('pe', lambda e, r=r, hh=hh: e.transpose(out=psT[0:64, hh * 128:(hh + 1) * 128], in_=qt[:, r * 8 + hh, 0:64],
                                                                     identity=self.ident[:, :]), reads=[qt, self.ident], writes=[psT])
                    k.op('act', lambda e, r=r: e.copy(out=QgT[0:64, r * 8:(r + 1) * 8, :], in_=psT[0:64, :].rearrange("p (h t) -> p h t", h=8)),
                         reads=[psT], writes=[QgT])
                for hh in range(16):
                    k.op('pe', lambda e, hh=hh: e.matmul(psG[:, 256 + hh * 16:256 + hh * 16 + own], lhsT=QgT[0:64, hh, :], rhs=KmHi[0:64, hh, 0:own],
                                                         start=True, stop=False), reads=[QgT, KmHi], writes=[psG])
                    k.op('pe', lambda e, hh=hh: e.matmul(psG[:, 256 + hh * 16:256 + hh * 16 + own], lhsT=QgT[0:64, hh, :], rhs=KmLo[0:64, hh, 0:own],
                                                         start=False, stop=True), reads=[QgT, KmLo], writes=[psG])
                k.op('dve', lambda e: e.tensor_copy(out=gate[:, :, 0:own], in_=psG[:, 256:512].rearrange("p (h n) -> p h n", h=16)[:, :, 0:own]),
                     reads=[psG], writes=[gate])
                for hh in range(16):
                    k.op('dve', lambda e, hh=hh: e.max(out=mx[:, hh, :], in_=gate[:, hh, :]), reads=[gate], writes=[mx])
                k.op('dve', lambda e: e.tensor_tensor(out=sel[:, :, :], in0=gate[:, :, :], in1=mx[:, :, 2:3].to_broadcast([128, 16, 16]), op=ALU.is_ge),
                     reads=[gate, mx], writes=[sel])
                k.op('dve', lambda e: e.tensor_scalar(out=qt[:, :, 64:80], in0=sel[:, :, :], scalar1=1.0, scalar2=BIG, op0=ALU.subtract, op1=ALU.mult),
                     reads=[sel], writes=[qt])
                k.op('pool', lambda e: e.memset(qt[:, :, 64 + own:65 + own], 0.0), writes=[qt])
            for hh in range(16):
                k.op('pe', lambda e, hh=hh: e.matmul(psKM[0:64, hh:hh + 1], lhsT=Kf[:, hh * 64:(hh + 1) * 64], rhs=self.onesf[:, 0:1],
                                                     start=True, stop=True), reads=[Kf, self.onesf], writes=[psKM])
            if s == 0:
                k.op('act', lambda e: e.copy(out=kmA[:, :], in_=psKM[0:64, 0:16]), reads=[psKM], writes=[kmA])
            else:
                k.op('dve', lambda e: e.tensor_tensor(out=kmS[:, :], in0=psKM[0:64, 0:16], in1=kmA[:, :], op=ALU.add), reads=[psKM, kmA], writes=[kmS])
                k.op('dve', lambda e: e.tensor_scalar(out=kmS[:, :], in0=kmS[:, :], scalar1=1.0 / 256, scalar2=None, op0=ALU.mult), reads=[kmS], writes=[kmS])
                k.op('dve', lambda e: e.tensor_copy(out=KmHi[:, :, own], in_=kmS[:, :]), reads=[kmS], writes=[KmHi])
                k.op('dve', lambda e: e.tensor_copy(out=kmH[:, :], in_=KmHi[:, :, own]), reads=[KmHi], writes=[kmH])
                k.op('dve', lambda e: e.tensor_tensor(out=KmLo[:, :, own], in0=kmS[:, :], in1=kmH[:, :], op=ALU.subtract), reads=[kmS, kmH], writes=[KmLo])
            for (src, dst) in ((qt, qts), (kt, kts)):
                for r in range(2):
                    for hh in range(8):
                        k.op('pe', lambda e, src=src, r=r, hh=hh: e.transpose(
                            out=psT[0:R, hh * 128:(hh + 1) * 128], in_=src[:, r * 8 + hh, 0:R], identity=self.ident[:, :]),
                            reads=[src, self.ident], writes=[psT])
                    k.op('act', lambda e, dst=dst, r=r: e.copy(
                        out=dst[0:R, r * 8:(r + 1) * 8, s * 128:(s + 1) * 128], in_=psT[0:R, :].rearrange("p (h t) -> p h t", h=8)),
                        reads=[psT], writes=[dst])
            k.dma('sp', Vd[t * 128:(t + 1) * 128, :, :], vt[:, :, :], 'stv%d' % (t % 2), in_b=vt)
            if s == 1:
                c0 = T2 * 256
                k.dma('sp', QTd[0:R, :, c0:c0 + 256], qts[0:R, :, :], 'stq%d' % (T2 % 2), in_b=qts)
                k.dma('sp', KTd[0:R, :, c0:c0 + 256], kts[0:R, :, :], 'stk%d' % (T2 % 2), in_b=kts)

        for i in range(33):
            if i < 32:
                stageA(i)
            if i >= 1:
                stageB(i - 1)
        k.end_phase()

    def build(self):
        nc, I = self.nc, self.I
        phases = self.cfg.get('phases', None)

        def on(name):
            return phases is None or name in phases

        with ExitStack() as es:
            es.enter_context(nc.Block())
            k = KB(nc, es)
            self.k = k
            self.setup_globals(k)
            heads0 = [dict(R=96, scale=96 ** -0.5, mask='c')] * 8 + [dict(R=72, scale=0.125, mask='d')] * 8
            heads1 = [dict(R=88, scale=0.125, mask='c')] * 16
            if on('n0'):
                self.phase_norm0(k)
            if on('p0'):
                self.phase_p0(k)
            if on('a0'):
                self.phase_attn(k, heads0)
            if on('o1_0'):
                self.phase_o1(k, I['e_w_out'][0], I['x'], self.hA, I['ff_norm'][0, :])
            if on('o2_0'):
                self.phase_o2(k, I['w_ff1'][0], I['w_ff2'][0], self.hA, self.hB)
            if on('o3_0'):
                self.phase_o3(k, I['w_ple_gate'][0], I['w_ple_proj'][0], I['p'][0], self.hB, self.hC, I['ple_norm'][0, :], I['mix_norm'][1, :])
            if on('p1'):
                self.phase_p1(k)
            if on('a1'):
                self.phase_attn(k, heads1)
            if on('o1_1'):
                self.phase_o1(k, I['o_w_out'][0], self.hC, self.hA, I['ff_norm'][1, :])
            if on('o2_1'):
                self.phase_o2(k, I['w_ff1'][1], I['w_ff2'][1], self.hA, self.hB)
            if on('o3_1'):
                self.phase_o3(k, I['w_ple_gate'][1], I['w_ple_proj'][1], I['p'][1], self.hB, self.y, I['ple_norm'][1, :], None)
            k.barrier()
        return nc


def make_in_maps(inputs, consts, n_cores=8, extra=None):
    maps = []
    shared = {}
    for name in IN_SHAPES:
        if name in ('x', 'p', 'positions'):
            continue
        shared[name] = np.ascontiguousarray(np.asarray(inputs[name], dtype=np.float32))
    shared.update(consts)
    for c in range(n_cores):
        m = dict(shared)
        m['x'] = np.ascontiguousarray(np.asarray(inputs['x'][c], dtype=np.float32))
        m['p'] = np.ascontiguousarray(np.asarray(inputs['p'][:, c], dtype=np.float32))
        pos = np.asarray(inputs['positions'][c], dtype=np.int32)
        m['positions'] = np.ascontiguousarray(pos.reshape(32, 128).T)
        if extra:
            m.update(extra[c])
        maps.append(m)
    return maps


def kernel(**inputs):
    prog = Prog()
    nc = prog.build()
    maps = make_in_maps(inputs, host_consts())
    res = run_bass_kernel_spmd(nc, maps, core_ids=list(range(8)))
    out = np.stack([np.asarray(r["y"], dtype=np.float32) for r in res.results], axis=0)
    return out
```

```python
import numpy as np
from contextlib import ExitStack
import concourse.bass as bass
import concourse.mybir as mybir
from concourse.bass_utils import run_bass_kernel_spmd

F32 = mybir.dt.float32
BF16 = mybir.dt.bfloat16
I32 = mybir.dt.int32
ALU = mybir.AluOpType
AF = mybir.ActivationFunctionType
AX = mybir.AxisListType

S = 4096
D = 1024
NT = 32
EPS = 1e-6
BIG = 4096.0
NEG = -1.0e30
MAGIC = 12582912.0
TWO_PI = 2.0 * np.pi


class Buf:
    def __init__(self, t, name=''):
        self.t = t
        self.name = name
        self.w = None
        self.r = {}

    def __getitem__(self, idx):
        return self.t[idx]


class KB:
    ENG = ['pe', 'act', 'dve', 'pool', 'sp']

    def __init__(self, nc, es, n_dsem=20):
        self.nc = nc
        self.es = es
        self.e = {'pe': nc.tensor, 'act': nc.scalar, 'dve': nc.vector, 'pool': nc.gpsimd, 'sp': nc.sync}
        self.sem = {n: es.enter_context(nc.semaphore('s_' + n)) for n in self.ENG}
        self.cnt = {n: 0 for n in self.ENG}
        self.seen = {n: {} for n in self.ENG}
        self.dsems = [[es.enter_context(nc.semaphore('d_%d' % i)), 0] for i in range(n_dsem)]
        self.dnames = {}
        self.nbuf = 0
        self.pes = None
        self.ninst = 0

    def begin_phase(self):
        self.pes = ExitStack()
        self.dnames = {}

    def end_phase(self):
        self.barrier()
        self.pes.close()
        self.pes = None

    def sbuf(self, shape, dt, name=None, glob=False):
        self.nbuf += 1
        name = (name or 'b') + '_%d' % self.nbuf
        st = self.es if glob else self.pes
        t = st.enter_context(self.nc.sbuf_tensor(name, list(shape), dt))
        return Buf(t, name)

    def psum(self, shape, dt, name=None):
        self.nbuf += 1
        name = (name or 'p') + '_%d' % self.nbuf
        t = self.pes.enter_context(self.nc.psum_tensor(name, list(shape), dt))
        return t

    def dsem(self, name):
        if name not in self.dnames:
            self.dnames[name] = len(self.dnames)
            assert len(self.dnames) <= len(self.dsems), 'too many dma sems'
        return self.dnames[name]

    def _need(self, eng, deps):
        E = self.e[eng]
        best = {}
        for key, val in deps:
            if val > best.get(key, 0):
                best[key] = val
        for key, val in best.items():
            if key == ('e', 'pe') and eng == 'pe':
                continue
            if self.seen[eng].get(key, 0) >= val:
                continue
            if key[0] == 'e':
                E.wait_ge(self.sem[key[1]], val)
            else:
                E.wait_ge(self.dsems[key[1]][0], val)
            self.seen[eng][key] = val
            self.ninst += 1

    @staticmethod
    def _deps(reads, writes):
        deps = []
        for b in reads:
            if b.w is not None:
                deps.append((b.w[0:2], b.w[2]))
        for b in writes:
            if b.w is not None:
                deps.append((b.w[0:2], b.w[2]))
            for key, val in b.r.items():
                deps.append((key, val))
        return deps

    def op(self, eng, fn, reads=(), writes=()):
        self._need(eng, self._deps(reads, writes))
        ins = fn(self.e[eng])
        self.cnt[eng] += 1
        c = self.cnt[eng]
        ins.then_inc(self.sem[eng], 1)
        self.ninst += 1
        for b in reads:
            b.r[('e', eng)] = c
        for b in writes:
            b.w = ('e', eng, c)
            b.r = {}
        return ins

    def dma(self, q, out_ap, in_ap, sem, out_b=None, in_b=None, **kw):
        self._need(q, self._deps([in_b] if in_b else [], [out_b] if out_b else []))
        ins = self.e[q].dma_start(out=out_ap, in_=in_ap, **kw)
        si = self.dsem(sem)
        s = self.dsems[si]
        s[1] += 16
        ins.then_inc(s[0], 16)
        self.ninst += 1
        if in_b is not None:
            in_b.r[('d', si)] = s[1]
        if out_b is not None:
            out_b.w = ('d', si, s[1])
            out_b.r = {}
        return ins

    def barrier(self):
        for eng in self.ENG:
            deps = [(('e', x), self.cnt[x]) for x in self.ENG if x != eng and self.cnt[x] > 0]
            deps += [(('d', i), s[1]) for i, s in enumerate(self.dsems) if s[1] > 0]
            self._need(eng, deps)


def _bf16_round(x):
    x = np.asarray(x, np.float32)
    u = x.view(np.uint32)
    r = ((u >> 16) & 1) + 0x7FFF
    return ((u + r) & 0xFFFF0000).view(np.float32)


def _slopes(n):
    return (2.0 ** (-8.0 * np.arange(1, n + 1, dtype=np.float64) / n))


def host_consts():
    c = {}
    kk = np.arange(128)[:, None]
    qq = np.arange(128)[None, :]
    blocks = []
    for dl in range(-3, 4):
        dist = 128 * dl + qq - kk
        blocks.append(np.where(dist >= 0, 0.0, -BIG))
    c['c_maskc'] = np.concatenate(blocks, 1).astype(np.float32)
    blocks = []
    for dl in range(-3, 20):
        dist = 128 * dl + qq - kk
        m = ((dist >= 0) & (dist <= 128)).astype(np.float64) + ((dist >= 0) & (dist <= 512) & (dist % 4 == 0)) \
            + ((dist >= 0) & (dist <= 2048) & (dist % 16 == 0))
        with np.errstate(divide='ignore'):
            v = np.where(m > 0, 8.0 * np.log(np.maximum(m, 1e-30)), -BIG)
        blocks.append(v)
    c['c_maskd'] = np.concatenate(blocks, 1).astype(np.float32)
    half = 16
    inv = (10000.0 ** (-np.arange(half, dtype=np.float32) / half)).astype(np.float32)
    c['c_invf'] = (inv / np.float32(TWO_PI)).astype(np.float32).reshape(1, 16)
    p = np.arange(128, dtype=np.float32)[:, None]
    t = np.arange(32, dtype=np.float32)[None, :]
    z = np.zeros((128, 32), np.float32)
    c['c_tokq'] = np.stack([-(p + z), -(p + z), -128.0 * (t + z), -128.0 * (t + z)], -1).astype(np.float32)
    c['c_tokk'] = np.stack([p + z, p + z, t + z, t + z], -1).astype(np.float32)
    for nm, nh in (('0', 8), ('1', 16)):
        M = 8.0 * _slopes(nh)
        hi = _bf16_round(M.astype(np.float32))
        lo = _bf16_round((M - hi.astype(np.float64)).astype(np.float32))
        c['c_hq' + nm] = np.stack([hi, lo, 128.0 * hi, 128.0 * lo], -1).astype(np.float32).reshape(1, nh * 4)
        c['c_hk' + nm] = np.stack([hi, lo, hi, lo], -1).astype(np.float32).reshape(1, nh * 4)
    return c


CONST_SHAPES = {
    'c_maskc': [128, 7 * 128], 'c_maskd': [128, 23 * 128], 'c_invf': [1, 16],
    'c_tokq': [128, 32, 4], 'c_tokk': [128, 32, 4],
    'c_hq0': [1, 32], 'c_hk0': [1, 32], 'c_hq1': [1, 64], 'c_hk1': [1, 64],
}

IN_SHAPES = {
    'x': ([S, D], F32), 'p': ([2, S, 256], F32), 'positions': ([128, 32], I32),
    'e_w_in': ([1, 1024, 2208], F32), 'e_cq_norm': ([1, 384], F32), 'e_ckv_norm': ([1, 256], F32),
    'e_w_uq': ([1, 384, 768], F32), 'e_w_ukv': ([1, 256, 1024], F32),
    'e_qn_nope': ([1, 64], F32), 'e_qn_rope': ([1, 32], F32), 'e_kn_nope': ([1, 64], F32), 'e_kn_rope': ([1, 32], F32),
    'e_dil_qn': ([1, 64], F32), 'e_dil_kn': ([1, 64], F32), 'e_w_out': ([1, 1024, 1024], F32),
    'o_w_in': ([1, 1024, 3072], F32), 'o_qn': ([1, 64], F32), 'o_kn': ([1, 64], F32), 'o_w_out': ([1, 1024, 1024], F32),
    'mix_norm': ([2, 1024], F32), 'ff_norm': ([2, 1024], F32), 'w_ff1': ([2, 1024, 4096], F32),
    'w_ff2': ([2, 4096, 1024], F32), 'ple_norm': ([2, 1024], F32), 'w_ple_gate': ([2, 1024, 1024], F32),
    'w_ple_proj': ([2, 256, 1024], F32),
}


class Prog:
    def __init__(self, cfg=None):
        cfg = cfg or {}
        self.cfg = cfg
        self.nc = bass.Bass("TRN2", target_bir_lowering=False)
        nc = self.nc
        self.I = {}
        for name, (shape, dt) in IN_SHAPES.items():
            self.I[name] = nc.dram_tensor(name, shape, dt, kind="ExternalInput")
        for name, shape in CONST_SHAPES.items():
            self.I[name] = nc.dram_tensor(name, shape, F32, kind="ExternalInput")
        self.y = nc.dram_tensor("y", [S, D], F32, kind="ExternalOutput")
        ext_in = cfg.get('scr_in', ())
        ext_out = cfg.get('scr_out', ())

        def scr(name, shape, dt):
            if name in ext_in:
                return nc.dram_tensor(name, shape, dt, kind="ExternalInput")
            if name in ext_out:
                return nc.dram_tensor(name, shape, dt, kind="ExternalOutput")
            return nc.dram_tensor(name, shape, dt)

        self.hnT = scr('hnT', [8, 128, S], BF16)
        self.QT = scr('QT', [16, 128, S], BF16)
        self.KT = scr('KT', [16, 128, S], BF16)
        self.V = scr('V', [16, S, 128], BF16)
        self.mixT = scr('mixT', [8, 128, S], BF16)
        self.hA = scr('hA', [S, D], F32)
        self.hB = scr('hB', [S, D], F32)
        self.hC = scr('hC', [S, D], F32)

    def load_bc(self, k, dst, dst_ap, row_ap, n, sem='cst'):
        k.dma('sp', dst_ap, row_ap.partition_broadcast(128), sem, out_b=dst)

    def load_w(self, k, dst, w2d, KC, N, sem='w'):
        src = w2d.rearrange("(kc p) n -> p kc n", p=128)
        si = k.dsem(sem)
        vals = []
        for k0 in range(0, KC, 8):
            k1 = min(KC, k0 + 8)
            for c0 in range(0, N, 512):
                n = min(512, N - c0)
                if len(vals) >= 6:
                    k._need('pool', [(('d', si), vals[-6])])
                k.dma('pool', dst[:, k0:k1, c0:c0 + n], src[:, k0:k1, c0:c0 + n], sem, out_b=dst)
                vals.append(k.dsems[si][1])

    def rstd(self, k, ss, ss_ap, out, out_ap, n):
        k.op('act', lambda e: e.activation(out=out_ap, in_=ss_ap, func=AF.Sqrt, scale=1.0 / n, bias=self.epsb[:, 0:1]),
             reads=[ss, self.epsb], writes=[out])
        k.op('dve', lambda e: e.reciprocal(out=out_ap, in_=out_ap), reads=[out], writes=[out])

    def setup_globals(self, k):
        self.ident = k.sbuf([128, 128], BF16, 'ident', glob=True)
        self.onesf = k.sbuf([128, 128], F32, 'onesf', glob=True)
        self.epsb = k.sbuf([128, 1], F32, 'epsb', glob=True)
        self.onesb = k.sbuf([128, 2], BF16, 'onesb', glob=True)
        identf = k.sbuf([128, 128], F32, 'identf', glob=True)
        k.op('pool', lambda e: e.memset(self.onesf[:, :], 1.0), writes=[self.onesf])
        k.op('pool', lambda e: e.memset(self.epsb[:, :], EPS), writes=[self.epsb])
        k.op('pool', lambda e: e.memset(self.onesb[:, :], 1.0), writes=[self.onesb])
        k.op('pool', lambda e: e.affine_select(out=identf[:, :], in_=self.onesf[:, :], pattern=[[-1, 128]],
                                               compare_op=ALU.is_equal, fill=0.0, base=0, channel_multiplier=1),
             reads=[self.onesf], writes=[identf])
        k.op('dve', lambda e: e.tensor_copy(out=self.ident[:, :], in_=identf[:, :]), reads=[identf], writes=[self.ident])

    class NormT:
        def __init__(self, P, k, psB):
            self.P = P
            self.k = k
            self.junk = [k.sbuf([128, 1024], BF16, 'nj') for _ in range(2)]
            self.ss = [k.sbuf([128, 2], F32, 'nss') for _ in range(2)]
            self.hn = [k.sbuf([128, 1024], BF16, 'nhn') for _ in range(2)]
            self.ps = [Buf(psB[:, i * 1024:(i + 1) * 1024], 'npsT%d' % i) for i in range(psB.shape[1] // 1024)]
            self.i = 0

        def run_a(self, hbuf, hap, gain):
            k, P = self.k, self.P
            i = self.i
            self.i += 1
            junk, ss, hn = self.junk[i % 2], self.ss[i % 2], self.hn[i % 2]
            k.op('act', lambda e: e.activation(out=junk[:, :], in_=hap, func=AF.Square, accum_out=ss[:, 0:1]),
                 reads=[hbuf], writes=[junk, ss])
            P.rstd(k, ss, ss[:, 0:1], ss, ss[:, 1:2], 1024)
            k.op('dve', lambda e: e.scalar_tensor_tensor(out=hn[:, :], in0=hap, scalar=ss[:, 1:2], in1=gain[:, :],
                                                         op0=ALU.mult, op1=ALU.mult), reads=[hbuf, ss, gain], writes=[hn])
            return i

        def run_b(self, i, stage, col0):
            k, P = self.k, self.P
            hn = self.hn[i % 2]
            ps = self.ps[i % len(self.ps)]
            for c in range(8):
                k.op('pe', lambda e, c=c: e.transpose(out=ps[:, c * 128:(c + 1) * 128], in_=hn[:, c * 128:(c + 1) * 128],
                                                      identity=P.ident[:, :]), reads=[hn, P.ident], writes=[ps])
            k.op('act', lambda e: e.copy(out=stage[:, :, col0:col0 + 128], in_=ps[:, :].rearrange("p (c t) -> p c t", c=8)),
                 reads=[ps], writes=[stage])

        def run(self, hbuf, hap, gain, stage, col0):
            i = self.run_a(hbuf, hap, gain)
            self.run_b(i, stage, col0)

    def phase_norm0(self, k):
        I = self.I
        k.begin_phase()
        gain = k.sbuf([128, 1024], F32, 'gain')
        self.load_bc(k, gain, gain[:, :], I['mix_norm'][0, :], 1024)
        psB = k.psum([128, 2048], BF16)
        nt = self.NormT(self, k, psB)
        hts = [k.sbuf([128, 1024], F32, 'ht') for _ in range(3)]
        stages = [k.sbuf([128, 8, 512], BF16, 'stg') for _ in range(2)]
        dstT = self.hnT[:, :, :].rearrange("c p t -> p c t")
        deferred = []
        for T in range(8):
            stage = stages[T % 2]
            for s in range(4):
                t = 4 * T + s
                ht = hts[t % 3]
                k.dma('sp', ht[:, :], I['x'][t * 128:(t + 1) * 128, :], 'ld%d' % (t % 3), out_b=ht)
                i = nt.run_a(ht, ht[:, :], gain)
                for f in deferred:
                    f()
                deferred = [lambda i=i, stage=stage, s=s: nt.run_b(i, stage, s * 128)]
            deferred.append(lambda T=T, stage=stage: k.dma('sp', dstT[:, :, T * 512:(T + 1) * 512], stage[:, :, :], 'st%d' % (T % 2), in_b=stage))
        for f in deferred:
            f()
        k.end_phase()

    def phase_p0(self, k):
        I = self.I
        k.begin_phase()
        Win = k.sbuf([128, 8, 2208], BF16, 'Win')
        Wuq = k.sbuf([128, 3, 768], BF16, 'Wuq')
        Wukv = k.sbuf([128, 2, 1024], BF16, 'Wukv')
        self.load_w(k, Win, I['e_w_in'][0], 8, 2208)
        self.load_w(k, Wuq, I['e_w_uq'][0], 3, 768)
        self.load_w(k, Wukv, I['e_w_ukv'][0], 2, 1024)
        g_cq = k.sbuf([128, 384], F32); self.load_bc(k, g_cq, g_cq[:, :], I['e_cq_norm'][0, :], 384)
        g_ckv = k.sbuf([128, 256], F32); self.load_bc(k, g_ckv, g_ckv[:, :], I['e_ckv_norm'][0, :], 256)
        g64 = k.sbuf([128, 4, 64], F32)
        for i, nm in enumerate(['e_qn_nope', 'e_kn_nope', 'e_dil_qn', 'e_dil_kn']):
            self.load_bc(k, g64, g64[:, i, :], I[nm][0, :], 64)
        gcol = k.sbuf([128, 4], F32, 'gcol')
        k.op('pool', lambda e: e.memset(gcol[:, :], 1.0), writes=[gcol])
        for i, nm in enumerate(['e_qn_nope', 'e_kn_nope', 'e_dil_qn', 'e_dil_kn']):
            k.dma('sp', gcol[0:64, i:i + 1], I[nm][0, :].rearrange("(p o) -> p o", o=1), 'cst', out_b=gcol)
        g32 = k.sbuf([128, 2, 32], F32)
        for i, nm in enumerate(['e_qn_rope', 'e_kn_rope']):
            self.load_bc(k, g32, g32[:, i, :], I[nm][0, :], 32)
        tokq = k.sbuf([128, 32, 4], F32); k.dma('sp', tokq[:, :, :], I['c_tokq'][:, :, :], 'cst', out_b=tokq)
        tokk = k.sbuf([128, 32, 4], F32); k.dma('sp', tokk[:, :, :], I['c_tokk'][:, :, :], 'cst', out_b=tokk)
        hq = k.sbuf([128, 8, 4], F32); self.load_bc(k, hq, hq[:, :, :].rearrange("p h c -> p (h c)"), I['c_hq0'][0, :], 32)
        hk = k.sbuf([128, 8, 4], F32); self.load_bc(k, hk, hk[:, :, :].rearrange("p h c -> p (h c)"), I['c_hk0'][0, :], 32)
        invf = k.sbuf([128, 16], F32); self.load_bc(k, invf, invf[:, :], I['c_invf'][0, :], 16)
        posi = k.sbuf([128, 32], I32); k.dma('sp', posi[:, :], I['positions'][:, :], 'cst', out_b=posi)
        posf = k.sbuf([128, 32], F32)
        k.op('dve', lambda e: e.tensor_copy(out=posf[:, :], in_=posi[:, :]), reads=[posi], writes=[posf])
        xt = k.sbuf([128, 32, 16], F32)
        k.op('dve', lambda e: e.tensor_tensor(out=xt[:, :, :], in0=posf[:, :].unsqueeze(2).to_broadcast([128, 32, 16]),
                                              in1=invf[:, :].unsqueeze(1).to_broadcast([128, 32, 16]), op=ALU.mult),
             reads=[posf, invf], writes=[xt])
        sin_all = k.sbuf([128, 32, 16], F32)
        cos_all = k.sbuf([128, 32, 16], F32)
        t1 = k.sbuf([128, 32, 16], F32)
        t2 = k.sbuf([128, 32, 16], F32)
        for dst, shift in ((sin_all, 0.0), (cos_all, 0.25)):
            k.op('dve', lambda e, shift=shift: e.tensor_scalar(out=t2[:, :, :], in0=xt[:, :, :], scalar1=shift, scalar2=None, op0=ALU.add),
                 reads=[xt], writes=[t2])
            k.op('dve', lambda e: e.tensor_scalar(out=t1[:, :, :], in0=t2[:, :, :], scalar1=MAGIC, scalar2=None, op0=ALU.add),
                 reads=[t2], writes=[t1])
            k.op('dve', lambda e: e.scalar_tensor_tensor(out=t1[:, :, :], in0=t1[:, :, :], scalar=MAGIC, in1=t2[:, :, :],
                                                         op0=ALU.subtract, op1=ALU.subtract), reads=[t1, t2], writes=[t1])
            k.op('act', lambda e, dst=dst: e.activation(out=dst[:, :, :], in_=t1[:, :, :], func=AF.Sin, scale=-TWO_PI * (1 - 1e-6)),
                 reads=[t1], writes=[dst])

        psF = k.psum([128, 3584], F32)
        psB = k.psum([128, 1024], BF16)
        bank = [Buf(psF[:, i * 512:(i + 1) * 512], 'bank%d' % i) for i in range(7)]
        psT = Buf(psB[:, :], 'psT')

        hnTs = [k.sbuf([128, 8, 256], BF16, 'hnTs') for _ in range(2)]
        QTs = [k.sbuf([128, 16, 256], BF16, 'QTs') for _ in range(2)]
        KTs = [k.sbuf([128, 16, 256], BF16, 'KTs') for _ in range(2)]
        Vtm = [k.sbuf([128, 16, 128], BF16, 'Vtm') for _ in range(2)]
        Qtm = [k.sbuf([128, 16, 96], BF16, 'Qtm') for _ in range(2)]
        Ktm = [k.sbuf([128, 16, 96], BF16, 'Ktm') for _ in range(2)]
        for i in range(2):
            k.op('pool', lambda e, i=i: e.memset(Vtm[i][:, :, 64:128], 1.0), writes=[Vtm[i]])
            k.op('pool', lambda e, i=i: e.memset(Qtm[i][:, :, :], 0.0), writes=[Qtm[i]])
            k.op('pool', lambda e, i=i: e.memset(Ktm[i][:, :, :], 0.0), writes=[Ktm[i]])
            k.op('dve', lambda e, i=i: e.tensor_copy(out=Qtm[i][:, 8:16, 64:68], in_=hq[:, :, :]), reads=[hq], writes=[Qtm[i]])
            k.op('dve', lambda e, i=i: e.tensor_copy(out=Ktm[i][:, 8:16, 68:72], in_=hk[:, :, :]), reads=[hk], writes=[Ktm[i]])
        junk = k.sbuf([128, 512], F32, 'junk')
        ssA = k.sbuf([128, 8], F32, 'ssA')
        krs = k.sbuf([128, 32], F32, 'krs')
        krn = k.sbuf([128, 32], F32, 'krn')
        krr_l = [k.sbuf([128, 32], F32, 'krr') for _ in range(2)]
        rka = k.sbuf([128, 1, 16], F32, 'rka')
        rkb = k.sbuf([128, 1, 16], F32, 'rkb')
        cq_bf = k.sbuf([128, 384], BF16, 'cq_bf')
        ckv_bf = k.sbuf([128, 256], BF16, 'ckv_bf')
        cT = k.sbuf([128, 5, 128], BF16, 'cT')
        qs_l = [k.sbuf([128, 768], F32, 'qs') for _ in range(2)]
        kvs_l = [k.sbuf([128, 1024], F32, 'kvs') for _ in range(2)]
        dqs_l = [k.sbuf([128, 512], F32, 'dqs') for _ in range(2)]
        dks_l = [k.sbuf([128, 512], F32, 'dks') for _ in range(2)]
        tmpA = k.sbuf([128, 1024], F32, 'tmpA')
        tmpB = k.sbuf([128, 1024], F32, 'tmpB')
        ssq = k.sbuf([128, 40], F32, 'ssq')
        rsq = k.sbuf([128, 40], F32, 'rsq')
        qrn = k.sbuf([128, 8, 32], F32, 'qrn')
        ra = k.sbuf([128, 8, 16], F32, 'ra')
        rb = k.sbuf([128, 8, 16], F32, 'rb')
        rc = k.sbuf([128, 8, 16], F32, 'rc')
        rd = k.sbuf([128, 8, 16], F32, 'rd')

        srcT = self.hnT[:, :, :].rearrange("c p t -> p c t")
        QTd = self.QT[:, :, :].rearrange("h r t -> r h t")
        KTd = self.KT[:, :, :].rearrange("h r t -> r h t")
        Vd = self.V[:, :, :].rearrange("h t c -> t h c")

        def hview(ap, h, d):
            return ap.rearrange("p (h d) -> p h d", h=h)

        def sumsq(src3, H, dh, dst_ap, tmp):
            t3 = tmp[:, 0:H * dh].rearrange("p (h d) -> p h d", h=H)
            k.op('dve', lambda e: e.tensor_tensor(out=t3, in0=src3[1], in1=src3[1], op=ALU.mult), reads=[src3[0]], writes=[tmp])
            k.op('dve', lambda e: e.tensor_reduce(out=dst_ap, in_=t3, axis=AX.X, op=ALU.add), reads=[tmp], writes=[ssq])

        def headnorm(src3, H, dh, rs_ap, gain_ap, dst, dst_ap, tmp, eng2='pool'):
            t3 = tmp[:, 0:H * dh].rearrange("p (h d) -> p h d", h=H)
            k.op('dve', lambda e: e.tensor_tensor(out=t3, in0=src3[1], in1=rs_ap.unsqueeze(2).to_broadcast([128, H, dh]), op=ALU.mult),
                 reads=[src3[0], rsq], writes=[tmp])
            k.op(eng2, lambda e: e.tensor_tensor(out=dst_ap, in0=t3, in1=gain_ap.unsqueeze(1).to_broadcast([128, H, dh]), op=ALU.mult),
                 reads=[tmp, g64, g32], writes=[dst])

        k.dma('sp', hnTs[0][:, :, :], srcT[:, :, 0:256], 'ld0', out_b=hnTs[0])

        def stageA(t):
            T2, s = divmod(t, 2)
            hs = hnTs[T2 % 2]
            if s == 0 and T2 + 1 < 16:
                k.dma('sp', hnTs[(T2 + 1) % 2][:, :, :], srcT[:, :, (T2 + 1) * 256:(T2 + 2) * 256], 'ld%d' % ((T2 + 1) % 2),
                      out_b=hnTs[(T2 + 1) % 2])
            vt = Vtm[t % 2]
            qs, kvs, dqs, dks, krr = qs_l[t % 2], kvs_l[t % 2], dqs_l[t % 2], dks_l[t % 2], krr_l[t % 2]
            ra, rb = rka, rkb
            for (bk, c0, n) in ((0, 0, 384), (1, 384, 288), (4, 672, 512), (5, 1184, 512), (6, 1696, 512)):
                for kc in range(8):
                    k.op('pe', lambda e, bk=bk, c0=c0, n=n, kc=kc: e.matmul(
                        bank[bk][:, 0:n], lhsT=hs[:, kc, s * 128:(s + 1) * 128], rhs=Win[:, kc, c0:c0 + n],
                        start=(kc == 0), stop=(kc == 7)), reads=[hs, Win], writes=[bank[bk]])
            k.op('act', lambda e: e.activation(out=junk[:, 0:384], in_=bank[0][:, 0:384], func=AF.Square, accum_out=ssA[:, 0:1]),
                 reads=[bank[0]], writes=[junk, ssA])
            k.op('act', lambda e: e.activation(out=junk[:, 0:256], in_=bank[1][:, 0:256], func=AF.Square, accum_out=ssA[:, 1:2]),
                 reads=[bank[1]], writes=[junk, ssA])
            k.op('act', lambda e: e.copy(out=krs[:, :], in_=bank[1][:, 256:288]), reads=[bank[1]], writes=[krs])
            k.op('act', lambda e: e.activation(out=junk[:, 0:32], in_=krs[:, :], func=AF.Square, accum_out=ssA[:, 2:3]),
                 reads=[krs], writes=[junk, ssA])
            self.rstd(k, ssA, ssA[:, 0:1], ssA, ssA[:, 4:5], 384)
            self.rstd(k, ssA, ssA[:, 1:2], ssA, ssA[:, 5:6], 256)
            self.rstd(k, ssA, ssA[:, 2:3], ssA, ssA[:, 6:7], 32)
            k.op('dve', lambda e: e.scalar_tensor_tensor(out=cq_bf[:, :], in0=bank[0][:, 0:384], scalar=ssA[:, 4:5], in1=g_cq[:, :],
                                                         op0=ALU.mult, op1=ALU.mult), reads=[bank[0], ssA, g_cq], writes=[cq_bf])
            k.op('dve', lambda e: e.scalar_tensor_tensor(out=ckv_bf[:, :], in0=bank[1][:, 0:256], scalar=ssA[:, 5:6], in1=g_ckv[:, :],
                                                         op0=ALU.mult, op1=ALU.mult), reads=[bank[1], ssA, g_ckv], writes=[ckv_bf])
            k.op('dve', lambda e: e.scalar_tensor_tensor(out=krn[:, :], in0=krs[:, :], scalar=ssA[:, 6:7], in1=g32[:, 1, :],
                                                         op0=ALU.mult, op1=ALU.mult), reads=[krs, ssA, g32], writes=[krn])
            cs = cos_all[:, t, :]
            sn = sin_all[:, t, :]
            k.op('pool', lambda e: e.tensor_tensor(out=ra[:, 0, :], in0=krn[:, 0:16], in1=cs, op=ALU.mult), reads=[krn, cos_all], writes=[ra])
            k.op('pool', lambda e: e.tensor_tensor(out=rb[:, 0, :], in0=krn[:, 16:32], in1=sn, op=ALU.mult), reads=[krn, sin_all], writes=[rb])
            k.op('pool', lambda e: e.tensor_tensor(out=krr[:, 0:16], in0=ra[:, 0, :], in1=rb[:, 0, :], op=ALU.subtract), reads=[ra, rb], writes=[krr])
            k.op('pool', lambda e: e.tensor_tensor(out=ra[:, 0, :], in0=krn[:, 0:16], in1=sn, op=ALU.mult), reads=[krn, sin_all], writes=[ra])
            k.op('pool', lambda e: e.tensor_tensor(out=rb[:, 0, :], in0=krn[:, 16:32], in1=cs, op=ALU.mult), reads=[krn, cos_all], writes=[rb])
            k.op('pool', lambda e: e.tensor_tensor(out=krr[:, 16:32], in0=ra[:, 0, :], in1=rb[:, 0, :], op=ALU.add), reads=[ra, rb], writes=[krr])
            for c in range(3):
                k.op('pe', lambda e, c=c: e.transpose(out=psT[:, c * 128:(c + 1) * 128], in_=cq_bf[:, c * 128:(c + 1) * 128],
                                                      identity=self.ident[:, :]), reads=[cq_bf, self.ident], writes=[psT])
            for c in range(2):
                k.op('pe', lambda e, c=c: e.transpose(out=psT[:, (3 + c) * 128:(4 + c) * 128], in_=ckv_bf[:, c * 128:(c + 1) * 128],
                                                      identity=self.ident[:, :]), reads=[ckv_bf, self.ident], writes=[psT])
            k.op('act', lambda e: e.copy(out=cT[:, :, :], in_=psT[:, 0:640].rearrange("p (c t) -> p c t", c=5)), reads=[psT], writes=[cT])
            for (bk, c0, n) in ((0, 0, 512), (1, 512, 256)):
                for kc in range(3):
                    k.op('pe', lambda e, bk=bk, c0=c0, n=n, kc=kc: e.matmul(
                        bank[bk][:, 0:n], lhsT=cT[:, kc, :], rhs=Wuq[:, kc, c0:c0 + n], start=(kc == 0), stop=(kc == 2)),
                        reads=[cT, Wuq], writes=[bank[bk]])
            for (bk, c0, n) in ((2, 0, 512), (3, 512, 512)):
                for kc in range(2):
                    k.op('pe', lambda e, bk=bk, c0=c0, n=n, kc=kc: e.matmul(
                        bank[bk][:, 0:n], lhsT=cT[:, 3 + kc, :], rhs=Wukv[:, kc, c0:c0 + n], start=(kc == 0), stop=(kc == 1)),
                        reads=[cT, Wukv], writes=[bank[bk]])
            k.op('act', lambda e: e.copy(out=qs[:, 0:512], in_=bank[0][:, 0:512]), reads=[bank[0]], writes=[qs])
            k.op('act', lambda e: e.copy(out=qs[:, 512:768], in_=bank[1][:, 0:256]), reads=[bank[1]], writes=[qs])
            k.op('act', lambda e: e.copy(out=kvs[:, 0:512], in_=bank[2][:, 0:512]), reads=[bank[2]], writes=[kvs])
            k.op('act', lambda e: e.copy(out=kvs[:, 512:1024], in_=bank[3][:, 0:512]), reads=[bank[3]], writes=[kvs])
            k.op('act', lambda e: e.copy(out=dqs[:, :], in_=bank[4][:, 0:512]), reads=[bank[4]], writes=[dqs])
            k.op('act', lambda e: e.copy(out=dks[:, :], in_=bank[5][:, 0:512]), reads=[bank[5]], writes=[dks])
            k.op('act', lambda e: e.copy(out=vt[:, 8:16, 0:64], in_=bank[6][:, 0:512].rearrange("p (h d) -> p h d", h=8)),
                 reads=[bank[6]], writes=[vt])
            q3 = qs[:, :].rearrange("p (h d) -> p h d", h=8)
            kv3 = kvs[:, :].rearrange("p (h d) -> p h d", h=8)
            dq3 = dqs[:, :].rearrange("p (h d) -> p h d", h=8)
            dk3 = dks[:, :].rearrange("p (h d) -> p h d", h=8)
            k.op('pool', lambda e: e.tensor_copy(out=vt[:, 0:8, 0:64], in_=kv3[:, :, 64:128]), reads=[kvs], writes=[vt])

        def stageB(t):
            T2, s = divmod(t, 2)
            qts, kts = QTs[T2 % 2], KTs[T2 % 2]
            vt, qt, kt = Vtm[t % 2], Qtm[t % 2], Ktm[t % 2]
            qs, kvs, dqs, dks, krr = qs_l[t % 2], kvs_l[t % 2], dqs_l[t % 2], dks_l[t % 2], krr_l[t % 2]
            cs = cos_all[:, t, :]
            sn = sin_all[:, t, :]
            q3 = qs[:, :].rearrange("p (h d) -> p h d", h=8)
            kv3 = kvs[:, :].rearrange("p (h d) -> p h d", h=8)
            dq3 = dqs[:, :].rearrange("p (h d) -> p h d", h=8)
            dk3 = dks[:, :].rearrange("p (h d) -> p h d", h=8)
            sumsq((qs, q3[:, :, 0:64]), 8, 64, ssq[:, 0:8], tmpA)
            sumsq((kvs, kv3[:, :, 0:64]), 8, 64, ssq[:, 8:16], tmpA)
            sumsq((dqs, dq3), 8, 64, ssq[:, 16:24], tmpA)
            sumsq((dks, dk3), 8, 64, ssq[:, 24:32], tmpA)
            sumsq((qs, q3[:, :, 64:96]), 8, 32, ssq[:, 32:40], tmpA)
            self.rstd(k, ssq, ssq[:, 0:32], rsq, rsq[:, 0:32], 64)
            self.rstd(k, ssq, ssq[:, 32:40], rsq, rsq[:, 32:40], 32)
            def norm1(eng, srcb, src3, rs_ap, dst, dst_ap):
                k.op(eng, lambda e: e.tensor_tensor(out=dst_ap, in0=src3, in1=rs_ap.unsqueeze(2).to_broadcast([128, 8, 64]), op=ALU.mult),
                     reads=[srcb, rsq], writes=[dst])
            norm1('dve', qs, q3[:, :, 0:64], rsq[:, 0:8], qt, qt[:, 0:8, 0:64])
            norm1('pool', kvs, kv3[:, :, 0:64], rsq[:, 8:16], kt, kt[:, 0:8, 0:64])
            norm1('dve', dqs, dq3, rsq[:, 16:24], qt, qt[:, 8:16, 0:64])
            norm1('pool', dks, dk3, rsq[:, 24:32], kt, kt[:, 8:16, 0:64])
            headnorm((qs, q3[:, :, 64:96]), 8, 32, rsq[:, 32:40], g32[:, 0, :], qrn, qrn[:, :, :], tmpA)
            csb = cs.unsqueeze(1).to_broadcast([128, 8, 16])
            snb = sn.unsqueeze(1).to_broadcast([128, 8, 16])
            k.op('dve', lambda e: e.tensor_tensor(out=ra[:, :, :], in0=qrn[:, :, 0:16], in1=csb, op=ALU.mult), reads=[qrn, cos_all], writes=[ra])
            k.op('dve', lambda e: e.tensor_tensor(out=rb[:, :, :], in0=qrn[:, :, 16:32], in1=snb, op=ALU.mult), reads=[qrn, sin_all], writes=[rb])
            k.op('pool', lambda e: e.tensor_tensor(out=rc[:, :, :], in0=qrn[:, :, 0:16], in1=snb, op=ALU.mult), reads=[qrn, sin_all], writes=[rc])
            k.op('pool', lambda e: e.tensor_tensor(out=rd[:, :, :], in0=qrn[:, :, 16:32], in1=csb, op=ALU.mult), reads=[qrn, cos_all], writes=[rd])
            k.op('dve', lambda e: e.tensor_tensor(out=qt[:, 0:8, 64:80], in0=ra[:, :, :], in1=rb[:, :, :], op=ALU.subtract), reads=[ra, rb], writes=[qt])
            k.op('pool', lambda e: e.tensor_tensor(out=qt[:, 0:8, 80:96], in0=rc[:, :, :], in1=rd[:, :, :], op=ALU.add), reads=[rc, rd], writes=[qt])
            k.op('pool', lambda e: e.tensor_copy(out=kt[:, 0:8, 64:96], in_=krr[:, :].unsqueeze(1).to_broadcast([128, 8, 32])),
                 reads=[krr], writes=[kt])
            k.op('pool', lambda e: e.tensor_copy(out=qt[:, 8:16, 68:72], in_=tokq[:, t, :].unsqueeze(1).to_broadcast([128, 8, 4])),
                 reads=[tokq], writes=[qt])
            k.op('pool', lambda e: e.tensor_copy(out=kt[:, 8:16, 64:68], in_=tokk[:, t, :].unsqueeze(1).to_broadcast([128, 8, 4])),
                 reads=[tokk], writes=[kt])
            for (src, dst, h0, R, gc) in ((qt, qts, 0, 96, 0), (qt, qts, 8, 72, 2), (kt, kts, 0, 96, 1), (kt, kts, 8, 72, 3)):
                for hh in range(8):
                    k.op('pe', lambda e, src=src, h0=h0, hh=hh, R=R: e.transpose(
                        out=psT[0:R, hh * 128:(hh + 1) * 128], in_=src[:, h0 + hh, 0:R], identity=self.ident[:, :]),
                        reads=[src, self.ident], writes=[psT])
                k.op('act', lambda e, dst=dst, h0=h0, R=R, gc=gc: e.mul(
                    out=dst[0:R, h0:h0 + 8, s * 128:(s + 1) * 128], in_=psT[0:R, :].rearrange("p (h t) -> p h t", h=8),
                    mul=gcol[0:R, gc:gc + 1]), reads=[psT, gcol], writes=[dst])
            k.dma('sp', Vd[t * 128:(t + 1) * 128, :, :], vt[:, :, :], 'stv%d' % (t % 2), in_b=vt)
            if s == 1:
                c0 = T2 * 256
                k.dma('sp', QTd[0:96, 0:8, c0:c0 + 256], qts[0:96, 0:8, :], 'stq%d' % (T2 % 2), in_b=qts)
                k.dma('sp', QTd[0:72, 8:16, c0:c0 + 256], qts[0:72, 8:16, :], 'stq%d' % (T2 % 2), in_b=qts)
                k.dma('sp', KTd[0:96, 0:8, c0:c0 + 256], kts[0:96, 0:8, :], 'stk%d' % (T2 % 2), in_b=kts)
                k.dma('sp', KTd[0:72, 8:16, c0:c0 + 256], kts[0:72, 8:16, :], 'stk%d' % (T2 % 2), in_b=kts)

        for i in range(33):
            if i < 32:
                stageA(i)
            if i >= 1:
                stageB(i - 1)
        k.end_phase()

    def phase_attn(self, k, heads):
        I = self.I
        k.begin_phase()
        maskc = k.sbuf([128, 7 * 128], BF16, 'maskc')
        k.dma('pool', maskc[:, :], I['c_maskc'][:, :], 'cst', out_b=maskc)
        use_d = any(h['mask'] == 'd' for h in heads)
        if use_d:
            maskd = k.sbuf([128, 23 * 128], BF16, 'maskd')
            for c0 in range(0, 23 * 128, 512):
                n = min(512, 23 * 128 - c0)
                k.dma('pool', maskd[:, c0:c0 + n], I['c_maskd'][:, c0:c0 + n], 'cst', out_b=maskd)
        psF = k.psum([128, 5 * 512], F32)
        Sb = [Buf(psF[:, i * 512:(i + 1) * 512], 'S%d' % i) for i in range(3)]
        Ob = [Buf(psF[:, (3 + i) * 512:(4 + i) * 512], 'O%d' % i) for i in range(2)]
        Pb = [k.sbuf([128, 512], BF16, 'P') for _ in range(3)]
        QTh = [k.sbuf([128, S], BF16, 'QTh') for _ in range(2)]
        KTh = [k.sbuf([128, S], BF16, 'KTh') for _ in range(2)]
        Vh = [k.sbuf([128, 32, 128], BF16, 'Vh') for _ in range(2)]
        rden = [k.sbuf([128, 512], F32, 'rden') for _ in range(2)]
        ost = [k.sbuf([64, S], BF16, 'ost') for _ in range(2)]

        def load_head(h):
            R = heads[h]['R']
            i = h % 2
            k.dma('sp', QTh[i][0:R, :], self.QT[h, 0:R, :], 'ldq%d' % i, out_b=QTh[i])
            k.dma('sp', KTh[i][0:R, :], self.KT[h, 0:R, :], 'ldk%d' % i, out_b=KTh[i])
            k.dma('sp', Vh[i][:, :, :], self.V[h, :, :].rearrange("(b p) c -> p b c", p=128), 'ldv%d' % i, out_b=Vh[i])

        load_head(0)
        gi = 0
        for h, hd in enumerate(heads):
            if h + 1 < len(heads):
                load_head(h + 1)
            R, scale, mk = hd['R'], hd['scale'], hd['mask']
            qT, kT, vv = QTh[h % 2], KTh[h % 2], Vh[h % 2]
            os_ = ost[h % 2]
            pairs = []
            for g in range(8):
                jlo = max(0, 4 * g - 16) if mk == 'd' else 0
                js = list(range(jlo, 4 * g + 4))
                for j in js:
                    pairs.append((g, j, j == js[0], j == js[-1]))

            def cols(g, j):
                d0 = 4 * g - j
                b0 = max(0, -d0)
                b1 = 4 if mk != 'd' else min(4, 17 - d0)
                return b0 * 128, b1 * 128

            def emit_qk(i):
                g, j, first, last = pairs[i]
                Sx = Sb[i % 3]
                d0 = 4 * g - j
                c0, c1 = cols(g, j)
                if mk == 'd':
                    need_mask, tab = True, maskd
                else:
                    need_mask, tab = (d0 <= 0), maskc
                off = (d0 + 3) * 128
                k.op('pe', lambda e: e.matmul(Sx[:, c0:c1], lhsT=kT[0:R, j * 128:(j + 1) * 128], rhs=qT[0:R, g * 512 + c0:g * 512 + c1],
                                              start=True, stop=not need_mask), reads=[kT, qT], writes=[Sx])
                if need_mask:
                    k.op('pe', lambda e: e.matmul(Sx[:, c0:c1], lhsT=self.ident[:, :], rhs=tab[:, off + c0:off + c1], start=False, stop=True),
                         reads=[self.ident, tab], writes=[Sx])
                Px = Pb[i % 3]
                k.op('act', lambda e: e.activation(out=Px[:, c0:c1], in_=Sx[:, c0:c1], func=AF.Exp, scale=float(scale)), reads=[Sx], writes=[Px])

            def emit_pv(i):
                nonlocal gi
                g, j, first, last = pairs[i]
                c0, c1 = cols(g, j)
                Px = Pb[i % 3]
                Ox = Ob[(gi) % 2]
                k.op('pe', lambda e: e.matmul(Ox[:, c0:c1], lhsT=vv[:, j, :], rhs=Px[:, c0:c1], start=first, stop=last), reads=[vv, Px], writes=[Ox])
                if last:
                    rd = rden[gi % 2]
                    k.op('dve', lambda e: e.reciprocal(out=rd[64:128, :], in_=Ox[64:128, :]), reads=[Ox], writes=[rd])
                    k.op('dve', lambda e: e.tensor_tensor(out=os_[0:64, g * 512:(g + 1) * 512], in0=Ox[0:64, :], in1=rd[64:128, :], op=ALU.mult),
                         reads=[Ox, rd], writes=[os_])
                    gi += 1

            LA = 2
            n = len(pairs)
            for i in range(n + LA):
                if i < n:
                    emit_qk(i)
                if i - LA >= 0:
                    emit_pv(i - LA)
            k.dma('sp', self.mixT[h // 2, (h % 2) * 64:(h % 2) * 64 + 64, :], os_[0:64, :], 'sto%d' % (h % 2), in_b=os_)
        k.end_phase()

    def phase_o1(self, k, w_out, h_in, h_out, gain_row):
        k.begin_phase()
        Wo = k.sbuf([128, 8, 1024], BF16, 'Wo')
        self.load_w(k, Wo, w_out, 8, 1024)
        gain = k.sbuf([128, 1024], F32, 'gain')
        self.load_bc(k, gain, gain[:, :], gain_row, 1024)
        psF = k.psum([128, 2048], F32)
        psB = k.psum([128, 2048], BF16)
        mixb = [Buf(psF[:, i * 1024:(i + 1) * 1024], 'mix%d' % i) for i in range(2)]
        nt = self.NormT(self, k, psB)
        mTs = [k.sbuf([128, 8, 512], BF16, 'mTs') for _ in range(2)]
        hts = [k.sbuf([128, 1024], F32, 'ht') for _ in range(3)]
        h1s = [k.sbuf([128, 1024], F32, 'h1') for _ in range(3)]
        stages = [k.sbuf([128, 8, 512], BF16, 'stg') for _ in range(2)]
        srcT = self.mixT[:, :, :].rearrange("c p t -> p c t")
        dstT = self.hnT[:, :, :].rearrange("c p t -> p c t")
        k.dma('sp', mTs[0][:, :, :], srcT[:, :, 0:512], 'ldm0', out_b=mTs[0])
        deferred = []
        for T in range(8):
            if T + 1 < 8:
                k.dma('sp', mTs[(T + 1) % 2][:, :, :], srcT[:, :, (T + 1) * 512:(T + 2) * 512], 'ldm%d' % ((T + 1) % 2), out_b=mTs[(T + 1) % 2])
            ms = mTs[T % 2]
            stage = stages[T % 2]
            for s in range(4):
                t = 4 * T + s
                ht, h1 = hts[t % 3], h1s[t % 3]
                mb = mixb[t % 2]
                k.dma('sp', ht[:, :], h_in[t * 128:(t + 1) * 128, :], 'ldh%d' % (t % 3), out_b=ht)
                for half in range(2):
                    for c in range(8):
                        k.op('pe', lambda e, half=half, c=c: e.matmul(
                            mb[:, half * 512:(half + 1) * 512], lhsT=ms[:, c, s * 128:(s + 1) * 128],
                            rhs=Wo[:, c, half * 512:(half + 1) * 512], start=(c == 0), stop=(c == 7)), reads=[ms, Wo], writes=[mb])
                for f in deferred:
                    f()
                deferred = []
                for half in range(2):
                    hsl = slice(half * 512, (half + 1) * 512)
                    k.op('dve', lambda e, hsl=hsl: e.tensor_tensor(out=h1[:, hsl], in0=mb[:, hsl], in1=ht[:, hsl], op=ALU.add), reads=[mb, ht], writes=[h1])
                k.dma('sp', h_out[t * 128:(t + 1) * 128, :], h1[:, :], 'sth%d' % (t % 3), in_b=h1)
                i = nt.run_a(h1, h1[:, :], gain)
                deferred.append(lambda i=i, stage=stage, s=s: nt.run_b(i, stage, s * 128))
            deferred.append(lambda T=T, stage=stage: k.dma('sp', dstT[:, :, T * 512:(T + 1) * 512], stage[:, :, :], 'st%d' % (T % 2), in_b=stage))
        for f in deferred:
            f()
        k.end_phase()

    def phase_o2(self, k, w1, w2, h_in, h_out):
        k.begin_phase()
        W1 = k.sbuf([128, 8, 4096], BF16, 'W1')
        W2 = k.sbuf([128, 32, 1024], BF16, 'W2')
        self.load_w(k, W1, w1, 8, 4096)
        self.load_w(k, W2, w2, 32, 1024)
        psF = k.psum([128, 4096], F32)
        f1 = [Buf(psF[:, i * 512:i * 512 + 256], 'f1_%d' % i) for i in range(4)]
        f2 = [Buf(psF[:, 2048 + i * 1024:3072 + i * 1024], 'f2_%d' % i) for i in range(2)]
        xTs = [k.sbuf([128, 8, 256], BF16, 'xTs') for _ in range(2)]
        uT = k.sbuf([128, 32, 256], BF16, 'uT')
        rl = [k.sbuf([128, 256], F32, 'rl') for _ in range(2)]
        hts = [k.sbuf([128, 1024], F32, 'ht') for _ in range(2)]
        h2s = [k.sbuf([128, 1024], F32, 'h2') for _ in range(2)]
        srcT = self.hnT[:, :, :].rearrange("c p t -> p c t")
        dbg = self.cfg.get('o2_dbg', 3)
        k.dma('sp', xTs[0][:, :, :], srcT[:, :, 0:256], 'ldx0', out_b=xTs[0])
        for T2 in range(16 if dbg > 1 else 0):
            if T2 + 1 < 16:
                k.dma('sp', xTs[(T2 + 1) % 2][:, :, :], srcT[:, :, (T2 + 1) * 256:(T2 + 2) * 256], 'ldx%d' % ((T2 + 1) % 2),
                      out_b=xTs[(T2 + 1) % 2])
            xs = xTs[T2 % 2]
            for fc in range(32):
                fb = f1[fc % 4]
                r = rl[fc % 2]
                for kc in range(8):
                    k.op('pe', lambda e, fc=fc, kc=kc, fb=fb: e.matmul(fb[:, :], lhsT=W1[:, kc, fc * 128:(fc + 1) * 128], rhs=xs[:, kc, :],
                                                                       start=(kc == 0), stop=(kc == 7)), reads=[W1, xs], writes=[fb])
                k.op('act', lambda e, fb=fb, r=r: e.activation(out=r[:, :], in_=fb[:, :], func=AF.Relu), reads=[fb], writes=[r])
                k.op('dve', lambda e, fb=fb, r=r, fc=fc: e.tensor_tensor(out=uT[:, fc, :], in0=fb[:, :], in1=r[:, :], op=ALU.mult),
                     reads=[fb, r], writes=[uT])
            for s in range(2 if dbg > 2 else 0):
                t = 2 * T2 + s
                ht, h2 = hts[t % 2], h2s[t % 2]
                ob = f2[t % 2]
                k.dma('sp', ht[:, :], h_in[t * 128:(t + 1) * 128, :], 'ldh%d' % (t % 2), out_b=ht)
                for half in range(2):
                    for fc in range(32):
                        k.op('pe', lambda e, half=half, fc=fc: e.matmul(
                            ob[:, half * 512:(half + 1) * 512], lhsT=uT[:, fc, s * 128:(s + 1) * 128],
                            rhs=W2[:, fc, half * 512:(half + 1) * 512], start=(fc == 0), stop=(fc == 31)), reads=[uT, W2], writes=[ob])
                for half in range(2):
                    hsl = slice(half * 512, (half + 1) * 512)
                    k.op('dve', lambda e, hsl=hsl: e.tensor_tensor(out=h2[:, hsl], in0=ob[:, hsl], in1=ht[:, hsl], op=ALU.add), reads=[ob, ht], writes=[h2])
                k.dma('sp', h_out[t * 128:(t + 1) * 128, :], h2[:, :], 'sth%d' % (t % 2), in_b=h2)
        k.end_phase()

    def phase_o3(self, k, wg, wp, p_in, h_in, h_out, ple_gain_row, next_gain_row):
        k.begin_phase()
        Wg = k.sbuf([128, 8, 1024], BF16, 'Wg')
        Wp = k.sbuf([128, 2, 1024], BF16, 'Wp')
        self.load_w(k, Wg, wg, 8, 1024)
        self.load_w(k, Wp, wp, 2, 1024)
        gple = k.sbuf([128, 1024], F32, 'gple')
        self.load_bc(k, gple, gple[:, :], ple_gain_row, 1024)
        gain = None
        if next_gain_row is not None:
            gain = k.sbuf([128, 1024], F32, 'gain')
            self.load_bc(k, gain, gain[:, :], next_gain_row, 1024)
        psF = k.psum([128, 2048], F32)
        psB = k.psum([128, 3072], BF16)
        gb = Buf(psF[:, 0:1024], 'gb')
        pb = Buf(psF[:, 1024:2048], 'pb')
        psP = Buf(psB[:, 2048:2304], 'psP')
        nt = self.NormT(self, k, psB[:, 0:2048])
        xst = [k.sbuf([128, 8, 128], BF16, 'xst') for _ in range(2)]
        pts = [k.sbuf([128, 256], F32, 'pt') for _ in range(3)]
        pbf = [k.sbuf([128, 256], BF16, 'pbf') for _ in range(2)]
        pT = [k.sbuf([128, 2, 128], BF16, 'pT') for _ in range(2)]
        gs = [k.sbuf([128, 1024], F32, 'gs') for _ in range(2)]
        hts = [k.sbuf([128, 1024], F32, 'ht') for _ in range(3)]
        h3s = [k.sbuf([128, 1024], F32, 'h3') for _ in range(2)]
        stages = [k.sbuf([128, 8, 512], BF16, 'stg') for _ in range(2)] if gain is not None else None
        dstT = self.hnT[:, :, :].rearrange("c p t -> p c t")
        deferred = []

        def stageA(t):
            i2, i3 = t % 2, t % 3
            xs = xst[i2]
            k.dma('sp', pts[i3][:, :], p_in[t * 128:(t + 1) * 128, :], 'ldp%d' % i3, out_b=pts[i3])
            k.dma('sp', hts[i3][:, :], h_in[t * 128:(t + 1) * 128, :], 'ldh%d' % i3, out_b=hts[i3])
            nt.run(hts[i3], hts[i3][:, :], gple, xs, 0)
            k.op('pool', lambda e: e.tensor_copy(out=pbf[i2][:, :], in_=pts[i3][:, :]), reads=[pts[i3]], writes=[pbf[i2]])
            for c in range(2):
                k.op('pe', lambda e, c=c: e.transpose(out=psP[:, c * 128:(c + 1) * 128], in_=pbf[i2][:, c * 128:(c + 1) * 128],
                                                      identity=self.ident[:, :]), reads=[pbf[i2], self.ident], writes=[psP])
            k.op('act', lambda e: e.copy(out=pT[i2][:, :, :], in_=psP[:, :].rearrange("p (c t) -> p c t", c=2)), reads=[psP], writes=[pT[i2]])

        def stageM(t):
            i2 = t % 2
            xs = xst[i2]
            for half in range(2):
                for c in range(8):
                    k.op('pe', lambda e, half=half, c=c: e.matmul(
                        gb[:, half * 512:(half + 1) * 512], lhsT=xs[:, c, :],
                        rhs=Wg[:, c, half * 512:(half + 1) * 512], start=(c == 0), stop=(c == 7)), reads=[xs, Wg], writes=[gb])
            for half in range(2):
                for c in range(2):
                    k.op('pe', lambda e, half=half, c=c: e.matmul(
                        pb[:, half * 512:(half + 1) * 512], lhsT=pT[i2][:, c, :],
                        rhs=Wp[:, c, half * 512:(half + 1) * 512], start=(c == 0), stop=(c == 1)), reads=[pT[i2], Wp], writes=[pb])

        def stageB(t):
            T, s = divmod(t, 4)
            i2, i3 = t % 2, t % 3
            for half in range(2):
                hsl = slice(half * 512, (half + 1) * 512)
                k.op('act', lambda e, hsl=hsl: e.activation(out=gs[i2][:, hsl], in_=gb[:, hsl], func=AF.Sigmoid), reads=[gb], writes=[gs[i2]])
            for half in range(2):
                hsl = slice(half * 512, (half + 1) * 512)
                k.op('dve', lambda e, hsl=hsl: e.tensor_tensor(out=gs[i2][:, hsl], in0=pb[:, hsl], in1=gs[i2][:, hsl], op=ALU.mult), reads=[pb, gs[i2]], writes=[gs[i2]])
            k.op('pool', lambda e: e.tensor_tensor(out=h3s[i2][:, :], in0=gs[i2][:, :], in1=hts[i3][:, :], op=ALU.add),
                 reads=[gs[i2], hts[i3]], writes=[h3s[i2]])
            k.dma('pool', h_out[t * 128:(t + 1) * 128, :], h3s[i2][:, :], 'sth%d' % i2, in_b=h3s[i2])
            if gain is not None:
                i = nt.run_a(h3s[i2], h3s[i2][:, :], gain)
                deferred.append(lambda i=i, T=T, s=s: nt.run_b(i, stages[T % 2], s * 128))
                if s == 3:
                    deferred.append(lambda T=T: k.dma('sp', dstT[:, :, T * 512:(T + 1) * 512], stages[T % 2][:, :, :], 'st%d' % (T % 2), in_b=stages[T % 2]))

        for i in range(33):
            if i >= 1:
                stageM(i - 1)
                for f in deferred:
                    f()
                del deferred[:]
            if i < 32:
                stageA(i)
            if i >= 1:
                stageB(i - 1)
        for f in deferred:
            f()
        k.end_phase()

    def phase_p1(self, k):
        I = self.I
        k.begin_phase()
        Win = k.sbuf([128, 8, 3072], BF16, 'Win')
        self.load_w(k, Win, I['o_w_in'][0], 8, 3072)
        g64 = k.sbuf([128, 2, 64], F32)
        for i, nm in enumerate(['o_qn', 'o_kn']):
            self.load_bc(k, g64, g64[:, i, :], I[nm][0, :], 64)
        gcol = k.sbuf([128, 2], F32, 'gcol')
        k.op('pool', lambda e: e.memset(gcol[:, :], 1.0), writes=[gcol])
        for i, nm in enumerate(['o_qn', 'o_kn']):
            k.dma('sp', gcol[0:64, i:i + 1], I[nm][0, :].rearrange("(p o) -> p o", o=1), 'cst', out_b=gcol)
        tokq = k.sbuf([128, 32, 4], F32); k.dma('sp', tokq[:, :, :], I['c_tokq'][:, :, :], 'cst', out_b=tokq)
        tokk = k.sbuf([128, 32, 4], F32); k.dma('sp', tokk[:, :, :], I['c_tokk'][:, :, :], 'cst', out_b=tokk)
        hq = k.sbuf([128, 16, 4], F32); self.load_bc(k, hq, hq[:, :, :].rearrange("p h c -> p (h c)"), I['c_hq1'][0, :], 64)
        hk = k.sbuf([128, 16, 4], F32); self.load_bc(k, hk, hk[:, :, :].rearrange("p h c -> p (h c)"), I['c_hk1'][0, :], 64)
        psF = k.psum([128, 3584], F32)
        psB = k.psum([128, 1024], BF16)
        bank = [Buf(psF[:, i * 512:(i + 1) * 512], 'bank%d' % i) for i in range(7)]
        psT = Buf(psB[:, :], 'psT')
        psKM = bank[6]
        psG = bank[6]
        R = 88
        hnTs = [k.sbuf([128, 8, 256], BF16, 'hnTs') for _ in range(2)]
        QTs = [k.sbuf([128, 16, 256], BF16, 'QTs') for _ in range(2)]
        KTs = [k.sbuf([128, 16, 256], BF16, 'KTs') for _ in range(2)]
        Vtm = [k.sbuf([128, 16, 128], BF16, 'Vtm') for _ in range(2)]
        Qtm = [k.sbuf([128, 16, 96], BF16, 'Qtm') for _ in range(2)]
        Ktm = [k.sbuf([128, 16, 96], BF16, 'Ktm') for _ in range(2)]
        for i in range(2):
            k.op('pool', lambda e, i=i: e.memset(Vtm[i][:, :, 64:128], 1.0), writes=[Vtm[i]])
            k.op('pool', lambda e, i=i: e.memset(Qtm[i][:, :, :], 0.0), writes=[Qtm[i]])
            k.op('pool', lambda e, i=i: e.memset(Ktm[i][:, :, :], 0.0), writes=[Ktm[i]])
            k.op('dve', lambda e, i=i: e.tensor_copy(out=Qtm[i][:, :, 80:84], in_=hq[:, :, :]), reads=[hq], writes=[Qtm[i]])
            k.op('dve', lambda e, i=i: e.tensor_copy(out=Ktm[i][:, :, 84:88], in_=hk[:, :, :]), reads=[hk], writes=[Ktm[i]])
        qs_l = [k.sbuf([128, 1024], F32, 'qs') for _ in range(2)]
        ks_l = [k.sbuf([128, 1024], F32, 'ks') for _ in range(2)]
        Kf = k.sbuf([128, 1024], F32, 'Kf')
        tmpA = k.sbuf([128, 1024], F32, 'tmpA')
        tmpB = k.sbuf([128, 1024], F32, 'tmpB')
        ssq = k.sbuf([128, 32], F32, 'ssq')
        rsq = k.sbuf([128, 32], F32, 'rsq')
        QgT = k.sbuf([64, 16, 128], BF16, 'QgT')
        kmA = k.sbuf([64, 16], F32, 'kmA')
        kmS = k.sbuf([64, 16], F32, 'kmS')
        kmH = k.sbuf([64, 16], F32, 'kmH')
        KmHi = k.sbuf([64, 16, 16], BF16, 'KmHi')
        KmLo = k.sbuf([64, 16, 16], BF16, 'KmLo')
        gate = k.sbuf([128, 16, 16], F32, 'gate')
        mx = k.sbuf([128, 16, 8], F32, 'mx')
        sel = k.sbuf([128, 16, 16], F32, 'sel')
        k.op('pool', lambda e: e.memset(gate[:, :, :], NEG), writes=[gate])
        k.op('pool', lambda e: e.memset(KmHi[:, :, :], 0.0), writes=[KmHi])
        k.op('pool', lambda e: e.memset(KmLo[:, :, :], 0.0), writes=[KmLo])

        srcT = self.hnT[:, :, :].rearrange("c p t -> p c t")
        QTd = self.QT[:, :, :].rearrange("h r t -> r h t")
        KTd = self.KT[:, :, :].rearrange("h r t -> r h t")
        Vd = self.V[:, :, :].rearrange("h t c -> t h c")

        k.dma('sp', hnTs[0][:, :, :], srcT[:, :, 0:256], 'ld0', out_b=hnTs[0])

        def stageA(t):
            T2, s = divmod(t, 2)
            hs = hnTs[T2 % 2]
            if s == 0 and T2 + 1 < 16:
                k.dma('sp', hnTs[(T2 + 1) % 2][:, :, :], srcT[:, :, (T2 + 1) * 256:(T2 + 2) * 256], 'ld%d' % ((T2 + 1) % 2),
                      out_b=hnTs[(T2 + 1) % 2])
            vt = Vtm[t % 2]
            qs, ks = qs_l[t % 2], ks_l[t % 2]
            for bk in range(6):
                for kc in range(8):
                    k.op('pe', lambda e, bk=bk, kc=kc: e.matmul(
                        bank[bk][:, :], lhsT=hs[:, kc, s * 128:(s + 1) * 128], rhs=Win[:, kc, bk * 512:(bk + 1) * 512],
                        start=(kc == 0), stop=(kc == 7)), reads=[hs, Win], writes=[bank[bk]])
            k.op('act', lambda e: e.copy(out=qs[:, 0:512], in_=bank[0][:, :]), reads=[bank[0]], writes=[qs])
            k.op('act', lambda e: e.copy(out=qs[:, 512:1024], in_=bank[1][:, :]), reads=[bank[1]], writes=[qs])
            k.op('act', lambda e: e.copy(out=ks[:, 0:512], in_=bank[2][:, :]), reads=[bank[2]], writes=[ks])
            k.op('act', lambda e: e.copy(out=ks[:, 512:1024], in_=bank[3][:, :]), reads=[bank[3]], writes=[ks])
            k.op('act', lambda e: e.copy(out=vt[:, 0:8, 0:64], in_=bank[4][:, :].rearrange("p (h d) -> p h d", h=8)), reads=[bank[4]], writes=[vt])
            k.op('act', lambda e: e.copy(out=vt[:, 8:16, 0:64], in_=bank[5][:, :].rearrange("p (h d) -> p h d", h=8)), reads=[bank[5]], writes=[vt])

        def stageB(t):
            T2, s = divmod(t, 2)
            own = T2
            qts, kts = QTs[T2 % 2], KTs[T2 % 2]
            vt, qt, kt = Vtm[t % 2], Qtm[t % 2], Ktm[t % 2]
            qs, ks = qs_l[t % 2], ks_l[t % 2]
            q3 = qs[:, :].rearrange("p (h d) -> p h d", h=16)
            k3 = ks[:, :].rearrange("p (h d) -> p h d", h=16)
            kf3 = Kf[:, :].rearrange("p (h d) -> p h d", h=16)
            tA3 = tmpA[:, :].rearrange("p (h d) -> p h d", h=16)
            tB3 = tmpB[:, :].rearrange("p (h d) -> p h d", h=16)
            k.op('dve', lambda e: e.tensor_tensor(out=tA3, in0=q3, in1=q3, op=ALU.mult), reads=[qs], writes=[tmpA])
            k.op('dve', lambda e: e.tensor_reduce(out=ssq[:, 0:16], in_=tA3, axis=AX.X, op=ALU.add), reads=[tmpA], writes=[ssq])
            k.op('pool', lambda e: e.tensor_tensor(out=tB3, in0=k3, in1=k3, op=ALU.mult), reads=[ks], writes=[tmpB])
            k.op('dve', lambda e: e.tensor_reduce(out=ssq[:, 16:32], in_=tB3, axis=AX.X, op=ALU.add), reads=[tmpB], writes=[ssq])
            self.rstd(k, ssq, ssq[:, :], rsq, rsq[:, :], 64)
            k.op('dve', lambda e: e.tensor_tensor(out=qt[:, :, 0:64], in0=q3, in1=rsq[:, 0:16].unsqueeze(2).to_broadcast([128, 16, 64]), op=ALU.mult),
                 reads=[qs, rsq], writes=[qt])
            k.op('pool', lambda e: e.tensor_tensor(out=kt[:, :, 0:64], in0=k3, in1=rsq[:, 16:32].unsqueeze(2).to_broadcast([128, 16, 64]), op=ALU.mult),
                 reads=[ks, rsq], writes=[kt])
            k.op('pool', lambda e: e.memset(kt[:, :, 64:80], 0.0), writes=[kt])
            k.op('pool', lambda e: e.memset(kt[:, :, 64 + own:65 + own], 1.0), writes=[kt])
            k.op('pool', lambda e: e.tensor_copy(out=kt[:, :, 80:84], in_=tokk[:, t, :].unsqueeze(1).to_broadcast([128, 16, 4])),
                 reads=[tokk], writes=[kt])
            k.op('pool', lambda e: e.tensor_copy(out=qt[:, :, 84:88], in_=tokq[:, t, :].unsqueeze(1).to_broadcast([128, 16, 4])),
                 reads=[tokq], writes=[qt])
            if own == 0:
                k.op('pool', lambda e: e.memset(qt[:, :, 64:80], 0.0), writes=[qt])
            else:
                for r in range(2):
                    for hh in range(8):
                        k.op('pe', lambda e, r=r, hh=hh: e.transpose(out=psT[0:64, hh * 128:(hh + 1) * 128], in_=qt[:, r * 8 + hh, 0:64],
                                                                     identity=self.ident[:, :]), reads=[qt, self.ident], writes=[psT])
                    k.op('act', lambda e, r=r: e.mul(out=QgT[0:64, r * 8:(r + 1) * 8, :], in_=psT[0:64, :].rearrange("p (h t) -> p h t", h=8),
                                                     mul=gcol[0:64, 0:1]), reads=[psT, gcol], writes=[QgT])
                for hh in range(16):
                    k.op('pe', lambda e, hh=hh: e.matmul(psG[:, 256 + hh * 16:256 + hh * 16 + own], lhsT=QgT[0:64, hh, :], rhs=KmHi[0:64, hh, 0:own],
                                                         start=True, stop=False), reads=[QgT, KmHi], writes=[psG])
                    k.op('pe', lambda e, hh=hh: e.matmul(psG[:, 256 + hh * 16:256 + hh * 16 + own], lhsT=QgT[0:64, hh, :], rhs=KmLo[0:64, hh, 0:own],
                                                         start=False, stop=True), reads=[QgT, KmLo], writes=[psG])
                k.op('dve', lambda e: e.tensor_copy(out=gate[:, :, 0:own], in_=psG[:, 256:512].rearrange("p (h n) -> p h n", h=16)[:, :, 0:own]),
                     reads=[psG], writes=[gate])
                for hh in range(16):
                    k.op('dve', lambda e, hh=hh: e.max(out=mx[:, hh, :], in_=gate[:, hh, :]), reads=[gate], writes=[mx])
                k.op('dve', lambda e: e.tensor_tensor(out=sel[:, :, :], in0=gate[:, :, :], in1=mx[:, :, 2:3].to_broadcast([128, 16, 16]), op=ALU.is_ge),
                     reads=[gate, mx], writes=[sel])
                k.op('dve', lambda e: e.tensor_scalar(out=qt[:, :, 64:80], in0=sel[:, :, :], scalar1=1.0, scalar2=BIG, op0=ALU.subtract, op1=ALU.mult),
                     reads=[sel], writes=[qt])
                k.op('pool', lambda e: e.memset(qt[:, :, 64 + own:65 + own], 0.0), writes=[qt])
            for hh in range(16):
                k.op('pe', lambda e, hh=hh: e.matmul(psKM[0:64, hh:hh + 1], lhsT=kt[:, hh, 0:64], rhs=self.onesb[:, 0:1],
                                                     start=True, stop=True), reads=[kt, self.onesb], writes=[psKM])
            if s == 0:
                k.op('act', lambda e: e.copy(out=kmA[:, :], in_=psKM[0:64, 0:16]), reads=[psKM], writes=[kmA])
            else:
                k.op('dve', lambda e: e.tensor_tensor(out=kmS[:, :], in0=psKM[0:64, 0:16], in1=kmA[:, :], op=ALU.add), reads=[psKM, kmA], writes=[kmS])
                k.op('dve', lambda e: e.tensor_scalar(out=kmS[:, :], in0=kmS[:, :], scalar1=1.0 / 256, scalar2=gcol[0:64, 1:2], op0=ALU.mult, op1=ALU.mult),
                     reads=[kmS, gcol], writes=[kmS])
                k.op('dve', lambda e: e.tensor_copy(out=KmHi[:, :, own], in_=kmS[:, :]), reads=[kmS], writes=[KmHi])
                k.op('dve', lambda e: e.tensor_copy(out=kmH[:, :], in_=KmHi[:, :, own]), reads=[KmHi], writes=[kmH])
                k.op('dve', lambda e: e.tensor_tensor(out=KmLo[:, :, own], in0=kmS[:, :], in1=kmH[:, :], op=ALU.subtract), reads=[kmS, kmH], writes=[KmLo])
            for (src, dst, gc) in ((qt, qts, 0), (kt, kts, 1)):
                for r in range(2):
                    for hh in range(8):
                        k.op('pe', lambda e, src=src, r=r, hh=hh: e.transpose(
                            out=psT[0:R, hh * 128:(hh + 1) * 128], in_=src[:, r * 8 + hh, 0:R], identity=self.ident[:, :]),
                            reads=[src, self.ident], writes=[psT])
                    k.op('act', lambda e, dst=dst, r=r, gc=gc: e.mul(
                        out=dst[0:R, r * 8:(r + 1) * 8, s * 128:(s + 1) * 128], in_=psT[0:R, :].rearrange("p (h t) -> p h t", h=8),
                        mul=gcol[0:R, gc:gc + 1]), reads=[psT, gcol], writes=[dst])
            k.dma('sp', Vd[t * 128:(t + 1) * 128, :, :], vt[:, :, :], 'stv%d' % (t % 2), in_b=vt)
            if s == 1:
                c0 = T2 * 256
                k.dma('sp', QTd[0:R, :, c0:c0 + 256], qts[0:R, :, :], 'stq%d' % (T2 % 2), in_b=qts)
                k.dma('sp', KTd[0:R, :, c0:c0 + 256], kts[0:R, :, :], 'stk%d' % (T2 % 2), in_b=kts)

        for i in range(33):
            if i < 32:
                stageA(i)
            if i >= 1:
                stageB(i - 1)
        k.end_phase()

    def build(self):
        nc, I = self.nc, self.I
        phases = self.cfg.get('phases', None)

        def on(name):
            return phases is None or name in phases

        with ExitStack() as es:
            es.enter_context(nc.Block())
            k = KB(nc, es)
            self.k = k
            self.setup_globals(k)
            heads0 = [dict(R=96, scale=96 ** -0.5, mask='c')] * 8 + [dict(R=72, scale=0.125, mask='d')] * 8
            heads1 = [dict(R=88, scale=0.125, mask='c')] * 16
            if on('n0'):
                self.phase_norm0(k)
            if on('p0'):
                self.phase_p0(k)
            if on('a0'):
                self.phase_attn(k, heads0)
            if on('o1_0'):
                self.phase_o1(k, I['e_w_out'][0], I['x'], self.hA, I['ff_norm'][0, :])
            if on('o2_0'):
                self.phase_o2(k, I['w_ff1'][0], I['w_ff2'][0], self.hA, self.hB)
            if on('o3_0'):
                self.phase_o3(k, I['w_ple_gate'][0], I['w_ple_proj'][0], I['p'][0], self.hB, self.hC, I['ple_norm'][0, :], I['mix_norm'][1, :])
            if on('p1'):
                self.phase_p1(k)
            if on('a1'):
                self.phase_attn(k, heads1)
            if on('o1_1'):
                self.phase_o1(k, I['o_w_out'][0], self.hC, self.hA, I['ff_norm'][1, :])
            if on('o2_1'):
                self.phase_o2(k, I['w_ff1'][1], I['w_ff2'][1], self.hA, self.hB)
            if on('o3_1'):
                self.phase_o3(k, I['w_ple_gate'][1], I['w_ple_proj'][1], I['p'][1], self.hB, self.y, I['ple_norm'][1, :], None)
            k.barrier()
        return nc


def make_in_maps(inputs, consts, n_cores=8, extra=None):
    maps = []
    shared = {}
    for name in IN_SHAPES:
        if name in ('x', 'p', 'positions'):
            continue
        shared[name] = np.ascontiguousarray(np.asarray(inputs[name], dtype=np.float32))
    shared.update(consts)
    for c in range(n_cores):
        m = dict(shared)
        m['x'] = np.ascontiguousarray(np.asarray(inputs['x'][c], dtype=np.float32))
        m['p'] = np.ascontiguousarray(np.asarray(inputs['p'][:, c], dtype=np.float32))
        pos = np.asarray(inputs['positions'][c], dtype=np.int32)
        m['positions'] = np.ascontiguousarray(pos.reshape(32, 128).T)
        if extra:
            m.update(extra[c])
        maps.append(m)
    return maps


def kernel(**inputs):
    prog = Prog()
    nc = prog.build()
    maps = make_in_maps(inputs, host_consts())
    res = run_bass_kernel_spmd(nc, maps, core_ids=list(range(8)))
    out = np.stack([np.asarray(r["y"], dtype=np.float32) for r in res.results], axis=0)
    return out
```

```python
import numpy as np
from contextlib import ExitStack
import concourse.bass as bass
import concourse.mybir as mybir
from concourse.bass_utils import run_bass_kernel_spmd

F32 = mybir.dt.float32
BF16 = mybir.dt.bfloat16
I32 = mybir.dt.int32
ALU = mybir.AluOpType
AF = mybir.ActivationFunctionType
AX = mybir.AxisListType

S = 4096
D = 1024
NT = 32
EPS = 1e-6
BIG = 4096.0
NEG = -1.0e30
MAGIC = 12582912.0
TWO_PI = 2.0 * np.pi


class Buf:
    def __init__(self, t, name=''):
        self.t = t
        self.name = name
        self.w = None
        self.r = {}

    def __getitem__(self, idx):
        return self.t[idx]


class KB:
    ENG = ['pe', 'act', 'dve', 'pool', 'sp']

    def __init__(self, nc, es, n_dsem=20):
        self.nc = nc
        self.es = es
        self.e = {'pe': nc.tensor, 'act': nc.scalar, 'dve': nc.vector, 'pool': nc.gpsimd, 'sp': nc.sync}
        self.sem = {n: es.enter_context(nc.semaphore('s_' + n)) for n in self.ENG}
        self.cnt = {n: 0 for n in self.ENG}
        self.seen = {n: {} for n in self.ENG}
        self.dsems = [[es.enter_context(nc.semaphore('d_%d' % i)), 0] for i in range(n_dsem)]
        self.dnames = {}
        self.nbuf = 0
        self.pes = None
        self.ninst = 0

    def begin_phase(self):
        self.pes = ExitStack()
        self.dnames = {}

    def end_phase(self):
        self.barrier()
        self.pes.close()
        self.pes = None

    def sbuf(self, shape, dt, name=None, glob=False):
        self.nbuf += 1
        name = (name or 'b') + '_%d' % self.nbuf
        st = self.es if glob else self.pes
        t = st.enter_context(self.nc.sbuf_tensor(name, list(shape), dt))
        return Buf(t, name)

    def psum(self, shape, dt, name=None):
        self.nbuf += 1
        name = (name or 'p') + '_%d' % self.nbuf
        t = self.pes.enter_context(self.nc.psum_tensor(name, list(shape), dt))
        return t

    def dsem(self, name):
        if name not in self.dnames:
            self.dnames[name] = len(self.dnames)
            assert len(self.dnames) <= len(self.dsems), 'too many dma sems'
        return self.dnames[name]

    def _need(self, eng, deps):
        E = self.e[eng]
        best = {}
        for key, val in deps:
            if val > best.get(key, 0):
                best[key] = val
        for key, val in best.items():
            if key == ('e', 'pe') and eng == 'pe':
                continue
            if self.seen[eng].get(key, 0) >= val:
                continue
            if key[0] == 'e':
                E.wait_ge(self.sem[key[1]], val)
            else:
                E.wait_ge(self.dsems[key[1]][0], val)
            self.seen[eng][key] = val
            self.ninst += 1

    @staticmethod
    def _deps(reads, writes):
        deps = []
        for b in reads:
            if b.w is not None:
                deps.append((b.w[0:2], b.w[2]))
        for b in writes:
            if b.w is not None:
                deps.append((b.w[0:2], b.w[2]))
            for key, val in b.r.items():
                deps.append((key, val))
        return deps

    def op(self, eng, fn, reads=(), writes=()):
        self._need(eng, self._deps(reads, writes))
        ins = fn(self.e[eng])
        self.cnt[eng] += 1
        c = self.cnt[eng]
        ins.then_inc(self.sem[eng], 1)
        self.ninst += 1
        for b in reads:
            b.r[('e', eng)] = c
        for b in writes:
            b.w = ('e', eng, c)
            b.r = {}
        return ins

    def dma(self, q, out_ap, in_ap, sem, out_b=None, in_b=None, **kw):
        self._need(q, self._deps([in_b] if in_b else [], [out_b] if out_b else []))
        ins = self.e[q].dma_start(out=out_ap, in_=in_ap, **kw)
        si = self.dsem(sem)
        s = self.dsems[si]
        s[1] += 16
        ins.then_inc(s[0], 16)
        self.ninst += 1
        if in_b is not None:
            in_b.r[('d', si)] = s[1]
        if out_b is not None:
            out_b.w = ('d', si, s[1])
            out_b.r = {}
        return ins

    def barrier(self):
        for eng in self.ENG:
            deps = [(('e', x), self.cnt[x]) for x in self.ENG if x != eng and self.cnt[x] > 0]
            deps += [(('d', i), s[1]) for i, s in enumerate(self.dsems) if s[1] > 0]
            self._need(eng, deps)


def _bf16_round(x):
    x = np.asarray(x, np.float32)
    u = x.view(np.uint32)
    r = ((u >> 16) & 1) + 0x7FFF
    return ((u + r) & 0xFFFF0000).view(np.float32)


def _slopes(n):
    return (2.0 ** (-8.0 * np.arange(1, n + 1, dtype=np.float64) / n))


def host_consts():
    c = {}
    kk = np.arange(128)[:, None]
    qq = np.arange(128)[None, :]
    blocks = []
    for dl in range(-3, 4):
        dist = 128 * dl + qq - kk
        blocks.append(np.where(dist >= 0, 0.0, -BIG))
    c['c_maskc'] = np.concatenate(blocks, 1).astype(np.float32)
    blocks = []
    for dl in range(-3, 20):
        dist = 128 * dl + qq - kk
        m = ((dist >= 0) & (dist <= 128)).astype(np.float64) + ((dist >= 0) & (dist <= 512) & (dist % 4 == 0)) \
            + ((dist >= 0) & (dist <= 2048) & (dist % 16 == 0))
        with np.errstate(divide='ignore'):
            v = np.where(m > 0, 8.0 * np.log(np.maximum(m, 1e-30)), -BIG)
        blocks.append(v)
    c['c_maskd'] = np.concatenate(blocks, 1).astype(np.float32)
    half = 16
    inv = (10000.0 ** (-np.arange(half, dtype=np.float32) / half)).astype(np.float32)
    c['c_invf'] = (inv / np.float32(TWO_PI)).astype(np.float32).reshape(1, 16)
    p = np.arange(128, dtype=np.float32)[:, None]
    t = np.arange(32, dtype=np.float32)[None, :]
    z = np.zeros((128, 32), np.float32)
    c['c_tokq'] = np.stack([-(p + z), -(p + z), -128.0 * (t + z), -128.0 * (t + z)], -1).astype(np.float32)
    c['c_tokk'] = np.stack([p + z, p + z, t + z, t + z], -1).astype(np.float32)
    for nm, nh in (('0', 8), ('1', 16)):
        M = 8.0 * _slopes(nh)
        hi = _bf16_round(M.astype(np.float32))
        lo = _bf16_round((M - hi.astype(np.float64)).astype(np.float32))
        c['c_hq' + nm] = np.stack([hi, lo, 128.0 * hi, 128.0 * lo], -1).astype(np.float32).reshape(1, nh * 4)
        c['c_hk' + nm] = np.stack([hi, lo, hi, lo], -1).astype(np.float32).reshape(1, nh * 4)
    return c


CONST_SHAPES = {
    'c_maskc': [128, 7 * 128], 'c_maskd': [128, 23 * 128], 'c_invf': [1, 16],
    'c_tokq': [128, 32, 4], 'c_tokk': [128, 32, 4],
    'c_hq0': [1, 32], 'c_hk0': [1, 32], 'c_hq1': [1, 64], 'c_hk1': [1, 64],
}

IN_SHAPES = {
    'x': ([S, D], F32), 'p': ([2, S, 256], F32), 'positions': ([128, 32], I32),
    'e_w_in': ([1, 1024, 2208], F32), 'e_cq_norm': ([1, 384], F32), 'e_ckv_norm': ([1, 256], F32),
    'e_w_uq': ([1, 384, 768], F32), 'e_w_ukv': ([1, 256, 1024], F32),
    'e_qn_nope': ([1, 64], F32), 'e_qn_rope': ([1, 32], F32), 'e_kn_nope': ([1, 64], F32), 'e_kn_rope': ([1, 32], F32),
    'e_dil_qn': ([1, 64], F32), 'e_dil_kn': ([1, 64], F32), 'e_w_out': ([1, 1024, 1024], F32),
    'o_w_in': ([1, 1024, 3072], F32), 'o_qn': ([1, 64], F32), 'o_kn': ([1, 64], F32), 'o_w_out': ([1, 1024, 1024], F32),
    'mix_norm': ([2, 1024], F32), 'ff_norm': ([2, 1024], F32), 'w_ff1': ([2, 1024, 4096], F32),
    'w_ff2': ([2, 4096, 1024], F32), 'ple_norm': ([2, 1024], F32), 'w_ple_gate': ([2, 1024, 1024], F32),
    'w_ple_proj': ([2, 256, 1024], F32),
}


class Prog:
    def __init__(self, cfg=None):
        cfg = cfg or {}
        self.cfg = cfg
        self.nc = bass.Bass("TRN2", target_bir_lowering=False)
        nc = self.nc
        self.I = {}
        for name, (shape, dt) in IN_SHAPES.items():
            self.I[name] = nc.dram_tensor(name, shape, dt, kind="ExternalInput")
        for name, shape in CONST_SHAPES.items():
            self.I[name] = nc.dram_tensor(name, shape, F32, kind="ExternalInput")
        self.y = nc.dram_tensor("y", [S, D], F32, kind="ExternalOutput")
        ext_in = cfg.get('scr_in', ())
        ext_out = cfg.get('scr_out', ())

        def scr(name, shape, dt):
            if name in ext_in:
                return nc.dram_tensor(name, shape, dt, kind="ExternalInput")
            if name in ext_out:
                return nc.dram_tensor(name, shape, dt, kind="ExternalOutput")
            return nc.dram_tensor(name, shape, dt)

        self.hnT = scr('hnT', [8, 128, S], BF16)
        self.QT = scr('QT', [16, 128, S], BF16)
        self.KT = scr('KT', [16, 128, S], BF16)
        self.V = scr('V', [16, S, 128], BF16)
        self.mixT = scr('mixT', [8, 128, S], BF16)
        self.hA = scr('hA', [S, D], F32)
        self.hB = scr('hB', [S, D], F32)
        self.hC = scr('hC', [S, D], F32)

    def load_bc(self, k, dst, dst_ap, row_ap, n, sem='cst'):
        k.dma('sp', dst_ap, row_ap.partition_broadcast(128), sem, out_b=dst)

    def load_w(self, k, dst, w2d, KC, N, sem='w'):
        src = w2d.rearrange("(kc p) n -> p kc n", p=128)
        si = k.dsem(sem)
        vals = []
        for k0 in range(0, KC, 8):
            k1 = min(KC, k0 + 8)
            for c0 in range(0, N, 512):
                n = min(512, N - c0)
                if len(vals) >= 6:
                    k._need('pool', [(('d', si), vals[-6])])
                k.dma('pool', dst[:, k0:k1, c0:c0 + n], src[:, k0:k1, c0:c0 + n], sem, out_b=dst)
                vals.append(k.dsems[si][1])

    def rstd(self, k, ss, ss_ap, out, out_ap, n):
        k.op('act', lambda e: e.activation(out=out_ap, in_=ss_ap, func=AF.Sqrt, scale=1.0 / n, bias=self.epsb[:, 0:1]),
             reads=[ss, self.epsb], writes=[out])
        k.op('dve', lambda e: e.reciprocal(out=out_ap, in_=out_ap), reads=[out], writes=[out])

    def setup_globals(self, k):
        self.ident = k.sbuf([128, 128], BF16, 'ident', glob=True)
        self.onesf = k.sbuf([128, 128], F32, 'onesf', glob=True)
        self.epsb = k.sbuf([128, 1], F32, 'epsb', glob=True)
        self.onesb = k.sbuf([128, 2], BF16, 'onesb', glob=True)
        identf = k.sbuf([128, 128], F32, 'identf', glob=True)
        k.op('pool', lambda e: e.memset(self.onesf[:, :], 1.0), writes=[self.onesf])
        k.op('pool', lambda e: e.memset(self.epsb[:, :], EPS), writes=[self.epsb])
        k.op('pool', lambda e: e.memset(self.onesb[:, :], 1.0), writes=[self.onesb])
        k.op('pool', lambda e: e.affine_select(out=identf[:, :], in_=self.onesf[:, :], pattern=[[-1, 128]],
                                               compare_op=ALU.is_equal, fill=0.0, base=0, channel_multiplier=1),
             reads=[self.onesf], writes=[identf])
        k.op('dve', lambda e: e.tensor_copy(out=self.ident[:, :], in_=identf[:, :]), reads=[identf], writes=[self.ident])

    class NormT:
        def __init__(self, P, k, psB):
            self.P = P
            self.k = k
            self.junk = [k.sbuf([128, 1024], BF16, 'nj') for _ in range(2)]
            self.ss = [k.sbuf([128, 2], F32, 'nss') for _ in range(2)]
            self.hn = [k.sbuf([128, 1024], BF16, 'nhn') for _ in range(2)]
            self.ps = [Buf(psB[:, i * 1024:(i + 1) * 1024], 'npsT%d' % i) for i in range(psB.shape[1] // 1024)]
            self.i = 0

        def run_a(self, hbuf, hap, gain):
            k, P = self.k, self.P
            i = self.i
            self.i += 1
            junk, ss, hn = self.junk[i % 2], self.ss[i % 2], self.hn[i % 2]
            k.op('act', lambda e: e.activation(out=junk[:, :], in_=hap, func=AF.Square, accum_out=ss[:, 0:1]),
                 reads=[hbuf], writes=[junk, ss])
            P.rstd(k, ss, ss[:, 0:1], ss, ss[:, 1:2], 1024)
            k.op('dve', lambda e: e.scalar_tensor_tensor(out=hn[:, :], in0=hap, scalar=ss[:, 1:2], in1=gain[:, :],
                                                         op0=ALU.mult, op1=ALU.mult), reads=[hbuf, ss, gain], writes=[hn])
            return i

        def run_b1(self, i):
            k, P = self.k, self.P
            hn = self.hn[i % 2]
            ps = self.ps[i % len(self.ps)]
            for c in range(8):
                k.op('pe', lambda e, c=c: e.transpose(out=ps[:, c * 128:(c + 1) * 128], in_=hn[:, c * 128:(c + 1) * 128],
                                                      identity=P.ident[:, :]), reads=[hn, P.ident], writes=[ps])

        def run_b2(self, i, stage, col0):
            k = self.k
            ps = self.ps[i % len(self.ps)]
            k.op('act', lambda e: e.copy(out=stage[:, :, col0:col0 + 128], in_=ps[:, :].rearrange("p (c t) -> p c t", c=8)),
                 reads=[ps], writes=[stage])

        def run_b(self, i, stage, col0):
            self.run_b1(i)
            self.run_b2(i, stage, col0)

        def run(self, hbuf, hap, gain, stage, col0):
            i = self.run_a(hbuf, hap, gain)
            self.run_b(i, stage, col0)

    def phase_norm0(self, k):
        I = self.I
        k.begin_phase()
        gain = k.sbuf([128, 1024], F32, 'gain')
        self.load_bc(k, gain, gain[:, :], I['mix_norm'][0, :], 1024)
        psB = k.psum([128, 2048], BF16)
        nt = self.NormT(self, k, psB)
        hts = [k.sbuf([128, 1024], F32, 'ht') for _ in range(3)]
        stages = [k.sbuf([128, 8, 512], BF16, 'stg') for _ in range(2)]
        dstT = self.hnT[:, :, :].rearrange("c p t -> p c t")
        deferred = []
        for T in range(8):
            stage = stages[T % 2]
            for s in range(4):
                t = 4 * T + s
                ht = hts[t % 3]
                k.dma('sp', ht[:, :], I['x'][t * 128:(t + 1) * 128, :], 'ld%d' % (t % 3), out_b=ht)
                i = nt.run_a(ht, ht[:, :], gain)
                for f in deferred:
                    f()
                deferred = [lambda i=i, stage=stage, s=s: nt.run_b(i, stage, s * 128)]
            deferred.append(lambda T=T, stage=stage: k.dma('sp', dstT[:, :, T * 512:(T + 1) * 512], stage[:, :, :], 'st%d' % (T % 2), in_b=stage))
        for f in deferred:
            f()
        k.end_phase()

    def phase_p0(self, k):
        I = self.I
        k.begin_phase()
        Win = k.sbuf([128, 8, 2208], BF16, 'Win')
        Wuq = k.sbuf([128, 3, 768], BF16, 'Wuq')
        Wukv = k.sbuf([128, 2, 1024], BF16, 'Wukv')
        self.load_w(k, Win, I['e_w_in'][0], 8, 2208)
        self.load_w(k, Wuq, I['e_w_uq'][0], 3, 768)
        self.load_w(k, Wukv, I['e_w_ukv'][0], 2, 1024)
        g_cq = k.sbuf([128, 384], F32); self.load_bc(k, g_cq, g_cq[:, :], I['e_cq_norm'][0, :], 384)
        g_ckv = k.sbuf([128, 256], F32); self.load_bc(k, g_ckv, g_ckv[:, :], I['e_ckv_norm'][0, :], 256)
        g64 = k.sbuf([128, 4, 64], F32)
        for i, nm in enumerate(['e_qn_nope', 'e_kn_nope', 'e_dil_qn', 'e_dil_kn']):
            self.load_bc(k, g64, g64[:, i, :], I[nm][0, :], 64)
        gcol = k.sbuf([128, 4], F32, 'gcol')
        k.op('pool', lambda e: e.memset(gcol[:, :], 1.0), writes=[gcol])
        for i, nm in enumerate(['e_qn_nope', 'e_kn_nope', 'e_dil_qn', 'e_dil_kn']):
            k.dma('sp', gcol[0:64, i:i + 1], I[nm][0, :].rearrange("(p o) -> p o", o=1), 'cst', out_b=gcol)
        g32 = k.sbuf([128, 2, 32], F32)
        for i, nm in enumerate(['e_qn_rope', 'e_kn_rope']):
            self.load_bc(k, g32, g32[:, i, :], I[nm][0, :], 32)
        tokq = k.sbuf([128, 32, 4], F32); k.dma('sp', tokq[:, :, :], I['c_tokq'][:, :, :], 'cst', out_b=tokq)
        tokk = k.sbuf([128, 32, 4], F32); k.dma('sp', tokk[:, :, :], I['c_tokk'][:, :, :], 'cst', out_b=tokk)
        hq = k.sbuf([128, 8, 4], F32); self.load_bc(k, hq, hq[:, :, :].rearrange("p h c -> p (h c)"), I['c_hq0'][0, :], 32)
        hk = k.sbuf([128, 8, 4], F32); self.load_bc(k, hk, hk[:, :, :].rearrange("p h c -> p (h c)"), I['c_hk0'][0, :], 32)
        invf = k.sbuf([128, 16], F32); self.load_bc(k, invf, invf[:, :], I['c_invf'][0, :], 16)
        posi = k.sbuf([128, 32], I32); k.dma('sp', posi[:, :], I['positions'][:, :], 'cst', out_b=posi)
        posf = k.sbuf([128, 32], F32)
        k.op('dve', lambda e: e.tensor_copy(out=posf[:, :], in_=posi[:, :]), reads=[posi], writes=[posf])
        xt = k.sbuf([128, 32, 16], F32)
        k.op('dve', lambda e: e.tensor_tensor(out=xt[:, :, :], in0=posf[:, :].unsqueeze(2).to_broadcast([128, 32, 16]),
                                              in1=invf[:, :].unsqueeze(1).to_broadcast([128, 32, 16]), op=ALU.mult),
             reads=[posf, invf], writes=[xt])
        sin_all = k.sbuf([128, 32, 16], F32)
        cos_all = k.sbuf([128, 32, 16], F32)
        t1 = k.sbuf([128, 32, 16], F32)
        t2 = k.sbuf([128, 32, 16], F32)
        for dst, shift in ((sin_all, 0.0), (cos_all, 0.25)):
            k.op('dve', lambda e, shift=shift: e.tensor_scalar(out=t2[:, :, :], in0=xt[:, :, :], scalar1=shift, scalar2=None, op0=ALU.add),
                 reads=[xt], writes=[t2])
            k.op('dve', lambda e: e.tensor_scalar(out=t1[:, :, :], in0=t2[:, :, :], scalar1=MAGIC, scalar2=None, op0=ALU.add),
                 reads=[t2], writes=[t1])
            k.op('dve', lambda e: e.scalar_tensor_tensor(out=t1[:, :, :], in0=t1[:, :, :], scalar=MAGIC, in1=t2[:, :, :],
                                                         op0=ALU.subtract, op1=ALU.subtract), reads=[t1, t2], writes=[t1])
            k.op('act', lambda e, dst=dst: e.activation(out=dst[:, :, :], in_=t1[:, :, :], func=AF.Sin, scale=-TWO_PI * (1 - 1e-6)),
                 reads=[t1], writes=[dst])

        psF = k.psum([128, 3584], F32)
        psB = k.psum([128, 1024], BF16)
        bank = [Buf(psF[:, i * 512:(i + 1) * 512], 'bank%d' % i) for i in range(7)]
        psT = Buf(psB[:, :], 'psT')

        hnTs = [k.sbuf([128, 8, 256], BF16, 'hnTs') for _ in range(2)]
        QTs = [k.sbuf([128, 16, 256], BF16, 'QTs') for _ in range(2)]
        KTs = [k.sbuf([128, 16, 256], BF16, 'KTs') for _ in range(2)]
        Vtm = [k.sbuf([128, 16, 128], BF16, 'Vtm') for _ in range(2)]
        Qtm = [k.sbuf([128, 16, 96], BF16, 'Qtm') for _ in range(2)]
        Ktm = [k.sbuf([128, 16, 96], BF16, 'Ktm') for _ in range(2)]
        for i in range(2):
            k.op('pool', lambda e, i=i: e.memset(Vtm[i][:, :, 64:128], 1.0), writes=[Vtm[i]])
            k.op('pool', lambda e, i=i: e.memset(Qtm[i][:, :, :], 0.0), writes=[Qtm[i]])
            k.op('pool', lambda e, i=i: e.memset(Ktm[i][:, :, :], 0.0), writes=[Ktm[i]])
            k.op('dve', lambda e, i=i: e.tensor_copy(out=Qtm[i][:, 8:16, 64:68], in_=hq[:, :, :]), reads=[hq], writes=[Qtm[i]])
            k.op('dve', lambda e, i=i: e.tensor_copy(out=Ktm[i][:, 8:16, 68:72], in_=hk[:, :, :]), reads=[hk], writes=[Ktm[i]])
        junk = k.sbuf([128, 512], F32, 'junk')
        ssA = k.sbuf([128, 8], F32, 'ssA')
        krs = k.sbuf([128, 32], F32, 'krs')
        krn = k.sbuf([128, 32], F32, 'krn')
        krr_l = [k.sbuf([128, 32], F32, 'krr') for _ in range(2)]
        rka = k.sbuf([128, 1, 16], F32, 'rka')
        rkb = k.sbuf([128, 1, 16], F32, 'rkb')
        cq_bf = k.sbuf([128, 384], BF16, 'cq_bf')
        ckv_bf = k.sbuf([128, 256], BF16, 'ckv_bf')
        cT = k.sbuf([128, 5, 128], BF16, 'cT')
        qs_l = [k.sbuf([128, 768], F32, 'qs') for _ in range(2)]
        kvs_l = [k.sbuf([128, 1024], F32, 'kvs') for _ in range(2)]
        dqs_l = [k.sbuf([128, 512], F32, 'dqs') for _ in range(2)]
        dks_l = [k.sbuf([128, 512], F32, 'dks') for _ in range(2)]
        tmpA = k.sbuf([128, 1024], F32, 'tmpA')
        tmpB = k.sbuf([128, 1024], F32, 'tmpB')
        ssq = k.sbuf([128, 40], F32, 'ssq')
        rsq = k.sbuf([128, 40], F32, 'rsq')
        qrn = k.sbuf([128, 8, 32], F32, 'qrn')
        ra = k.sbuf([128, 8, 16], F32, 'ra')
        rb = k.sbuf([128, 8, 16], F32, 'rb')
        rc = k.sbuf([128, 8, 16], F32, 'rc')
        rd = k.sbuf([128, 8, 16], F32, 'rd')

        srcT = self.hnT[:, :, :].rearrange("c p t -> p c t")
        QTd = self.QT[:, :, :].rearrange("h r t -> r h t")
        KTd = self.KT[:, :, :].rearrange("h r t -> r h t")
        Vd = self.V[:, :, :].rearrange("h t c -> t h c")

        def hview(ap, h, d):
            return ap.rearrange("p (h d) -> p h d", h=h)

        def sumsq(src3, H, dh, dst_ap, tmp):
            t3 = tmp[:, 0:H * dh].rearrange("p (h d) -> p h d", h=H)
            k.op('dve', lambda e: e.tensor_tensor(out=t3, in0=src3[1], in1=src3[1], op=ALU.mult), reads=[src3[0]], writes=[tmp])
            k.op('dve', lambda e: e.tensor_reduce(out=dst_ap, in_=t3, axis=AX.X, op=ALU.add), reads=[tmp], writes=[ssq])

        def headnorm(src3, H, dh, rs_ap, gain_ap, dst, dst_ap, tmp, eng2='pool'):
            t3 = tmp[:, 0:H * dh].rearrange("p (h d) -> p h d", h=H)
            k.op('dve', lambda e: e.tensor_tensor(out=t3, in0=src3[1], in1=rs_ap.unsqueeze(2).to_broadcast([128, H, dh]), op=ALU.mult),
                 reads=[src3[0], rsq], writes=[tmp])
            k.op(eng2, lambda e: e.tensor_tensor(out=dst_ap, in0=t3, in1=gain_ap.unsqueeze(1).to_broadcast([128, H, dh]), op=ALU.mult),
                 reads=[tmp, g64, g32], writes=[dst])

        k.dma('sp', hnTs[0][:, :, :], srcT[:, :, 0:256], 'ld0', out_b=hnTs[0])

        def stageA(t):
            T2, s = divmod(t, 2)
            hs = hnTs[T2 % 2]
            if s == 0 and T2 + 1 < 16:
                k.dma('sp', hnTs[(T2 + 1) % 2][:, :, :], srcT[:, :, (T2 + 1) * 256:(T2 + 2) * 256], 'ld%d' % ((T2 + 1) % 2),
                      out_b=hnTs[(T2 + 1) % 2])
            vt = Vtm[t % 2]
            qs, kvs, dqs, dks, krr = qs_l[t % 2], kvs_l[t % 2], dqs_l[t % 2], dks_l[t % 2], krr_l[t % 2]
            ra, rb = rka, rkb
            for (bk, c0, n) in ((0, 0, 384), (1, 384, 288), (4, 672, 512), (5, 1184, 512), (6, 1696, 512)):
                for kc in range(8):
                    k.op('pe', lambda e, bk=bk, c0=c0, n=n, kc=kc: e.matmul(
                        bank[bk][:, 0:n], lhsT=hs[:, kc, s * 128:(s + 1) * 128], rhs=Win[:, kc, c0:c0 + n],
                        start=(kc == 0), stop=(kc == 7)), reads=[hs, Win], writes=[bank[bk]])
            k.op('act', lambda e: e.activation(out=junk[:, 0:384], in_=bank[0][:, 0:384], func=AF.Square, accum_out=ssA[:, 0:1]),
                 reads=[bank[0]], writes=[junk, ssA])
            k.op('act', lambda e: e.activation(out=junk[:, 0:256], in_=bank[1][:, 0:256], func=AF.Square, accum_out=ssA[:, 1:2]),
                 reads=[bank[1]], writes=[junk, ssA])
            k.op('act', lambda e: e.copy(out=krs[:, :], in_=bank[1][:, 256:288]), reads=[bank[1]], writes=[krs])
            k.op('act', lambda e: e.activation(out=junk[:, 0:32], in_=krs[:, :], func=AF.Square, accum_out=ssA[:, 2:3]),
                 reads=[krs], writes=[junk, ssA])
            self.rstd(k, ssA, ssA[:, 0:1], ssA, ssA[:, 4:5], 384)
            self.rstd(k, ssA, ssA[:, 1:2], ssA, ssA[:, 5:6], 256)
            self.rstd(k, ssA, ssA[:, 2:3], ssA, ssA[:, 6:7], 32)
            k.op('dve', lambda e: e.scalar_tensor_tensor(out=cq_bf[:, :], in0=bank[0][:, 0:384], scalar=ssA[:, 4:5], in1=g_cq[:, :],
                                                         op0=ALU.mult, op1=ALU.mult), reads=[bank[0], ssA, g_cq], writes=[cq_bf])
            k.op('dve', lambda e: e.scalar_tensor_tensor(out=ckv_bf[:, :], in0=bank[1][:, 0:256], scalar=ssA[:, 5:6], in1=g_ckv[:, :],
                                                         op0=ALU.mult, op1=ALU.mult), reads=[bank[1], ssA, g_ckv], writes=[ckv_bf])
            k.op('dve', lambda e: e.scalar_tensor_tensor(out=krn[:, :], in0=krs[:, :], scalar=ssA[:, 6:7], in1=g32[:, 1, :],
                                                         op0=ALU.mult, op1=ALU.mult), reads=[krs, ssA, g32], writes=[krn])
            cs = cos_all[:, t, :]
            sn = sin_all[:, t, :]
            k.op('pool', lambda e: e.tensor_tensor(out=ra[:, 0, :], in0=krn[:, 0:16], in1=cs, op=ALU.mult), reads=[krn, cos_all], writes=[ra])
            k.op('pool', lambda e: e.tensor_tensor(out=rb[:, 0, :], in0=krn[:, 16:32], in1=sn, op=ALU.mult), reads=[krn, sin_all], writes=[rb])
            k.op('pool', lambda e: e.tensor_tensor(out=krr[:, 0:16], in0=ra[:, 0, :], in1=rb[:, 0, :], op=ALU.subtract), reads=[ra, rb], writes=[krr])
            k.op('pool', lambda e: e.tensor_tensor(out=ra[:, 0, :], in0=krn[:, 0:16], in1=sn, op=ALU.mult), reads=[krn, sin_all], writes=[ra])
            k.op('pool', lambda e: e.tensor_tensor(out=rb[:, 0, :], in0=krn[:, 16:32], in1=cs, op=ALU.mult), reads=[krn, cos_all], writes=[rb])
            k.op('pool', lambda e: e.tensor_tensor(out=krr[:, 16:32], in0=ra[:, 0, :], in1=rb[:, 0, :], op=ALU.add), reads=[ra, rb], writes=[krr])
            for c in range(3):
                k.op('pe', lambda e, c=c: e.transpose(out=psT[:, c * 128:(c + 1) * 128], in_=cq_bf[:, c * 128:(c + 1) * 128],
                                                      identity=self.ident[:, :]), reads=[cq_bf, self.ident], writes=[psT])
            for c in range(2):
                k.op('pe', lambda e, c=c: e.transpose(out=psT[:, (3 + c) * 128:(4 + c) * 128], in_=ckv_bf[:, c * 128:(c + 1) * 128],
                                                      identity=self.ident[:, :]), reads=[ckv_bf, self.ident], writes=[psT])
            k.op('act', lambda e: e.copy(out=cT[:, :, :], in_=psT[:, 0:640].rearrange("p (c t) -> p c t", c=5)), reads=[psT], writes=[cT])
            for (bk, c0, n) in ((0, 0, 512), (1, 512, 256)):
                for kc in range(3):
                    k.op('pe', lambda e, bk=bk, c0=c0, n=n, kc=kc: e.matmul(
                        bank[bk][:, 0:n], lhsT=cT[:, kc, :], rhs=Wuq[:, kc, c0:c0 + n], start=(kc == 0), stop=(kc == 2)),
                        reads=[cT, Wuq], writes=[bank[bk]])
            for (bk, c0, n) in ((2, 0, 512), (3, 512, 512)):
                for kc in range(2):
                    k.op('pe', lambda e, bk=bk, c0=c0, n=n, kc=kc: e.matmul(
                        bank[bk][:, 0:n], lhsT=cT[:, 3 + kc, :], rhs=Wukv[:, kc, c0:c0 + n], start=(kc == 0), stop=(kc == 1)),
                        reads=[cT, Wukv], writes=[bank[bk]])
            k.op('act', lambda e: e.copy(out=qs[:, 0:512], in_=bank[0][:, 0:512]), reads=[bank[0]], writes=[qs])
            k.op('act', lambda e: e.copy(out=qs[:, 512:768], in_=bank[1][:, 0:256]), reads=[bank[1]], writes=[qs])
            k.op('act', lambda e: e.copy(out=kvs[:, 0:512], in_=bank[2][:, 0:512]), reads=[bank[2]], writes=[kvs])
            k.op('act', lambda e: e.copy(out=kvs[:, 512:1024], in_=bank[3][:, 0:512]), reads=[bank[3]], writes=[kvs])
            k.op('act', lambda e: e.copy(out=dqs[:, :], in_=bank[4][:, 0:512]), reads=[bank[4]], writes=[dqs])
            k.op('act', lambda e: e.copy(out=dks[:, :], in_=bank[5][:, 0:512]), reads=[bank[5]], writes=[dks])
            k.op('act', lambda e: e.copy(out=vt[:, 8:16, 0:64], in_=bank[6][:, 0:512].rearrange("p (h d) -> p h d", h=8)),
                 reads=[bank[6]], writes=[vt])
            q3 = qs[:, :].rearrange("p (h d) -> p h d", h=8)
            kv3 = kvs[:, :].rearrange("p (h d) -> p h d", h=8)
            dq3 = dqs[:, :].rearrange("p (h d) -> p h d", h=8)
            dk3 = dks[:, :].rearrange("p (h d) -> p h d", h=8)
            k.op('pool', lambda e: e.tensor_copy(out=vt[:, 0:8, 0:64], in_=kv3[:, :, 64:128]), reads=[kvs], writes=[vt])

        def stageBf(t):
            T2, s = divmod(t, 2)
            qts, kts = QTs[T2 % 2], KTs[T2 % 2]
            vt, qt, kt = Vtm[t % 2], Qtm[t % 2], Ktm[t % 2]
            qs, kvs, dqs, dks, krr = qs_l[t % 2], kvs_l[t % 2], dqs_l[t % 2], dks_l[t % 2], krr_l[t % 2]
            cs = cos_all[:, t, :]
            sn = sin_all[:, t, :]
            q3 = qs[:, :].rearrange("p (h d) -> p h d", h=8)
            kv3 = kvs[:, :].rearrange("p (h d) -> p h d", h=8)
            dq3 = dqs[:, :].rearrange("p (h d) -> p h d", h=8)
            dk3 = dks[:, :].rearrange("p (h d) -> p h d", h=8)
            sumsq((qs, q3[:, :, 0:64]), 8, 64, ssq[:, 0:8], tmpA)
            sumsq((kvs, kv3[:, :, 0:64]), 8, 64, ssq[:, 8:16], tmpA)
            sumsq((dqs, dq3), 8, 64, ssq[:, 16:24], tmpA)
            sumsq((dks, dk3), 8, 64, ssq[:, 24:32], tmpA)
            sumsq((qs, q3[:, :, 64:96]), 8, 32, ssq[:, 32:40], tmpA)
            self.rstd(k, ssq, ssq[:, 0:32], rsq, rsq[:, 0:32], 64)
            self.rstd(k, ssq, ssq[:, 32:40], rsq, rsq[:, 32:40], 32)
            def norm1(eng, srcb, src3, rs_ap, dst, dst_ap):
                k.op(eng, lambda e: e.tensor_tensor(out=dst_ap, in0=src3, in1=rs_ap.unsqueeze(2).to_broadcast([128, 8, 64]), op=ALU.mult),
                     reads=[srcb, rsq], writes=[dst])
            norm1('dve', qs, q3[:, :, 0:64], rsq[:, 0:8], qt, qt[:, 0:8, 0:64])
            norm1('pool', kvs, kv3[:, :, 0:64], rsq[:, 8:16], kt, kt[:, 0:8, 0:64])
            norm1('dve', dqs, dq3, rsq[:, 16:24], qt, qt[:, 8:16, 0:64])
            norm1('pool', dks, dk3, rsq[:, 24:32], kt, kt[:, 8:16, 0:64])
            headnorm((qs, q3[:, :, 64:96]), 8, 32, rsq[:, 32:40], g32[:, 0, :], qrn, qrn[:, :, :], tmpA)
            csb = cs.unsqueeze(1).to_broadcast([128, 8, 16])
            snb = sn.unsqueeze(1).to_broadcast([128, 8, 16])
            k.op('dve', lambda e: e.tensor_tensor(out=ra[:, :, :], in0=qrn[:, :, 0:16], in1=csb, op=ALU.mult), reads=[qrn, cos_all], writes=[ra])
            k.op('dve', lambda e: e.tensor_tensor(out=rb[:, :, :], in0=qrn[:, :, 16:32], in1=snb, op=ALU.mult), reads=[qrn, sin_all], writes=[rb])
            k.op('pool', lambda e: e.tensor_tensor(out=rc[:, :, :], in0=qrn[:, :, 0:16], in1=snb, op=ALU.mult), reads=[qrn, sin_all], writes=[rc])
            k.op('pool', lambda e: e.tensor_tensor(out=rd[:, :, :], in0=qrn[:, :, 16:32], in1=csb, op=ALU.mult), reads=[qrn, cos_all], writes=[rd])
            k.op('dve', lambda e: e.tensor_tensor(out=qt[:, 0:8, 64:80], in0=ra[:, :, :], in1=rb[:, :, :], op=ALU.subtract), reads=[ra, rb], writes=[qt])
            k.op('pool', lambda e: e.tensor_tensor(out=qt[:, 0:8, 80:96], in0=rc[:, :, :], in1=rd[:, :, :], op=ALU.add), reads=[rc, rd], writes=[qt])
            k.op('pool', lambda e: e.tensor_copy(out=kt[:, 0:8, 64:96], in_=krr[:, :].unsqueeze(1).to_broadcast([128, 8, 32])),
                 reads=[krr], writes=[kt])
            k.op('pool', lambda e: e.tensor_copy(out=qt[:, 8:16, 68:72], in_=tokq[:, t, :].unsqueeze(1).to_broadcast([128, 8, 4])),
                 reads=[tokq], writes=[qt])
            k.op('pool', lambda e: e.tensor_copy(out=kt[:, 8:16, 64:68], in_=tokk[:, t, :].unsqueeze(1).to_broadcast([128, 8, 4])),
                 reads=[tokk], writes=[kt])

        def stageBb(t):
            T2, s = divmod(t, 2)
            qts, kts = QTs[T2 % 2], KTs[T2 % 2]
            vt, qt, kt = Vtm[t % 2], Qtm[t % 2], Ktm[t % 2]
            qs, kvs, dqs, dks, krr = qs_l[t % 2], kvs_l[t % 2], dqs_l[t % 2], dks_l[t % 2], krr_l[t % 2]
            cs = cos_all[:, t, :]
            sn = sin_all[:, t, :]
            q3 = qs[:, :].rearrange("p (h d) -> p h d", h=8)
            kv3 = kvs[:, :].rearrange("p (h d) -> p h d", h=8)
            dq3 = dqs[:, :].rearrange("p (h d) -> p h d", h=8)
            dk3 = dks[:, :].rearrange("p (h d) -> p h d", h=8)
            for (src, dst, h0, R, gc) in ((qt, qts, 0, 96, 0), (qt, qts, 8, 72, 2), (kt, kts, 0, 96, 1), (kt, kts, 8, 72, 3)):
                for hh in range(8):
                    k.op('pe', lambda e, src=src, h0=h0, hh=hh, R=R: e.transpose(
                        out=psT[0:R, hh * 128:(hh + 1) * 128], in_=src[:, h0 + hh, 0:R], identity=self.ident[:, :]),
                        reads=[src, self.ident], writes=[psT])
                k.op('act', lambda e, dst=dst, h0=h0, R=R, gc=gc: e.mul(
                    out=dst[0:R, h0:h0 + 8, s * 128:(s + 1) * 128], in_=psT[0:R, :].rearrange("p (h t) -> p h t", h=8),
                    mul=gcol[0:R, gc:gc + 1]), reads=[psT, gcol], writes=[dst])
            k.dma('sp', Vd[t * 128:(t + 1) * 128, :, :], vt[:, :, :], 'stv%d' % (t % 2), in_b=vt)
            if s == 1:
                c0 = T2 * 256
                k.dma('sp', QTd[0:96, 0:8, c0:c0 + 256], qts[0:96, 0:8, :], 'stq%d' % (T2 % 2), in_b=qts)
                k.dma('sp', QTd[0:72, 8:16, c0:c0 + 256], qts[0:72, 8:16, :], 'stq%d' % (T2 % 2), in_b=qts)
                k.dma('sp', KTd[0:96, 0:8, c0:c0 + 256], kts[0:96, 0:8, :], 'stk%d' % (T2 % 2), in_b=kts)
                k.dma('sp', KTd[0:72, 8:16, c0:c0 + 256], kts[0:72, 8:16, :], 'stk%d' % (T2 % 2), in_b=kts)

        for i in range(33):
            if i >= 1:
                stageBf(i - 1)
            if i < 32:
                stageA(i)
            if i >= 1:
                stageBb(i - 1)
        k.end_phase()

    def phase_attn(self, k, heads):
        I = self.I
        k.begin_phase()
        maskc = k.sbuf([128, 7 * 128], BF16, 'maskc')
        k.dma('pool', maskc[:, :], I['c_maskc'][:, :], 'cst', out_b=maskc)
        use_d = any(h['mask'] == 'd' for h in heads)
        if use_d:
            maskd = k.sbuf([128, 23 * 128], BF16, 'maskd')
            for c0 in range(0, 23 * 128, 512):
                n = min(512, 23 * 128 - c0)
                k.dma('pool', maskd[:, c0:c0 + n], I['c_maskd'][:, c0:c0 + n], 'cst', out_b=maskd)
        psF = k.psum([128, 5 * 512], F32)
        Sb = [Buf(psF[:, i * 512:(i + 1) * 512], 'S%d' % i) for i in range(3)]
        Ob = [Buf(psF[:, (3 + i) * 512:(4 + i) * 512], 'O%d' % i) for i in range(2)]
        Pb = [k.sbuf([128, 512], BF16, 'P') for _ in range(3)]
        QTh = [k.sbuf([128, S], BF16, 'QTh') for _ in range(2)]
        KTh = [k.sbuf([128, S], BF16, 'KTh') for _ in range(2)]
        Vh = [k.sbuf([128, 32, 128], BF16, 'Vh') for _ in range(2)]
        rden = [k.sbuf([128, 512], F32, 'rden') for _ in range(2)]
        ost = [k.sbuf([64, S], BF16, 'ost') for _ in range(2)]

        def load_head(h):
            R = heads[h]['R']
            i = h % 2
            k.dma('sp', QTh[i][0:R, :], self.QT[h, 0:R, :], 'ldq%d' % i, out_b=QTh[i])
            k.dma('sp', KTh[i][0:R, :], self.KT[h, 0:R, :], 'ldk%d' % i, out_b=KTh[i])
            k.dma('sp', Vh[i][:, :, :], self.V[h, :, :].rearrange("(b p) c -> p b c", p=128), 'ldv%d' % i, out_b=Vh[i])

        load_head(0)
        gi = 0
        for h, hd in enumerate(heads):
            if h + 1 < len(heads):
                load_head(h + 1)
            R, scale, mk = hd['R'], hd['scale'], hd['mask']
            qT, kT, vv = QTh[h % 2], KTh[h % 2], Vh[h % 2]
            os_ = ost[h % 2]
            pairs = []
            for g in range(8):
                jlo = max(0, 4 * g - 16) if mk == 'd' else 0
                js = list(range(jlo, 4 * g + 4))
                for j in js:
                    pairs.append((g, j, j == js[0], j == js[-1]))

            def cols(g, j):
                d0 = 4 * g - j
                b0 = max(0, -d0)
                b1 = 4 if mk != 'd' else min(4, 17 - d0)
                return b0 * 128, b1 * 128

            def emit_qk(i):
                g, j, first, last = pairs[i]
                Sx = Sb[i % 3]
                d0 = 4 * g - j
                c0, c1 = cols(g, j)
                if mk == 'd':
                    need_mask, tab = True, maskd
                else:
                    need_mask, tab = (d0 <= 0), maskc
                off = (d0 + 3) * 128
                k.op('pe', lambda e: e.matmul(Sx[:, c0:c1], lhsT=kT[0:R, j * 128:(j + 1) * 128], rhs=qT[0:R, g * 512 + c0:g * 512 + c1],
                                              start=True, stop=not need_mask), reads=[kT, qT], writes=[Sx])
                if need_mask:
                    k.op('pe', lambda e: e.matmul(Sx[:, c0:c1], lhsT=self.ident[:, :], rhs=tab[:, off + c0:off + c1], start=False, stop=True),
                         reads=[self.ident, tab], writes=[Sx])
                Px = Pb[i % 3]
                k.op('act', lambda e: e.activation(out=Px[:, c0:c1], in_=Sx[:, c0:c1], func=AF.Exp, scale=float(scale)), reads=[Sx], writes=[Px])

            def emit_pv(i):
                nonlocal gi
                g, j, first, last = pairs[i]
                c0, c1 = cols(g, j)
                Px = Pb[i % 3]
                Ox = Ob[(gi) % 2]
                k.op('pe', lambda e: e.matmul(Ox[:, c0:c1], lhsT=vv[:, j, :], rhs=Px[:, c0:c1], start=first, stop=last), reads=[vv, Px], writes=[Ox])
                if last:
                    rd = rden[gi % 2]
                    k.op('dve', lambda e: e.reciprocal(out=rd[64:128, :], in_=Ox[64:128, :]), reads=[Ox], writes=[rd])
                    k.op('dve', lambda e: e.tensor_tensor(out=os_[0:64, g * 512:(g + 1) * 512], in0=Ox[0:64, :], in1=rd[64:128, :], op=ALU.mult),
                         reads=[Ox, rd], writes=[os_])
                    gi += 1

            LA = 2
            n = len(pairs)
            for i in range(n + LA):
                if i < n:
                    emit_qk(i)
                if i - LA >= 0:
                    emit_pv(i - LA)
            k.dma('sp', self.mixT[h // 2, (h % 2) * 64:(h % 2) * 64 + 64, :], os_[0:64, :], 'sto%d' % (h % 2), in_b=os_)
        k.end_phase()

    def phase_o1(self, k, w_out, h_in, h_out, gain_row):
        k.begin_phase()
        Wo = k.sbuf([128, 8, 1024], BF16, 'Wo')
        self.load_w(k, Wo, w_out, 8, 1024)
        gain = k.sbuf([128, 1024], F32, 'gain')
        self.load_bc(k, gain, gain[:, :], gain_row, 1024)
        psF = k.psum([128, 2048], F32)
        psB = k.psum([128, 2048], BF16)
        mixb = [Buf(psF[:, i * 1024:(i + 1) * 1024], 'mix%d' % i) for i in range(2)]
        nt = self.NormT(self, k, psB)
        mTs = [k.sbuf([128, 8, 512], BF16, 'mTs') for _ in range(2)]
        hts = [k.sbuf([128, 1024], F32, 'ht') for _ in range(3)]
        h1s = [k.sbuf([128, 1024], F32, 'h1') for _ in range(3)]
        stages = [k.sbuf([128, 8, 512], BF16, 'stg') for _ in range(2)]
        srcT = self.mixT[:, :, :].rearrange("c p t -> p c t")
        dstT = self.hnT[:, :, :].rearrange("c p t -> p c t")
        k.dma('sp', mTs[0][:, :, :], srcT[:, :, 0:512], 'ldm0', out_b=mTs[0])
        deferred = []
        for T in range(8):
            if T + 1 < 8:
                k.dma('sp', mTs[(T + 1) % 2][:, :, :], srcT[:, :, (T + 1) * 512:(T + 2) * 512], 'ldm%d' % ((T + 1) % 2), out_b=mTs[(T + 1) % 2])
            ms = mTs[T % 2]
            stage = stages[T % 2]
            for s in range(4):
                t = 4 * T + s
                ht, h1 = hts[t % 3], h1s[t % 3]
                mb = mixb[t % 2]
                k.dma('sp', ht[:, :], h_in[t * 128:(t + 1) * 128, :], 'ldh%d' % (t % 3), out_b=ht)
                for half in range(2):
                    for c in range(8):
                        k.op('pe', lambda e, half=half, c=c: e.matmul(
                            mb[:, half * 512:(half + 1) * 512], lhsT=ms[:, c, s * 128:(s + 1) * 128],
                            rhs=Wo[:, c, half * 512:(half + 1) * 512], start=(c == 0), stop=(c == 7)), reads=[ms, Wo], writes=[mb])
                for f in deferred:
                    f()
                deferred = []
                for half in range(2):
                    hsl = slice(half * 512, (half + 1) * 512)
                    k.op('dve', lambda e, hsl=hsl: e.tensor_tensor(out=h1[:, hsl], in0=mb[:, hsl], in1=ht[:, hsl], op=ALU.add), reads=[mb, ht], writes=[h1])
                k.dma('sp', h_out[t * 128:(t + 1) * 128, :], h1[:, :], 'sth%d' % (t % 3), in_b=h1)
                i = nt.run_a(h1, h1[:, :], gain)
                deferred.append(lambda i=i, stage=stage, s=s: nt.run_b(i, stage, s * 128))
            deferred.append(lambda T=T, stage=stage: k.dma('sp', dstT[:, :, T * 512:(T + 1) * 512], stage[:, :, :], 'st%d' % (T % 2), in_b=stage))
        for f in deferred:
            f()
        k.end_phase()

    def phase_o2(self, k, w1, w2, h_in, h_out):
        k.begin_phase()
        W1 = k.sbuf([128, 8, 4096], BF16, 'W1')
        W2 = k.sbuf([128, 32, 1024], BF16, 'W2')
        self.load_w(k, W1, w1, 8, 4096)
        self.load_w(k, W2, w2, 32, 1024)
        psF = k.psum([128, 4096], F32)
        f1 = [Buf(psF[:, i * 512:i * 512 + 256], 'f1_%d' % i) for i in range(4)]
        f2 = [Buf(psF[:, 2048 + i * 1024:3072 + i * 1024], 'f2_%d' % i) for i in range(2)]
        xTs = [k.sbuf([128, 8, 256], BF16, 'xTs') for _ in range(2)]
        uT = k.sbuf([128, 32, 256], BF16, 'uT')
        rl = [k.sbuf([128, 256], F32, 'rl') for _ in range(2)]
        hts = [k.sbuf([128, 1024], F32, 'ht') for _ in range(2)]
        h2s = [k.sbuf([128, 1024], F32, 'h2') for _ in range(2)]
        srcT = self.hnT[:, :, :].rearrange("c p t -> p c t")
        dbg = self.cfg.get('o2_dbg', 3)
        k.dma('sp', xTs[0][:, :, :], srcT[:, :, 0:256], 'ldx0', out_b=xTs[0])
        for T2 in range(16 if dbg > 1 else 0):
            if T2 + 1 < 16:
                k.dma('sp', xTs[(T2 + 1) % 2][:, :, :], srcT[:, :, (T2 + 1) * 256:(T2 + 2) * 256], 'ldx%d' % ((T2 + 1) % 2),
                      out_b=xTs[(T2 + 1) % 2])
            xs = xTs[T2 % 2]
            for fc in range(32):
                fb = f1[fc % 4]
                r = rl[fc % 2]
                for kc in range(8):
                    k.op('pe', lambda e, fc=fc, kc=kc, fb=fb: e.matmul(fb[:, :], lhsT=W1[:, kc, fc * 128:(fc + 1) * 128], rhs=xs[:, kc, :],
                                                                       start=(kc == 0), stop=(kc == 7)), reads=[W1, xs], writes=[fb])
                k.op('act', lambda e, fb=fb, r=r: e.activation(out=r[:, :], in_=fb[:, :], func=AF.Relu), reads=[fb], writes=[r])
                k.op('dve', lambda e, fb=fb, r=r, fc=fc: e.tensor_tensor(out=uT[:, fc, :], in0=fb[:, :], in1=r[:, :], op=ALU.mult),
                     reads=[fb, r], writes=[uT])
            for s in range(2 if dbg > 2 else 0):
                t = 2 * T2 + s
                ht, h2 = hts[t % 2], h2s[t % 2]
                ob = f2[t % 2]
                k.dma('sp', ht[:, :], h_in[t * 128:(t + 1) * 128, :], 'ldh%d' % (t % 2), out_b=ht)
                for half in range(2):
                    for fc in range(32):
                        k.op('pe', lambda e, half=half, fc=fc: e.matmul(
                            ob[:, half * 512:(half + 1) * 512], lhsT=uT[:, fc, s * 128:(s + 1) * 128],
                            rhs=W2[:, fc, half * 512:(half + 1) * 512], start=(fc == 0), stop=(fc == 31)), reads=[uT, W2], writes=[ob])
                for half in range(2):
                    hsl = slice(half * 512, (half + 1) * 512)
                    k.op('dve', lambda e, hsl=hsl: e.tensor_tensor(out=h2[:, hsl], in0=ob[:, hsl], in1=ht[:, hsl], op=ALU.add), reads=[ob, ht], writes=[h2])
                k.dma('sp', h_out[t * 128:(t + 1) * 128, :], h2[:, :], 'sth%d' % (t % 2), in_b=h2)
        k.end_phase()

    def phase_o3(self, k, wg, wp, p_in, h_in, h_out, ple_gain_row, next_gain_row):
        k.begin_phase()
        Wg = k.sbuf([128, 8, 1024], BF16, 'Wg')
        Wp = k.sbuf([128, 2, 1024], BF16, 'Wp')
        self.load_w(k, Wg, wg, 8, 1024)
        self.load_w(k, Wp, wp, 2, 1024)
        gple = k.sbuf([128, 1024], F32, 'gple')
        self.load_bc(k, gple, gple[:, :], ple_gain_row, 1024)
        gain = None
        if next_gain_row is not None:
            gain = k.sbuf([128, 1024], F32, 'gain')
            self.load_bc(k, gain, gain[:, :], next_gain_row, 1024)
        psF = k.psum([128, 2048], F32)
        psB = k.psum([128, 3072], BF16)
        gbh = [Buf(psF[:, i * 512:(i + 1) * 512], 'gb%d' % i) for i in range(2)]
        pbh = [Buf(psF[:, 1024 + i * 512:1536 + i * 512], 'pb%d' % i) for i in range(2)]
        psP = Buf(psB[:, 2048:2304], 'psP')
        nt = self.NormT(self, k, psB[:, 0:2048])
        xst = [k.sbuf([128, 8, 128], BF16, 'xst') for _ in range(2)]
        pts = [k.sbuf([128, 256], F32, 'pt') for _ in range(3)]
        pbf = [k.sbuf([128, 256], BF16, 'pbf') for _ in range(2)]
        pT = [k.sbuf([128, 2, 128], BF16, 'pT') for _ in range(2)]
        gs = [k.sbuf([128, 1024], F32, 'gs') for _ in range(2)]
        hts = [k.sbuf([128, 1024], F32, 'ht') for _ in range(3)]
        h3s = [k.sbuf([128, 1024], F32, 'h3') for _ in range(2)]
        stages = [k.sbuf([128, 8, 512], BF16, 'stg') for _ in range(2)] if gain is not None else None
        dstT = self.hnT[:, :, :].rearrange("c p t -> p c t")
        deferred = []

        def stageA1(t):
            i2, i3 = t % 2, t % 3
            k.dma('sp', pts[i3][:, :], p_in[t * 128:(t + 1) * 128, :], 'ldp%d' % i3, out_b=pts[i3])
            k.dma('sp', hts[i3][:, :], h_in[t * 128:(t + 1) * 128, :], 'ldh%d' % i3, out_b=hts[i3])
            ni = nt.run_a(hts[i3], hts[i3][:, :], gple)
            nt.run_b1(ni)
            k.op('pool', lambda e: e.tensor_copy(out=pbf[i2][:, :], in_=pts[i3][:, :]), reads=[pts[i3]], writes=[pbf[i2]])
            for c in range(2):
                k.op('pe', lambda e, c=c: e.transpose(out=psP[:, c * 128:(c + 1) * 128], in_=pbf[i2][:, c * 128:(c + 1) * 128],
                                                      identity=self.ident[:, :]), reads=[pbf[i2], self.ident], writes=[psP])
            return ni

        def stageA2(t, ni):
            i2 = t % 2
            nt.run_b2(ni, xst[i2], 0)
            k.op('act', lambda e: e.copy(out=pT[i2][:, :, :], in_=psP[:, :].rearrange("p (c t) -> p c t", c=2)), reads=[psP], writes=[pT[i2]])

        def stageM(t):
            i2 = t % 2
            xs = xst[i2]
            for half in range(2):
                for c in range(8):
                    k.op('pe', lambda e, half=half, c=c: e.matmul(
                        gbh[half][:, :], lhsT=xs[:, c, :],
                        rhs=Wg[:, c, half * 512:(half + 1) * 512], start=(c == 0), stop=(c == 7)), reads=[xs, Wg], writes=[gbh[half]])
                for c in range(2):
                    k.op('pe', lambda e, half=half, c=c: e.matmul(
                        pbh[half][:, :], lhsT=pT[i2][:, c, :],
                        rhs=Wp[:, c, half * 512:(half + 1) * 512], start=(c == 0), stop=(c == 1)), reads=[pT[i2], Wp], writes=[pbh[half]])

        def stageB(t):
            T, s = divmod(t, 4)
            i2, i3 = t % 2, t % 3
            for half in range(2):
                hsl = slice(half * 512, (half + 1) * 512)
                k.op('act', lambda e, hsl=hsl, half=half: e.activation(out=gs[i2][:, hsl], in_=gbh[half][:, :], func=AF.Sigmoid),
                     reads=[gbh[half]], writes=[gs[i2]])
                k.op('dve', lambda e, hsl=hsl, half=half: e.tensor_tensor(out=gs[i2][:, hsl], in0=pbh[half][:, :], in1=gs[i2][:, hsl], op=ALU.mult),
                     reads=[pbh[half], gs[i2]], writes=[gs[i2]])
            k.op('pool', lambda e: e.tensor_tensor(out=h3s[i2][:, :], in0=gs[i2][:, :], in1=hts[i3][:, :], op=ALU.add),
                 reads=[gs[i2], hts[i3]], writes=[h3s[i2]])
            k.dma('pool', h_out[t * 128:(t + 1) * 128, :], h3s[i2][:, :], 'sth%d' % i2, in_b=h3s[i2])

        def stageC(t):
            T, s = divmod(t, 4)
            i2 = t % 2
            if gain is not None:
                i = nt.run_a(h3s[i2], h3s[i2][:, :], gain)
                deferred.append(lambda i=i, T=T, s=s: nt.run_b(i, stages[T % 2], s * 128))
                if s == 3:
                    deferred.append(lambda T=T: k.dma('sp', dstT[:, :, T * 512:(T + 1) * 512], stages[T % 2][:, :, :], 'st%d' % (T % 2), in_b=stages[T % 2]))

        for i in range(33):
            if i >= 1:
                stageM(i - 1)
                for f in deferred:
                    f()
                del deferred[:]
            ni = stageA1(i) if i < 32 else None
            if i >= 1:
                stageB(i - 1)
            if i < 32:
                stageA2(i, ni)
            if i >= 1:
                stageC(i - 1)
        for f in deferred:
            f()
        k.end_phase()

    def phase_p1(self, k):
        I = self.I
        k.begin_phase()
        Win = k.sbuf([128, 8, 3072], BF16, 'Win')
        self.load_w(k, Win, I['o_w_in'][0], 8, 3072)
        g64 = k.sbuf([128, 2, 64], F32)
        for i, nm in enumerate(['o_qn', 'o_kn']):
            self.load_bc(k, g64, g64[:, i, :], I[nm][0, :], 64)
        gcol = k.sbuf([128, 2], F32, 'gcol')
        k.op('pool', lambda e: e.memset(gcol[:, :], 1.0), writes=[gcol])
        for i, nm in enumerate(['o_qn', 'o_kn']):
            k.dma('sp', gcol[0:64, i:i + 1], I[nm][0, :].rearrange("(p o) -> p o", o=1), 'cst', out_b=gcol)
        tokq = k.sbuf([128, 32, 4], F32); k.dma('sp', tokq[:, :, :], I['c_tokq'][:, :, :], 'cst', out_b=tokq)
        tokk = k.sbuf([128, 32, 4], F32); k.dma('sp', tokk[:, :, :], I['c_tokk'][:, :, :], 'cst', out_b=tokk)
        hq = k.sbuf([128, 16, 4], F32); self.load_bc(k, hq, hq[:, :, :].rearrange("p h c -> p (h c)"), I['c_hq1'][0, :], 64)
        hk = k.sbuf([128, 16, 4], F32); self.load_bc(k, hk, hk[:, :, :].rearrange("p h c -> p (h c)"), I['c_hk1'][0, :], 64)
        psF = k.psum([128, 3584], F32)
        psB = k.psum([128, 1024], BF16)
        bank = [Buf(psF[:, i * 512:(i + 1) * 512], 'bank%d' % i) for i in range(7)]
        psT = Buf(psB[:, :], 'psT')
        psKM = bank[6]
        psG = bank[6]
        R = 88
        hnTs = [k.sbuf([128, 8, 256], BF16, 'hnTs') for _ in range(2)]
        QTs = [k.sbuf([128, 16, 256], BF16, 'QTs') for _ in range(2)]
        KTs = [k.sbuf([128, 16, 256], BF16, 'KTs') for _ in range(2)]
        Vtm = [k.sbuf([128, 16, 128], BF16, 'Vtm') for _ in range(2)]
        Qtm = [k.sbuf([128, 16, 96], BF16, 'Qtm') for _ in range(2)]
        Ktm = [k.sbuf([128, 16, 96], BF16, 'Ktm') for _ in range(2)]
        for i in range(2):
            k.op('pool', lambda e, i=i: e.memset(Vtm[i][:, :, 64:128], 1.0), writes=[Vtm[i]])
            k.op('pool', lambda e, i=i: e.memset(Qtm[i][:, :, :], 0.0), writes=[Qtm[i]])
            k.op('pool', lambda e, i=i: e.memset(Ktm[i][:, :, :], 0.0), writes=[Ktm[i]])
            k.op('dve', lambda e, i=i: e.tensor_copy(out=Qtm[i][:, :, 80:84], in_=hq[:, :, :]), reads=[hq], writes=[Qtm[i]])
            k.op('dve', lambda e, i=i: e.tensor_copy(out=Ktm[i][:, :, 84:88], in_=hk[:, :, :]), reads=[hk], writes=[Ktm[i]])
        qs_l = [k.sbuf([128, 1024], F32, 'qs') for _ in range(2)]
        ks_l = [k.sbuf([128, 1024], F32, 'ks') for _ in range(2)]
        Kf = k.sbuf([128, 1024], F32, 'Kf')
        tmpA = k.sbuf([128, 1024], F32, 'tmpA')
        tmpB = k.sbuf([128, 1024], F32, 'tmpB')
        ssq = k.sbuf([128, 32], F32, 'ssq')
        rsq = k.sbuf([128, 32], F32, 'rsq')
        QgT = k.sbuf([64, 16, 128], BF16, 'QgT')
        kmA = k.sbuf([64, 16], F32, 'kmA')
        kmS = k.sbuf([64, 16], F32, 'kmS')
        kmH = k.sbuf([64, 16], F32, 'kmH')
        KmHi = k.sbuf([64, 16, 16], BF16, 'KmHi')
        KmLo = k.sbuf([64, 16, 16], BF16, 'KmLo')
        gate = k.sbuf([128, 16, 16], F32, 'gate')
        mx = k.sbuf([128, 16, 8], F32, 'mx')
        sel = k.sbuf([128, 16, 16], F32, 'sel')
        k.op('pool', lambda e: e.memset(gate[:, :, :], NEG), writes=[gate])
        k.op('pool', lambda e: e.memset(KmHi[:, :, :], 0.0), writes=[KmHi])
        k.op('pool', lambda e: e.memset(KmLo[:, :, :], 0.0), writes=[KmLo])

        srcT = self.hnT[:, :, :].rearrange("c p t -> p c t")
        QTd = self.QT[:, :, :].rearrange("h r t -> r h t")
        KTd = self.KT[:, :, :].rearrange("h r t -> r h t")
        Vd = self.V[:, :, :].rearrange("h t c -> t h c")

        k.dma('sp', hnTs[0][:, :, :], srcT[:, :, 0:256], 'ld0', out_b=hnTs[0])

        def stageA(t):
            T2, s = divmod(t, 2)
            hs = hnTs[T2 % 2]
            if s == 0 and T2 + 1 < 16:
                k.dma('sp', hnTs[(T2 + 1) % 2][:, :, :], srcT[:, :, (T2 + 1) * 256:(T2 + 2) * 256], 'ld%d' % ((T2 + 1) % 2),
                      out_b=hnTs[(T2 + 1) % 2])
            vt = Vtm[t % 2]
            qs, ks = qs_l[t % 2], ks_l[t % 2]
            for bk in range(6):
                for kc in range(8):
                    k.op('pe', lambda e, bk=bk, kc=kc: e.matmul(
                        bank[bk][:, :], lhsT=hs[:, kc, s * 128:(s + 1) * 128], rhs=Win[:, kc, bk * 512:(bk + 1) * 512],
                        start=(kc == 0), stop=(kc == 7)), reads=[hs, Win], writes=[bank[bk]])
            k.op('act', lambda e: e.copy(out=qs[:, 0:512], in_=bank[0][:, :]), reads=[bank[0]], writes=[qs])
            k.op('act', lambda e: e.copy(out=qs[:, 512:1024], in_=bank[1][:, :]), reads=[bank[1]], writes=[qs])
            k.op('act', lambda e: e.copy(out=ks[:, 0:512], in_=bank[2][:, :]), reads=[bank[2]], writes=[ks])
            k.op('act', lambda e: e.copy(out=ks[:, 512:1024], in_=bank[3][:, :]), reads=[bank[3]], writes=[ks])
            k.op('act', lambda e: e.copy(out=vt[:, 0:8, 0:64], in_=bank[4][:, :].rearrange("p (h d) -> p h d", h=8)), reads=[bank[4]], writes=[vt])
            k.op('act', lambda e: e.copy(out=vt[:, 8:16, 0:64], in_=bank[5][:, :].rearrange("p (h d) -> p h d", h=8)), reads=[bank[5]], writes=[vt])

        def stageBf(t):
            T2, s = divmod(t, 2)
            own = T2
            qts, kts = QTs[T2 % 2], KTs[T2 % 2]
            vt, qt, kt = Vtm[t % 2], Qtm[t % 2], Ktm[t % 2]
            qs, ks = qs_l[t % 2], ks_l[t % 2]
            q3 = qs[:, :].rearrange("p (h d) -> p h d", h=16)
            k3 = ks[:, :].rearrange("p (h d) -> p h d", h=16)
            kf3 = Kf[:, :].rearrange("p (h d) -> p h d", h=16)
            tA3 = tmpA[:, :].rearrange("p (h d) -> p h d", h=16)
            tB3 = tmpB[:, :].rearrange("p (h d) -> p h d", h=16)
            k.op('dve', lambda e: e.tensor_tensor(out=tA3, in0=q3, in1=q3, op=ALU.mult), reads=[qs], writes=[tmpA])
            k.op('dve', lambda e: e.tensor_reduce(out=ssq[:, 0:16], in_=tA3, axis=AX.X, op=ALU.add), reads=[tmpA], writes=[ssq])
            k.op('pool', lambda e: e.tensor_tensor(out=tB3, in0=k3, in1=k3, op=ALU.mult), reads=[ks], writes=[tmpB])
            k.op('dve', lambda e: e.tensor_reduce(out=ssq[:, 16:32], in_=tB3, axis=AX.X, op=ALU.add), reads=[tmpB], writes=[ssq])
            self.rstd(k, ssq, ssq[:, :], rsq, rsq[:, :], 64)
            k.op('dve', lambda e: e.tensor_tensor(out=qt[:, :, 0:64], in0=q3, in1=rsq[:, 0:16].unsqueeze(2).to_broadcast([128, 16, 64]), op=ALU.mult),
                 reads=[qs, rsq], writes=[qt])
            k.op('pool', lambda e: e.tensor_tensor(out=kt[:, :, 0:64], in0=k3, in1=rsq[:, 16:32].unsqueeze(2).to_broadcast([128, 16, 64]), op=ALU.mult),
                 reads=[ks, rsq], writes=[kt])
            k.op('pool', lambda e: e.memset(kt[:, :, 64:80], 0.0), writes=[kt])
            k.op('pool', lambda e: e.memset(kt[:, :, 64 + own:65 + own], 1.0), writes=[kt])
            k.op('pool', lambda e: e.tensor_copy(out=kt[:, :, 80:84], in_=tokk[:, t, :].unsqueeze(1).to_broadcast([128, 16, 4])),
                 reads=[tokk], writes=[kt])
            k.op('pool', lambda e: e.tensor_copy(out=qt[:, :, 84:88], in_=tokq[:, t, :].unsqueeze(1).to_broadcast([128, 16, 4])),
                 reads=[tokq], writes=[qt])

        def stageBb(t):
            T2, s = divmod(t, 2)
            own = T2
            qts, kts = QTs[T2 % 2], KTs[T2 % 2]
            vt, qt, kt = Vtm[t % 2], Qtm[t % 2], Ktm[t % 2]
            qs, ks = qs_l[t % 2], ks_l[t % 2]
            if own == 0:
                k.op('pool', lambda e: e.memset(qt[:, :, 64:80], 0.0), writes=[qt])
            else:
                for r in range(2):
                    for hh in range(8):
                        k.op('pe', lambda e, r=r, hh=hh: e.transpose(out=psT[0:64, hh * 128:(hh + 1) * 128], in_=qt[:, r * 8 + hh, 0:64],
                                                                     identity=self.ident[:, :]), reads=[qt, self.ident], writes=[psT])
                    k.op('act', lambda e, r=r: e.mul(out=QgT[0:64, r * 8:(r + 1) * 8, :], in_=psT[0:64, :].rearrange("p (h t) -> p h t", h=8),
                                                     mul=gcol[0:64, 0:1]), reads=[psT, gcol], writes=[QgT])
                for hh in range(16):
                    k.op('pe', lambda e, hh=hh: e.matmul(psG[:, 256 + hh * 16:256 + hh * 16 + own], lhsT=QgT[0:64, hh, :], rhs=KmHi[0:64, hh, 0:own],
                                                         start=True, stop=False), reads=[QgT, KmHi], writes=[psG])
                    k.op('pe', lambda e, hh=hh: e.matmul(psG[:, 256 + hh * 16:256 + hh * 16 + own], lhsT=QgT[0:64, hh, :], rhs=KmLo[0:64, hh, 0:own],
                                                         start=False, stop=True), reads=[QgT, KmLo], writes=[psG])
                k.op('dve', lambda e: e.tensor_copy(out=gate[:, :, 0:own], in_=psG[:, 256:512].rearrange("p (h n) -> p h n", h=16)[:, :, 0:own]),
                     reads=[psG], writes=[gate])
                for hh in range(16):
                    k.op('dve', lambda e, hh=hh: e.max(out=mx[:, hh, :], in_=gate[:, hh, :]), reads=[gate], writes=[mx])
                k.op('dve', lambda e: e.tensor_tensor(out=sel[:, :, :], in0=gate[:, :, :], in1=mx[:, :, 2:3].to_broadcast([128, 16, 16]), op=ALU.is_ge),
                     reads=[gate, mx], writes=[sel])
                k.op('dve', lambda e: e.tensor_scalar(out=qt[:, :, 64:80], in0=sel[:, :, :], scalar1=1.0, scalar2=BIG, op0=ALU.subtract, op1=ALU.mult),
                     reads=[sel], writes=[qt])
                k.op('pool', lambda e: e.memset(qt[:, :, 64 + own:65 + own], 0.0), writes=[qt])
            for hh in range(16):
                k.op('pe', lambda e, hh=hh: e.matmul(psKM[0:64, hh:hh + 1], lhsT=kt[:, hh, 0:64], rhs=self.onesb[:, 0:1],
                                                     start=True, stop=True), reads=[kt, self.onesb], writes=[psKM])
            if s == 0:
                k.op('act', lambda e: e.copy(out=kmA[:, :], in_=psKM[0:64, 0:16]), reads=[psKM], writes=[kmA])
            else:
                k.op('dve', lambda e: e.tensor_tensor(out=kmS[:, :], in0=psKM[0:64, 0:16], in1=kmA[:, :], op=ALU.add), reads=[psKM, kmA], writes=[kmS])
                k.op('dve', lambda e: e.tensor_scalar(out=kmS[:, :], in0=kmS[:, :], scalar1=1.0 / 256, scalar2=gcol[0:64, 1:2], op0=ALU.mult, op1=ALU.mult),
                     reads=[kmS, gcol], writes=[kmS])
                k.op('dve', lambda e: e.tensor_copy(out=KmHi[:, :, own], in_=kmS[:, :]), reads=[kmS], writes=[KmHi])
                k.op('dve', lambda e: e.tensor_copy(out=kmH[:, :], in_=KmHi[:, :, own]), reads=[KmHi], writes=[kmH])
                k.op('dve', lambda e: e.tensor_tensor(out=KmLo[:, :, own], in0=kmS[:, :], in1=kmH[:, :], op=ALU.subtract), reads=[kmS, kmH], writes=[KmLo])
            for (src, dst, gc) in ((qt, qts, 0), (kt, kts, 1)):
                for r in range(2):
                    for hh in range(8):
                        k.op('pe', lambda e, src=src, r=r, hh=hh: e.transpose(
                            out=psT[0:R, hh * 128:(hh + 1) * 128], in_=src[:, r * 8 + hh, 0:R], identity=self.ident[:, :]),
                            reads=[src, self.ident], writes=[psT])
                    k.op('act', lambda e, dst=dst, r=r, gc=gc: e.mul(
                        out=dst[0:R, r * 8:(r + 1) * 8, s * 128:(s + 1) * 128], in_=psT[0:R, :].rearrange("p (h t) -> p h t", h=8),
                        mul=gcol[0:R, gc:gc + 1]), reads=[psT, gcol], writes=[dst])
            k.dma('sp', Vd[t * 128:(t + 1) * 128, :, :], vt[:, :, :], 'stv%d' % (t % 2), in_b=vt)
            if s == 1:
                c0 = T2 * 256
                k.dma('sp', QTd[0:R, :, c0:c0 + 256], qts[0:R, :, :], 'stq%d' % (T2 % 2), in_b=qts)
                k.dma('sp', KTd[0:R, :, c0:c0 + 256], kts[0:R, :, :], 'stk%d' % (T2 % 2), in_b=kts)

        for i in range(33):
            if i >= 1:
                stageBf(i - 1)
            if i < 32:
                stageA(i)
            if i >= 1:
                stageBb(i - 1)
        k.end_phase()

    def build(self):
        nc, I = self.nc, self.I
        phases = self.cfg.get('phases', None)

        def on(name):
            return phases is None or name in phases

        with ExitStack() as es:
            es.enter_context(nc.Block())
            k = KB(nc, es)
            self.k = k
            self.setup_globals(k)
            heads0 = [dict(R=96, scale=96 ** -0.5, mask='c')] * 8 + [dict(R=72, scale=0.125, mask='d')] * 8
            heads1 = [dict(R=88, scale=0.125, mask='c')] * 16
            if on('n0'):
                self.phase_norm0(k)
            if on('p0'):
                self.phase_p0(k)
            if on('a0'):
                self.phase_attn(k, heads0)
            if on('o1_0'):
                self.phase_o1(k, I['e_w_out'][0], I['x'], self.hA, I['ff_norm'][0, :])
            if on('o2_0'):
                self.phase_o2(k, I['w_ff1'][0], I['w_ff2'][0], self.hA, self.hB)
            if on('o3_0'):
                self.phase_o3(k, I['w_ple_gate'][0], I['w_ple_proj'][0], I['p'][0], self.hB, self.hC, I['ple_norm'][0, :], I['mix_norm'][1, :])
            if on('p1'):
                self.phase_p1(k)
            if on('a1'):
                self.phase_attn(k, heads1)
            if on('o1_1'):
                self.phase_o1(k, I['o_w_out'][0], self.hC, self.hA, I['ff_norm'][1, :])
            if on('o2_1'):
                self.phase_o2(k, I['w_ff1'][1], I['w_ff2'][1], self.hA, self.hB)
            if on('o3_1'):
                self.phase_o3(k, I['w_ple_gate'][1], I['w_ple_proj'][1], I['p'][1], self.hB, self.y, I['ple_norm'][1, :], None)
            k.barrier()
        return nc


def make_in_maps(inputs, consts, n_cores=8, extra=None):
    maps = []
    shared = {}
    for name in IN_SHAPES:
        if name in ('x', 'p', 'positions'):
            continue
        shared[name] = np.ascontiguousarray(np.asarray(inputs[name], dtype=np.float32))
    shared.update(consts)
    for c in range(n_cores):
        m = dict(shared)
        m['x'] = np.ascontiguousarray(np.asarray(inputs['x'][c], dtype=np.float32))
        m['p'] = np.ascontiguousarray(np.asarray(inputs['p'][:, c], dtype=np.float32))
        pos = np.asarray(inputs['positions'][c], dtype=np.int32)
        m['positions'] = np.ascontiguousarray(pos.reshape(32, 128).T)
        if extra:
            m.update(extra[c])
        maps.append(m)
    return maps


def kernel(**inputs):
    prog = Prog()
    nc = prog.build()
    maps = make_in_maps(inputs, host_consts())
    res = run_bass_kernel_spmd(nc, maps, core_ids=list(range(8)))
    out = np.stack([np.asarray(r["y"], dtype=np.float32) for r in res.results], axis=0)
    return out
```
